# Optimizing a Trainium2 kernel written in Bass

```python
import math
import jax, jax.numpy as jnp
from jax import lax
import numpy as np

D_MODEL = 1024
BATCH = 4
SEQ = 8192
DEPTH = 2

GRID_W = 64
CTX_LEN = 256
N_EVEN = (DEPTH + 1) // 2
N_ODD = DEPTH // 2
N_MOD = 6

HY_WIDTH = 512
HY_SHORT_CONV = 3
HY_EMB = 33
HY_BANDS = (HY_EMB - 1) // 2
HY_FFN = 64
HY_TARGET = 1e-2
HY_FAST_DECAY = 0.3
HY_SLOW_DECAY = 1.5
HY_DECAY_MIN = math.log(HY_TARGET) / HY_SLOW_DECAY
HY_DECAY_MAX = math.log(HY_TARGET) / HY_FAST_DECAY

MLA_HEADS = 8
MLA_NOPE = 64
MLA_ROPE = 32
MLA_V = 64
MLA_Q_RANK = 256
MLA_KV_RANK = 128
MLA_SCALE = (MLA_NOPE + MLA_ROPE) ** -0.5
Q_BLOCK = 128
ROPE_THETA = 10000.0

OFF_Q = 3 * HY_WIDTH
OFF_KV = OFF_Q + MLA_Q_RANK
OFF_KPE = OFF_KV + MLA_KV_RANK
IN_A = OFF_KPE + MLA_ROPE
MIX_A_OUT = HY_WIDTH + MLA_HEADS * MLA_V

SSD_INNER = 2 * D_MODEL
SSD_HEADDIM = 64
SSD_HEADS = SSD_INNER // SSD_HEADDIM
SSD_GROUPS = 4
SSD_STATE = 128
SSD_CONV = 3
SSD_CHUNK = 128
SSD_XBC = SSD_INNER + 2 * SSD_GROUPS * SSD_STATE
SSD_IN = SSD_INNER + SSD_XBC + 2 * SSD_HEADS

N_EXPERTS = 256
TOP_K = 8
N_EXPERT_GROUPS = 8
TOPK_GROUPS = 4
EXPERT_DIM = 256
SHARED_DIM = 256
ROUTED_SCALE = 2.5
MOE_BLOCK = 128

DN_ALPHA = (2 * DEPTH) ** 0.25
DN_BETA = (8 * DEPTH) ** -0.25
LN_EPS = 1e-5
RMS_EPS = 1e-6

kernel_name = "hybrid_hyena_mla_ssd_moe_diffusion"


def layer_norm(x, g, b):
    xf = x.astype(jnp.float32)
    mu = jnp.mean(xf, -1, keepdims=True)
    var = jnp.mean(jnp.square(xf - mu), -1, keepdims=True)
    return ((xf - mu) * lax.rsqrt(var + LN_EPS) * g.astype(jnp.float32) + b.astype(jnp.float32)).astype(x.dtype)


def rms_norm(x, g):
    xf = x.astype(jnp.float32)
    return (xf * lax.rsqrt(jnp.mean(xf * xf, -1, keepdims=True) + RMS_EPS) * g.astype(jnp.float32)).astype(x.dtype)


def modulate(x, shift, scale):
    return x * (1 + scale) + shift


def dwconv_centered(x, w, b):
    pad = w.shape[0] // 2
    y = lax.conv_general_dilated(x, w[:, None, :], window_strides=(1,), padding=[(pad, pad)],
                                 dimension_numbers=('NWC', 'WIO', 'NWC'), feature_group_count=x.shape[-1])
    return y + b


def swiglu(x, w_gu, w_down):
    gu = x @ w_gu
    g, u = jnp.split(gu, 2, axis=-1)
    return (jax.nn.silu(g) * u) @ w_down


def axial_rope_tables(n_tokens):
    rows = n_tokens // GRID_W
    row = jnp.repeat(jnp.arange(rows), GRID_W).astype(jnp.float32)
    col = jnp.tile(jnp.arange(GRID_W), rows).astype(jnp.float32)
    half = MLA_ROPE // 2
    inv = ROPE_THETA ** (-jnp.arange(0, half, 2, dtype=jnp.float32) / half)
    ang = jnp.concatenate([row[:, None] * inv, col[:, None] * inv], -1)
    return jnp.cos(ang), jnp.sin(ang)


def apply_rope(x, cos, sin):
    xf = x.astype(jnp.float32)
    x1, x2 = xf[..., 0::2], xf[..., 1::2]
    out = jnp.stack([x1 * cos - x2 * sin, x1 * sin + x2 * cos], -1).reshape(x.shape)
    return out.astype(x.dtype)


def hyena_filters(n, w1, b1, f1, w2, b2, f2, w3):
    f32 = jnp.float32
    t = jnp.linspace(0.0, 1.0, n, dtype=f32)[:, None]
    w = 2.0 * math.pi * jnp.arange(n, dtype=f32)[:, None] / n
    fr = jnp.linspace(1e-4, HY_BANDS - 1, HY_BANDS, dtype=f32)
    z = jnp.concatenate([t, jnp.cos(fr * w), -jnp.sin(fr * w)], -1)
    h = jnp.sin(f1.astype(f32) * (z @ w1.astype(f32) + b1.astype(f32)))
    h = jnp.sin(f2.astype(f32) * (h @ w2.astype(f32) + b2.astype(f32)))
    h = h @ w3.astype(f32)
    deltas = jnp.abs(jnp.linspace(HY_DECAY_MIN, HY_DECAY_MAX, HY_WIDTH, dtype=f32))
    decay = jnp.exp(-t * deltas)
    h_f = h[:, :HY_WIDTH] * decay
    h_b = h[:, HY_WIDTH:] * decay
    k = jnp.concatenate([h_f, jnp.zeros((1, HY_WIDTH), f32), h_b[:0:-1]], 0)
    return k / jnp.sum(jnp.abs(k), 0, keepdims=True)


def hyena_sequence(proj, conv_w, conv_b, filt, skip):
    n = proj.shape[1]
    u = dwconv_centered(proj, conv_w, conv_b)
    x0, x1, v = jnp.split(u, 3, axis=-1)
    z = (v * x1).astype(jnp.float32)
    k = hyena_filters(n, *filt)
    y = jnp.fft.irfft(jnp.fft.rfft(z, n=2 * n, axis=1) * jnp.fft.rfft(k, n=2 * n, axis=0)[None],
                      n=2 * n, axis=1)[:, :n]
    y = y + z * skip.astype(jnp.float32)
    return (y * x0.astype(jnp.float32)).astype(proj.dtype)


def mla_attend(q_nope, q_pe, k_nope, k_pe, v):
    s = (jnp.einsum('bhqd,bhkd->bhqk', q_nope, k_nope, preferred_element_type=jnp.float32)
         + jnp.einsum('bhqd,bkd->bhqk', q_pe, k_pe, preferred_element_type=jnp.float32)) * MLA_SCALE
    p = jax.nn.softmax(s, axis=-1).astype(v.dtype)
    return jnp.einsum('bhqk,bhkd->bhqd', p, v)


def attend_blocks(q_nope, q_pe, k_nope, k_pe, v):
    bsz, h, n, _ = q_nope.shape
    nb = n // Q_BLOCK

    def to_blocks(t):
        return jnp.moveaxis(t.reshape(bsz, h, nb, Q_BLOCK, t.shape[-1]), 2, 0)

    out = lax.map(lambda qb: mla_attend(qb[0], qb[1], k_nope, k_pe, v), (to_blocks(q_nope), to_blocks(q_pe)))
    return jnp.moveaxis(out, 0, 2).reshape(bsz, h, n, MLA_V)


def mixer_hyena_mla(h_lat, h_ctx, p, with_ctx_out):
    filt = (p['filt_w1'], p['filt_b1'], p['filt_freq1'], p['filt_w2'], p['filt_b2'], p['filt_freq2'], p['filt_w3'])

    def project(h):
        bsz, n = h.shape[:2]
        u = h @ p['w_in']
        q = (rms_norm(u[..., OFF_Q:OFF_KV], p['q_norm']) @ p['w_qb']).reshape(bsz, n, MLA_HEADS, -1).transpose(0, 2, 1, 3)
        kv = (rms_norm(u[..., OFF_KV:OFF_KPE], p['kv_norm']) @ p['w_kvb']).reshape(bsz, n, MLA_HEADS, -1).transpose(0, 2, 1, 3)
        return (u[..., :OFF_Q], q[..., :MLA_NOPE], q[..., MLA_NOPE:], kv[..., :MLA_NOPE], kv[..., MLA_NOPE:],
                u[..., OFF_KPE:])

    def merge(hy_out, att):
        bsz, _, n, _ = att.shape
        return jnp.concatenate([hy_out, att.transpose(0, 2, 1, 3).reshape(bsz, n, MLA_HEADS * MLA_V)], -1) @ p['w_out']

    hy_l, qn_l, qp_l, kn_l, v_l, kp_l = project(h_lat)
    hy_c, qn_c, qp_c, kn_c, v_c, kp_c = project(h_ctx)
    cos, sin = axial_rope_tables(h_lat.shape[1])
    qp_l = apply_rope(qp_l, cos, sin)
    kp_l = apply_rope(kp_l, cos, sin)
    kn = jnp.concatenate([kn_c, kn_l], 2)
    kp = jnp.concatenate([kp_c, kp_l], 1)
    vv = jnp.concatenate([v_c, v_l], 2)
    att_l = attend_blocks(qn_l, qp_l, kn, kp, vv)
    hyo_l = hyena_sequence(hy_l, p['conv_w'], p['conv_b'], filt, p['skip'])
    y_lat = merge(hyo_l, att_l)
    y_ctx = None
    if with_ctx_out:
        att_c = mla_attend(qn_c, qp_c, kn_c, kp_c, v_c)
        hyo_c = hyena_sequence(hy_c, p['conv_w'], p['conv_b'], filt, p['skip'])
        y_ctx = merge(hyo_c, att_c)
    return y_lat, y_ctx


def ssd_scan(x, dt, a_coef, bm, cm, s0):
    f32 = jnp.float32
    bsz, n, h, pd = x.shape
    g, ns = bm.shape[2], bm.shape[3]
    r = h // g
    nc = n // SSD_CHUNK
    a = dt.astype(f32) * a_coef.astype(f32)
    xdt = x.astype(f32) * dt.astype(f32)[..., None]

    def chunks(t):
        return jnp.moveaxis(t.reshape((bsz, nc, SSD_CHUNK) + t.shape[2:]), 1, 0)

    xs = (chunks(xdt.reshape(bsz, n, g, r, pd)), chunks(a.reshape(bsz, n, g, r)),
          chunks(bm.astype(f32)), chunks(cm.astype(f32)))
    tri = jnp.tril(jnp.ones((SSD_CHUNK, SSD_CHUNK), bool))

    def step(s, inp):
        xc, ac, bc, cc = inp
        acs = jnp.cumsum(ac, axis=1)
        acs_t = jnp.transpose(acs, (0, 2, 3, 1))
        seg = acs_t[..., :, None] - acs_t[..., None, :]
        decay = jnp.exp(jnp.where(tri, seg, -jnp.inf))
        cb = jnp.einsum('btgn,bsgn->bgts', cc, bc)
        y_diag = jnp.einsum('bgrts,bsgrp->btgrp', cb[:, :, None] * decay, xc)
        y_off = jnp.einsum('btgn,bgrpn->btgrp', cc, s) * jnp.exp(acs)[..., None]
        to_end = jnp.exp(acs[:, -1:] - acs)
        s_new = (jnp.exp(acs[:, -1])[..., None, None] * s
                 + jnp.einsum('bsgn,bsgrp->bgrpn', bc, xc * to_end[..., None]))
        return s_new, y_diag + y_off

    s_fin, ys = lax.scan(step, s0.astype(f32).reshape(bsz, g, r, pd, ns), xs)
    y = jnp.moveaxis(ys, 0, 1).reshape(bsz, n, h, pd)
    return y.astype(x.dtype), s_fin.reshape(bsz, h, pd, ns)


def mixer_ssd(h_lat, h_ctx, p, with_ctx_out):
    a_f = -jnp.exp(p['a_log_f'].astype(jnp.float32))
    a_b = -jnp.exp(p['a_log_b'].astype(jnp.float32))

    def pre(h):
        bsz, n = h.shape[:2]
        u = h @ p['w_in']
        z = u[..., :SSD_INNER]
        xbc = jax.nn.silu(dwconv_centered(u[..., SSD_INNER:SSD_INNER + SSD_XBC], p['conv_w'], p['conv_b']))
        dt = u[..., SSD_INNER + SSD_XBC:].astype(jnp.float32)
        xs = xbc[..., :SSD_INNER].reshape(bsz, n, SSD_HEADS, SSD_HEADDIM)
        gn = SSD_GROUPS * SSD_STATE
        bm = xbc[..., SSD_INNER:SSD_INNER + gn].reshape(bsz, n, SSD_GROUPS, SSD_STATE)
        cm = xbc[..., SSD_INNER + gn:].reshape(bsz, n, SSD_GROUPS, SSD_STATE)
        dt_f = jax.nn.softplus(dt[..., :SSD_HEADS] + p['dt_bias_f'].astype(jnp.float32))
        dt_b = jax.nn.softplus(dt[..., SSD_HEADS:] + p['dt_bias_b'].astype(jnp.float32))
        return z, xs, bm, cm, dt_f, dt_b

    def flip(t):
        return jnp.flip(t, axis=1)

    def post(y_f, y_b, xs, z):
        bsz, n = z.shape[:2]
        y = y_f + y_b + xs * p['d'][:, None]
        y = y.reshape(bsz, n, SSD_INNER) * jax.nn.silu(z)
        y = rms_norm(y.reshape(bsz, n, SSD_GROUPS, SSD_INNER // SSD_GROUPS),
                     p['norm_g'].reshape(SSD_GROUPS, -1)).reshape(bsz, n, SSD_INNER)
        return y @ p['w_out']

    zc, xc, bc, cc, dfc, dbc = pre(h_ctx)
    zl, xl, bl, cl, dfl, dbl = pre(h_lat)
    s0 = jnp.zeros((h_ctx.shape[0], SSD_HEADS, SSD_HEADDIM, SSD_STATE), jnp.float32)
    yc_f, sc_f = ssd_scan(xc, dfc, a_f, bc, cc, s0)
    yc_b, sc_b = ssd_scan(flip(xc), flip(dbc), a_b, flip(bc), flip(cc), s0)
    yl_f, _ = ssd_scan(xl, dfl, a_f, bl, cl, sc_f)
    yl_b, _ = ssd_scan(flip(xl), flip(dbl), a_b, flip(bl), flip(cl), sc_b)
    y_lat = post(yl_f, flip(yl_b), xl, zl)
    y_ctx = post(yc_f, flip(yc_b), xc, zc) if with_ctx_out else None
    return y_lat, y_ctx


def routed_experts(xf, idx, w, w_gu, w_down):
    t = xf.shape[0]
    tk = t * TOP_K
    n_blocks = -(-tk // MOE_BLOCK) + N_EXPERTS
    cap = n_blocks * MOE_BLOCK
    flat_e = idx.reshape(-1)
    order = jnp.argsort(flat_e)
    sorted_e = flat_e[order]
    counts = jnp.bincount(flat_e, length=N_EXPERTS)
    padded = (counts + MOE_BLOCK - 1) // MOE_BLOCK * MOE_BLOCK
    pad_end = jnp.cumsum(padded)
    pad_start = pad_end - padded
    start = jnp.cumsum(counts) - counts
    dest = pad_start[sorted_e] + jnp.arange(tk) - start[sorted_e]
    row_token = jnp.full((cap,), t, jnp.int32).at[dest].set((order // TOP_K).astype(jnp.int32))
    row_w = jnp.zeros((cap,), jnp.float32).at[dest].set(w.reshape(-1)[order])
    block_e = jnp.minimum(jnp.searchsorted(pad_end, jnp.arange(n_blocks) * MOE_BLOCK, side='right'), N_EXPERTS - 1)
    xpad = jnp.concatenate([xf, jnp.zeros((1, xf.shape[1]), xf.dtype)], 0)
    xb = xpad[row_token].reshape(n_blocks, MOE_BLOCK, xf.shape[1])
    yb = lax.map(lambda a: swiglu(a[0], w_gu[a[1]], w_down[a[1]]), (xb, block_e)).reshape(cap, -1)
    return jax.ops.segment_sum(yb * row_w[:, None].astype(yb.dtype), row_token, num_segments=t + 1)[:t]


def moe(xf, router_w, router_bias, w_gu, w_down, sh_gu, sh_down):
    t = xf.shape[0]
    scores = jax.nn.sigmoid(jnp.dot(xf, router_w, preferred_element_type=jnp.float32))
    choice = scores + router_bias.astype(jnp.float32)
    grp_score = lax.top_k(choice.reshape(t, N_EXPERT_GROUPS, -1), 2)[0].sum(-1)
    _, top_g = lax.top_k(grp_score, TOPK_GROUPS)
    gmask = jax.nn.one_hot(top_g, N_EXPERT_GROUPS, dtype=jnp.float32).sum(1) > 0
    masked = jnp.where(jnp.repeat(gmask, N_EXPERTS // N_EXPERT_GROUPS, axis=1), choice, -jnp.inf)
    _, idx = lax.top_k(masked, TOP_K)
    w = jnp.take_along_axis(scores, idx, axis=1)
    w = w / jnp.sum(w, -1, keepdims=True) * ROUTED_SCALE
    return routed_experts(xf, idx, w, w_gu, w_down) + swiglu(xf, sh_gu, sh_down)


def setup_inputs(seed: int = 0) -> dict:
    key = jax.random.key(seed)
    ks = iter(jax.random.split(key, 64))
    f32 = jnp.float32

    def nrm(shape, scale=1.0):
        return jax.random.normal(next(ks), shape, f32) * scale

    def gain(shape):
        return 1.0 + 0.01 * nrm(shape)

    def dt_bias(shape):
        u = jax.random.uniform(next(ks), shape, f32)
        dt = jnp.exp(u * (math.log(0.1) - math.log(1e-3)) + math.log(1e-3))
        return dt + jnp.log(-jnp.expm1(-dt))

    def a_log(shape):
        return jnp.log(jax.random.uniform(next(ks), shape, f32, 1.0, 16.0))

    D = D_MODEL
    return {
        "x": nrm((BATCH, SEQ, D)),
        "c": nrm((BATCH, D)),
        "ctx": nrm((BATCH, CTX_LEN, D)),
        "c_ctx": nrm((D,)),
        "mod_w": nrm((DEPTH, D, N_MOD * D), 0.5 * D ** -0.5),
        "mod_b": nrm((DEPTH, N_MOD * D), 0.01),
        "ln_mix_g": gain((DEPTH, D)),
        "ln_mix_b": nrm((DEPTH, D), 0.01),
        "ln_ffn_g": gain((DEPTH, D)),
        "ln_ffn_b": nrm((DEPTH, D), 0.01),
        "a_w_in": nrm((N_EVEN, D, IN_A), D ** -0.5),
        "hy_conv_w": nrm((N_EVEN, HY_SHORT_CONV, 3 * HY_WIDTH), HY_SHORT_CONV ** -0.5),
        "hy_conv_b": nrm((N_EVEN, 3 * HY_WIDTH), 0.01),
        "hy_filt_w1": nrm((N_EVEN, HY_EMB, HY_FFN), HY_EMB ** -0.5),
        "hy_filt_b1": nrm((N_EVEN, HY_FFN), 0.01),
        "hy_filt_freq1": gain((N_EVEN, HY_FFN)),
        "hy_filt_w2": nrm((N_EVEN, HY_FFN, HY_FFN), HY_FFN ** -0.5),
        "hy_filt_b2": nrm((N_EVEN, HY_FFN), 0.01),
        "hy_filt_freq2": gain((N_EVEN, HY_FFN)),
        "hy_filt_w3": nrm((N_EVEN, HY_FFN, 2 * HY_WIDTH), HY_FFN ** -0.5),
        "hy_skip": nrm((N_EVEN, HY_WIDTH), 0.5),
        "mla_q_norm": gain((N_EVEN, MLA_Q_RANK)),
        "mla_w_qb": nrm((N_EVEN, MLA_Q_RANK, MLA_HEADS * (MLA_NOPE + MLA_ROPE)), MLA_Q_RANK ** -0.5),
        "mla_kv_norm": gain((N_EVEN, MLA_KV_RANK)),
        "mla_w_kvb": nrm((N_EVEN, MLA_KV_RANK, MLA_HEADS * (MLA_NOPE + MLA_V)), MLA_KV_RANK ** -0.5),
        "a_w_out": nrm((N_EVEN, MIX_A_OUT, D), DN_BETA * MIX_A_OUT ** -0.5),
        "ssd_w_in": nrm((N_ODD, D, SSD_IN), D ** -0.5),
        "ssd_conv_w": nrm((N_ODD, SSD_CONV, SSD_XBC), SSD_CONV ** -0.5),
        "ssd_conv_b": nrm((N_ODD, SSD_XBC), 0.01),
        "ssd_dt_bias_f": dt_bias((N_ODD, SSD_HEADS)),
        "ssd_dt_bias_b": dt_bias((N_ODD, SSD_HEADS)),
        "ssd_a_log_f": a_log((N_ODD, SSD_HEADS)),
        "ssd_a_log_b": a_log((N_ODD, SSD_HEADS)),
        "ssd_d": gain((N_ODD, SSD_HEADS)),
        "ssd_norm_g": gain((N_ODD, SSD_INNER)),
        "ssd_w_out": nrm((N_ODD, SSD_INNER, D), DN_BETA * SSD_INNER ** -0.5),
        "router_w": nrm((DEPTH, D, N_EXPERTS), D ** -0.5),
        "router_bias": nrm((DEPTH, N_EXPERTS), 0.01),
        "exp_w_gu": nrm((DEPTH, N_EXPERTS, D, 2 * EXPERT_DIM), D ** -0.5),
        "exp_w_down": nrm((DEPTH, N_EXPERTS, EXPERT_DIM, D), DN_BETA * EXPERT_DIM ** -0.5),
        "sh_w_gu": nrm((DEPTH, D, 2 * SHARED_DIM), D ** -0.5),
        "sh_w_down": nrm((DEPTH, SHARED_DIM, D), DN_BETA * SHARED_DIM ** -0.5),
    }


def reference(x, c, ctx, c_ctx, mod_w, mod_b, ln_mix_g, ln_mix_b, ln_ffn_g, ln_ffn_b,
              a_w_in, hy_conv_w, hy_conv_b, hy_filt_w1, hy_filt_b1, hy_filt_freq1, hy_filt_w2, hy_filt_b2,
              hy_filt_freq2, hy_filt_w3, hy_skip, mla_q_norm, mla_w_qb, mla_kv_norm, mla_w_kvb, a_w_out,
              ssd_w_in, ssd_conv_w, ssd_conv_b, ssd_dt_bias_f, ssd_dt_bias_b, ssd_a_log_f, ssd_a_log_b, ssd_d,
              ssd_norm_g, ssd_w_out, router_w, router_bias, exp_w_gu, exp_w_down, sh_w_gu, sh_w_down):
    bsz, n_lat, d = x.shape
    n_ctx = ctx.shape[1]
    for l in range(DEPTH):
        last = l == DEPTH - 1
        i = l // 2
        mod = (jax.nn.silu(c) @ mod_w[l] + mod_b[l]).reshape(bsz, 1, N_MOD, d)
        mod_c = (jax.nn.silu(c_ctx) @ mod_w[l] + mod_b[l]).reshape(1, 1, N_MOD, d)
        sh1, sc1, g1, sh2, sc2, g2 = jnp.moveaxis(mod, 2, 0)
        csh1, csc1, cg1, csh2, csc2, cg2 = jnp.moveaxis(mod_c, 2, 0)
        h_lat = modulate(x, sh1, sc1)
        h_ctx = modulate(ctx, csh1, csc1)
        if l % 2 == 0:
            p = {"w_in": a_w_in[i], "conv_w": hy_conv_w[i], "conv_b": hy_conv_b[i],
                 "filt_w1": hy_filt_w1[i], "filt_b1": hy_filt_b1[i], "filt_freq1": hy_filt_freq1[i],
                 "filt_w2": hy_filt_w2[i], "filt_b2": hy_filt_b2[i], "filt_freq2": hy_filt_freq2[i],
                 "filt_w3": hy_filt_w3[i], "skip": hy_skip[i], "q_norm": mla_q_norm[i], "w_qb": mla_w_qb[i],
                 "kv_norm": mla_kv_norm[i], "w_kvb": mla_w_kvb[i], "w_out": a_w_out[i]}
            y_lat, y_ctx = mixer_hyena_mla(h_lat, h_ctx, p, not last)
        else:
            p = {"w_in": ssd_w_in[i], "conv_w": ssd_conv_w[i], "conv_b": ssd_conv_b[i],
                 "dt_bias_f": ssd_dt_bias_f[i], "dt_bias_b": ssd_dt_bias_b[i],
                 "a_log_f": ssd_a_log_f[i], "a_log_b": ssd_a_log_b[i], "d": ssd_d[i],
                 "norm_g": ssd_norm_g[i], "w_out": ssd_w_out[i]}
            y_lat, y_ctx = mixer_ssd(h_lat, h_ctx, p, not last)
        x = layer_norm(DN_ALPHA * x + g1 * y_lat, ln_mix_g[l], ln_mix_b[l])
        ff_x = modulate(x, sh2, sc2)
        moe_w = (router_w[l], router_bias[l], exp_w_gu[l], exp_w_down[l], sh_w_gu[l], sh_w_down[l])
        if last:
            out = moe(ff_x.reshape(-1, d), *moe_w).reshape(bsz, n_lat, d)
            x = layer_norm(DN_ALPHA * x + g2 * out, ln_ffn_g[l], ln_ffn_b[l])
        else:
            ctx = layer_norm(DN_ALPHA * ctx + cg1 * y_ctx, ln_mix_g[l], ln_mix_b[l])
            ff_c = modulate(ctx, csh2, csc2)
            out = moe(jnp.concatenate([ff_c, ff_x], 1).reshape(-1, d), *moe_w).reshape(bsz, n_ctx + n_lat, d)
            ctx = layer_norm(DN_ALPHA * ctx + cg2 * out[:, :n_ctx], ln_ffn_g[l], ln_ffn_b[l])
            x = layer_norm(DN_ALPHA * x + g2 * out[:, n_ctx:], ln_ffn_g[l], ln_ffn_b[l])
    return x
```

```python
import math
from contextlib import ExitStack
import numpy as np
import ml_dtypes
import concourse.bass as bass
import concourse.mybir as mybir
from concourse.bass_utils import run_bass_kernel_spmd

F32 = mybir.dt.float32
BF16 = mybir.dt.bfloat16
I32 = mybir.dt.int32
AF = mybir.ActivationFunctionType
ALU = mybir.AluOpType
AX = mybir.AxisListType
NPBF = ml_dtypes.bfloat16

ENGS = ["pe", "act", "dve", "pool", "sp"]
NDSEM = {"sp": 8, "pool": 6, "act": 2}
NCORES = 8

D = 1024
BATCH = 4
SEQ = 8192
CTX = 256
DEPTH = 2
GRID_W = 64
HYW = 512
HY_EMB = 33
HY_BANDS = 16
HY_DECAY_MIN = math.log(1e-2) / 1.5
HY_DECAY_MAX = math.log(1e-2) / 0.3
MLA_SCALE = 96 ** -0.5
DN_ALPHA = (2 * DEPTH) ** 0.25
LN_EPS = 1e-5
RMS_EPS = 1e-6
NE = 256
ROUTED_SCALE = 2.5
PI = math.pi


class Tile:
    def __init__(self, t, name):
        self.t = t
        self.name = name
        self.w = None
        self.r = {}

    def __getitem__(self, idx):
        return self.t[idx]


class Prog:
    def __init__(self):
        self.nc = bass.Bass("TRN2", target_bir_lowering=False)
        self.stk = [ExitStack()]
        self.ops = {e: [] for e in ENGS}
        self.cnt = {e: 0 for e in ENGS}
        self.seen = {e: {} for e in ENGS}
        self.sems = {}
        for e in ["pe", "act", "dve", "pool"]:
            self.sems[e] = self.stk[0].enter_context(self.nc.semaphore("s_" + e))
        self.dtot = {}
        self.drot = {q: 0 for q in NDSEM}
        for q, n in NDSEM.items():
            for i in range(n):
                k = "d_%s_%d" % (q, i)
                self.sems[k] = self.stk[0].enter_context(self.nc.semaphore(k))
                self.dtot[k] = 0
        self.nalloc = 0

    def dram(self, name, shape, dtype, kind="ExternalInput"):
        return self.nc.dram_tensor(name, list(shape), dtype, kind=kind)

    def push(self):
        self.stk.append(ExitStack())

    def pop(self):
        self.barrier()
        self.stk.pop().close()

    def sb(self, shape, dtype, name=None):
        self.nalloc += 1
        name = (name or "sb") + "_%d" % self.nalloc
        t = self.stk[-1].enter_context(self.nc.sbuf_tensor(name, list(shape), dtype))
        return Tile(t, name)

    def ps(self, shape, dtype, name=None):
        self.nalloc += 1
        name = (name or "ps") + "_%d" % self.nalloc
        t = self.stk[-1].enter_context(self.nc.psum_tensor(name, list(shape), dtype))
        return Tile(t, name)

    def _collect(self, eng, reads, writes):
        waits = {}

        def add(ev, same_ok):
            if ev is None:
                return
            k, v = ev
            if k == eng and not same_ok:
                return
            if waits.get(k, 0) < v:
                waits[k] = v

        for t in reads:
            add(t.w, True)
        for t in writes:
            add(t.w, False)
            for k, v in t.r.items():
                add((k, v), False)
        need = []
        for k, v in waits.items():
            if self.seen[eng].get(k, 0) >= v:
                continue
            self.seen[eng][k] = v
            need.append((k, v))
        return need

    def _commit(self, ev, reads, writes):
        k, v = ev
        for t in reads:
            if t.r.get(k, 0) < v:
                t.r[k] = v
        for t in writes:
            t.w = ev
            t.r = {}

    def op(self, eng, fn, reads=(), writes=()):
        need = self._collect(eng, reads, writes)
        self.cnt[eng] += 1
        ev = (eng, self.cnt[eng])
        self.ops[eng].append((need, fn, eng))
        self._commit(ev, reads, writes)
        return ev

    def dma(self, q, fn, reads=(), writes=()):
        need = self._collect(q, reads, writes)
        i = self.drot[q]
        self.drot[q] = (i + 1) % NDSEM[q]
        k = "d_%s_%d" % (q, i)
        if self.dtot[k] > 0 and self.seen[q].get(k, 0) < self.dtot[k]:
            self.seen[q][k] = self.dtot[k]
            need.append((k, self.dtot[k]))
        self.dtot[k] += 16
        ev = (k, self.dtot[k])
        self.ops[q].append((need, fn, k))
        self._commit(ev, reads, writes)
        return ev

    def barrier(self):
        allev = [(k, tot) for k, tot in self.dtot.items() if tot > 0]
        allev += [(e, self.cnt[e]) for e in ["pe", "act", "dve", "pool"] if self.cnt[e] > 0]
        for e in ENGS:
            need = []
            for k, v in allev:
                if k == e:
                    continue
                if self.seen[e].get(k, 0) >= v:
                    continue
                self.seen[e][k] = v
                need.append((k, v))
            if need:
                self.ops[e].append((need, None, None))

    def build(self):
        self.barrier()
        nc = self.nc
        sems = self.sems

        def replay(eng_name, e):
            for need, fn, inc in self.ops[eng_name]:
                for k, v in need:
                    e.wait_ge(sems[k], v)
                if fn is None:
                    continue
                ins = fn(e)
                if inc is not None:
                    ins.then_inc(sems[inc], 16 if inc.startswith("d_") else 1)

        with nc.Block() as block:
            @block.tensor
            def _(e):
                replay("pe", e)

            @block.scalar
            def _(e):
                replay("act", e)

            @block.vector
            def _(e):
                replay("dve", e)

            @block.gpsimd
            def _(e):
                replay("pool", e)

            @block.sync
            def _(e):
                replay("sp", e)
        while self.stk:
            self.stk.pop().close()
        return nc

    def load(self, t, out, in_, q="sp"):
        self.dma(q, lambda e: e.dma_start(out=out, in_=in_), writes=[t])

    def store(self, out, t, in_, q="sp"):
        self.dma(q, lambda e: e.dma_start(out=out, in_=in_), reads=[t])

    def mm(self, pt, out, lhsT, rhs, start=True, stop=True, reads=()):
        self.op("pe", lambda e: e.matmul(out, lhsT=lhsT, rhs=rhs, start=start, stop=stop),
                reads=reads, writes=[pt])

    def tr(self, pt, out, in_, ident, reads=()):
        self.op("pe", lambda e: e.transpose(out, in_, ident), reads=reads, writes=[pt])

    def act(self, ot, out, in_, func, bias=0.0, scale=1.0, reads=(), accum=None):
        if accum is None:
            fn = lambda e: e.activation(out=out, in_=in_, func=func, bias=bias, scale=scale)
        else:
            fn = lambda e: e.activation(out=out, in_=in_, func=func, bias=bias, scale=scale, accum_out=accum)
        self.op("act", fn, reads=reads, writes=[ot] if not isinstance(ot, (list, tuple)) else list(ot))

    def ts(self, eng, ot, out, in0, s1, s2, op0, op1=None, reads=()):
        if op1 is None:
            fn = lambda e: e.tensor_scalar(out=out, in0=in0, scalar1=s1, scalar2=None, op0=op0)
        else:
            fn = lambda e: e.tensor_scalar(out=out, in0=in0, scalar1=s1, scalar2=s2, op0=op0, op1=op1)
        self.op(eng, fn, reads=reads, writes=[ot])

    def tt(self, eng, ot, out, in0, in1, op, reads=()):
        self.op(eng, lambda e: e.tensor_tensor(out=out, in0=in0, in1=in1, op=op), reads=reads, writes=[ot])

    def stt(self, eng, ot, out, in0, scalar, in1, op0, op1, reads=()):
        self.op(eng, lambda e: e.scalar_tensor_tensor(out=out, in0=in0, scalar=scalar, in1=in1, op0=op0, op1=op1),
                reads=reads, writes=[ot])

    def cp(self, eng, ot, out, in_, reads=()):
        if eng == "act":
            self.op(eng, lambda e: e.copy(out=out, in_=in_), reads=reads, writes=[ot])
        else:
            self.op(eng, lambda e: e.tensor_copy(out=out, in_=in_), reads=reads, writes=[ot])

    def memset(self, eng, ot, out, val):
        self.op(eng, lambda e: e.memset(out, val), writes=[ot])


def run_spmd(P, in_maps):
    nc = P.build()
    res = run_bass_kernel_spmd(nc, in_maps, core_ids=list(range(NCORES)))
    return res.results


def c_(a, dt=np.float32):
    return np.ascontiguousarray(a, dtype=dt)


def pk(a):
    kp, n = a.shape
    return c_(a.reshape(kp // 128, 128, n).transpose(1, 0, 2))


def launch_mod(c, c_ctx, mod_w, mod_b):
    P = Prog()
    NCOL = 6 * D // NCORES
    cT = P.dram("cT", [128, 8, 5], F32)
    mw = P.dram("mw", [DEPTH, 128, 8, NCOL], F32)
    mb = P.dram("mb", [DEPTH, 5, NCOL], F32)
    out = P.dram("out", [DEPTH, 5, NCOL], F32, kind="ExternalOutput")
    cs = P.sb([128, 8, 5], F32)
    sc = P.sb([128, 8, 5], F32)
    P.load(cs, cs[:], cT.ap())
    P.act(sc, sc[:], cs[:], AF.Silu, reads=[cs])
    for l in range(DEPTH):
        w = P.sb([128, 8, NCOL], F32)
        b = P.sb([5, NCOL], F32)
        o = P.sb([5, NCOL], F32)
        P.load(w, w[:], mw.ap()[l])
        P.load(b, b[:], mb.ap()[l])
        for hf in range(2):
            pt = P.ps([128, 512], F32)
            for k in range(8):
                P.mm(pt, pt[0:5, 0:384], sc[:, k, :], w[:, k, hf * 384:(hf + 1) * 384],
                     start=(k == 0), stop=(k == 7), reads=[sc, w])
            P.tt("dve", o, o[:, hf * 384:(hf + 1) * 384], pt[0:5, 0:384], b[:, hf * 384:(hf + 1) * 384],
                 ALU.add, reads=[pt, b])
        P.store(out.ap()[l], o, o[:])
    cc = np.concatenate([c, c_ctx[None]], 0)
    cTh = c_(cc.T.reshape(8, 128, 5).transpose(1, 0, 2))
    maps = []
    for i in range(NCORES):
        sl = slice(i * NCOL, (i + 1) * NCOL)
        maps.append({
            "cT": cTh,
            "mw": c_(np.stack([pk(mod_w[l][:, sl]) for l in range(DEPTH)])),
            "mb": c_(np.stack([np.broadcast_to(mod_b[l][sl], (5, NCOL)) for l in range(DEPTH)])),
        })
    res = run_spmd(P, maps)
    mod = np.concatenate([r["out"] for r in res], axis=2)
    return mod.reshape(DEPTH, 5, 6, D)


def _filt_consts(n):
    f32 = np.float32
    t = np.linspace(0.0, 1.0, n, dtype=f32)
    w = (2.0 * math.pi * np.arange(n, dtype=f32) / n).astype(f32)
    fr = np.linspace(1e-4, HY_BANDS - 1, HY_BANDS, dtype=f32)
    z = np.concatenate([t[:, None], np.cos(fr[None] * w[:, None]), -np.sin(fr[None] * w[:, None])], -1).astype(f32)
    idx = np.concatenate([np.minimum(n - np.arange(n), n - 1), np.arange(n)])
    zext = c_(z[idx].T)
    text = c_(t[idx])
    return zext, text


def launch_filt(w1, b1, f1, w2, b2, f2, w3):
    P = Prog()
    CH = HYW // NCORES
    ns = [SEQ, CTX]
    d_w1 = P.dram("w1", [33, 64], F32)
    d_w2 = P.dram("w2", [64, 64], F32)
    d_vec = P.dram("vec", [64, 4], F32)
    d_w3 = P.dram("w3", [64, 2, CH], F32)
    d_nd = P.dram("nd", [CH, 1], F32)
    d_z = [P.dram("z%d" % i, [33, 2 * n], F32) for i, n in enumerate(ns)]
    d_t = [P.dram("t%d" % i, [CH, 2 * n], F32) for i, n in enumerate(ns)]
    d_o = [P.dram("o%d" % i, [CH, 2 * n], BF16, kind="ExternalOutput") for i, n in enumerate(ns)]
    w1s = P.sb([33, 64], F32); w2s = P.sb([64, 64], F32); vec = P.sb([64, 4], F32)
    w3s = P.sb([64, 2, CH], F32); nd = P.sb([CH, 1], F32)
    P.load(w1s, w1s[:], d_w1.ap()); P.load(w2s, w2s[:], d_w2.ap()); P.load(vec, vec[:], d_vec.ap())
    P.load(w3s, w3s[:], d_w3.ap()); P.load(nd, nd[:], d_nd.ap())
    off = P.sb([64, 4], F32)
    for j in range(2):
        P.ts("dve", off, off[:, j:j + 1], vec[:, 2 * j:2 * j + 1], vec[:, 2 * j + 1:2 * j + 2], 1.0 / (2 * PI),
             ALU.mult, ALU.mult, reads=[vec])
        P.ts("dve", off, off[:, 2 + j:3 + j], vec[:, 2 * j + 1:2 * j + 2], 1.0 / (2 * PI), None,
             ALU.mult, reads=[vec])
    pts = [P.ps([128, 512], F32) for _ in range(6)]
    for i, n in enumerate(ns):
        P.push()
        L = P.sb([CH, 2 * n], F32)
        zss = [P.sb([33, 512], F32) for _ in range(2)]
        txs = [P.sb([CH, 512], F32) for _ in range(2)]
        fb = [[P.sb([64, 512], F32) for _ in range(5)] for _ in range(2)]
        ki = P.sb([64, 512], I32); kf = P.sb([64, 512], F32)
        TW = min(512, n)
        for ti in range(2 * n // TW):
            sl = slice(ti * TW, (ti + 1) * TW)
            p1, p2, p3 = pts[(ti % 2) * 3:(ti % 2) * 3 + 3]
            a1, h1, a2, h2, dec = fb[ti % 2]
            zs = zss[ti % 2]; tx = txs[ti % 2]
            P.load(zs, zs[:, 0:TW], d_z[i].ap()[:, sl])
            P.load(tx, tx[:, 0:TW], d_t[i].ap()[:, sl])
            P.mm(p1, p1[0:64, 0:TW], w1s[:], zs[:, 0:TW], reads=[w1s, zs])
            P.ts("dve", a1, a1[:, 0:TW], p1[0:64, 0:TW], off[:, 2:3], off[:, 0:1], ALU.mult, ALU.add, reads=[p1, off])
            P.cp("dve", ki, ki[:, 0:TW], a1[:, 0:TW], reads=[a1])
            P.cp("dve", kf, kf[:, 0:TW], ki[:, 0:TW], reads=[ki])
            P.tt("dve", a1, a1[:, 0:TW], a1[:, 0:TW], kf[:, 0:TW], ALU.subtract, reads=[a1, kf])
            P.act(h1, h1[:, 0:TW], a1[:, 0:TW], AF.Sin, scale=2 * PI, reads=[a1])
            P.mm(p2, p2[0:64, 0:TW], w2s[:], h1[:, 0:TW], reads=[w2s, h1])
            P.ts("dve", a2, a2[:, 0:TW], p2[0:64, 0:TW], off[:, 3:4], off[:, 1:2], ALU.mult, ALU.add, reads=[p2, off])
            P.cp("dve", ki, ki[:, 0:TW], a2[:, 0:TW], reads=[a2])
            P.cp("dve", kf, kf[:, 0:TW], ki[:, 0:TW], reads=[ki])
            P.tt("dve", a2, a2[:, 0:TW], a2[:, 0:TW], kf[:, 0:TW], ALU.subtract, reads=[a2, kf])
            P.act(h2, h2[:, 0:TW], a2[:, 0:TW], AF.Sin, scale=2 * PI, reads=[a2])
            d = 1 if (ti * TW) < n else 0
            P.mm(p3, p3[0:CH, 0:TW], w3s[:, d, :], h2[:, 0:TW], reads=[w3s, h2])
            P.act(dec, dec[:, 0:TW], tx[:, 0:TW], AF.Exp, scale=nd[:, 0:1], reads=[tx, nd])
            P.tt("dve", L, L[:, sl], p3[0:CH, 0:TW], dec[:, 0:TW], ALU.mult, reads=[p3, dec])
        P.memset("dve", L, L[:, 0:1], 0.0)
        ab = P.sb([CH, 2 * n], F32)
        sm = P.sb([CH, 2], F32)
        P.act(ab, ab[:], L[:], AF.Abs, reads=[L])
        P.op("dve", lambda e, sm=sm, ab=ab: e.reduce_sum(out=sm[:, 0:1], in_=ab[:], axis=AX.X), reads=[ab], writes=[sm])
        P.op("dve", lambda e, sm=sm: e.reciprocal(out=sm[:, 1:2], in_=sm[:, 0:1]), reads=[sm], writes=[sm])
        Lb = P.sb([CH, 2 * n], BF16)
        P.ts("dve", Lb, Lb[:], L[:], sm[:, 1:2], None, ALU.mult, reads=[L, sm])
        P.store(d_o[i].ap(), Lb, Lb[:])
        P.pop()
    deltas = np.abs(np.linspace(HY_DECAY_MIN, HY_DECAY_MAX, HYW, dtype=np.float32))
    consts = [_filt_consts(n) for n in ns]
    maps = []
    for i in range(NCORES):
        ch = slice(i * CH, (i + 1) * CH)
        m = {"w1": c_(w1), "w2": c_(w2), "vec": c_(np.stack([b1, f1, b2, f2], 1)),
             "w3": c_(np.stack([w3[:, :HYW][:, ch], w3[:, HYW:][:, ch]], 1)),
             "nd": c_(-deltas[ch][:, None])}
        for j, n in enumerate(ns):
            m["z%d" % j] = consts[j][0]
            m["t%d" % j] = c_(np.broadcast_to(consts[j][1], (CH, 2 * n)))
        maps.append(m)
    res = run_spmd(P, maps)
    Ls = [np.concatenate([r["o%d" % j] for r in res], 0) for j in range(2)]
    return Ls


NT0 = 4228
INT_SEGS = [(1, 129, 0), (131, 4227, 128)]
NTI = 4224


def _ntiles(n, w=512):
    return [(s, min(w, n - s)) for s in range(0, n, w)]


def _core_tokens(b, s):
    ci = np.arange(s * 128 - 1, s * 128 + 129)
    li = np.arange(s * 4096 - 1, s * 4096 + 4097)
    return ci, li


def _gather_rows(arr, idx):
    n = arr.shape[0]
    ok = (idx >= 0) & (idx < n)
    out = arr[np.clip(idx, 0, n - 1)].copy()
    out[~ok] = 0
    return out, ok


def _rope_tables():
    f32 = np.float32
    rows = SEQ // GRID_W
    row = np.repeat(np.arange(rows), GRID_W).astype(f32)
    col = np.tile(np.arange(GRID_W), rows).astype(f32)
    half = 16
    inv = (10000.0 ** (-np.arange(0, half, 2, dtype=f32) / half)).astype(f32)
    ang = np.concatenate([row[:, None] * inv, col[:, None] * inv], -1)
    cos = np.cos(ang).astype(f32); sin = np.sin(ang).astype(f32)
    cosT = np.ones((96, SEQ), f32); sinT = np.zeros((96, SEQ), f32)
    for j in range(32):
        cosT[64 + j] = cos[:, j // 2]
        sinT[64 + j] = sin[:, j // 2] * (-1.0 if j % 2 == 0 else 1.0)
    return cosT, sinT


def launch_pre0(x, ctx, mod0, w_in, conv_w, conv_b, q_norm, w_qb, kv_norm, w_kvb):
    P = Prog()
    NT = NT0
    d_xT = P.dram("xT", [128, 8, NT], F32)
    d_mod = P.dram("mod", [128, 8, 4], F32)
    d_win = P.dram("win", [12, 128, 8, 128], F32)
    d_wq = P.dram("wq", [128, 8, 256], F32)
    d_wkv = P.dram("wkv", [128, 8, 128], F32)
    d_wkp = P.dram("wkp", [2, 128, 8, 96], F32)
    d_cw = P.dram("cw", [128, 12, 4], F32)
    d_hm = P.dram("hm", [128, 4], F32)
    d_cos = P.dram("cos", [96, NT], F32)
    d_sin = P.dram("sin", [96, NT], F32)
    d_gq = P.dram("gq", [128, 3], F32)
    d_wqb = P.dram("wqb", [2, 128, 2, 8, 96], F32)
    d_wkn = P.dram("wkn", [128, 8, 64], F32)
    d_wv = P.dram("wv", [128, 512], F32)
    o_x0 = P.dram("o_x0", [512, NTI], BF16, kind="ExternalOutput")
    o_z = P.dram("o_z", [512, NTI], BF16, kind="ExternalOutput")
    o_q = P.dram("o_q", [8, 96, NTI], BF16, kind="ExternalOutput")
    o_k = P.dram("o_k", [8, 96, NTI], BF16, kind="ExternalOutput")
    o_v = P.dram("o_v", [NTI, 512], BF16, kind="ExternalOutput")

    tiles = _ntiles(NT)
    pbank = [P.ps([128, 512], F32) for _ in range(8)]
    pb_i = [0]

    def bank():
        pb_i[0] = (pb_i[0] + 1) % 8
        return pbank[pb_i[0]]

    hT = P.sb([128, 8, NT], BF16, "hT")
    epsq = P.sb([128, 1], F32); P.memset("dve", epsq, epsq[:], RMS_EPS)
    mods = P.sb([128, 8, 4], F32)
    P.load(mods, mods[:], d_mod.ap())
    m1 = P.sb([128, 8, 2], F32)
    for j in range(2):
        P.ts("dve", m1, m1[:, :, j:j + 1], mods[:, :, 2 * j:2 * j + 1], 1.0, None, ALU.add, reads=[mods])
    P.push()
    xin = [P.sb([128, 8, 512], F32) for _ in range(2)]
    for ti, (s0, w) in enumerate(tiles):
        xt = xin[ti % 2]
        P.load(xt, xt[:, :, 0:w], d_xT.ap()[:, :, s0:s0 + w])
        for k in range(8):
            for (a, b_, j) in ((s0, min(s0 + w, 130), 0), (max(s0, 130), s0 + w, 1)):
                if b_ <= a:
                    continue
                P.ts("dve" if k % 2 == 0 else "pool", hT, hT[:, k, a:b_], xt[:, k, a - s0:b_ - s0],
                     m1[:, k, j:j + 1], mods[:, k, 2 * j + 1:2 * j + 2], ALU.mult, ALU.add, reads=[xt, m1, mods])
    P.pop()

    def project(pt_rows, wt, wsel, dst_fn):
        for (s0, w) in tiles:
            pt = bank()
            for k in range(8):
                P.mm(pt, pt[0:pt_rows, 0:w], wsel(k), hT[:, k, s0:s0 + w], start=(k == 0), stop=(k == 7),
                     reads=[wt, hT])
            dst_fn(pt, s0, w)

    P.push()
    cw = P.sb([128, 12, 4], F32); hm = P.sb([128, 4], F32)
    P.load(cw, cw[:], d_cw.ap()); P.load(hm, hm[:], d_hm.ap())
    wbuf = [P.sb([128, 8, 128], BF16) for _ in range(2)]
    urow = [P.sb([128, NT], F32) for _ in range(2)]
    cv = P.sb([128, NT], F32)
    x1c = P.sb([128, 4, NT], BF16)
    ob = [P.sb([128, NT], BF16) for _ in range(2)]
    for oc in range(12):
        wt = wbuf[oc % 2]; ur = urow[oc % 2]
        P.load(wt, wt[:], d_win.ap()[oc], q="pool")
        cnt = [0]

        def evac(pt, s0, w, ur=ur, cnt=cnt):
            cnt[0] += 1
            P.cp("act" if cnt[0] % 2 else "dve", ur, ur[:, s0:s0 + w], pt[:, 0:w], reads=[pt])
        project(128, wt, lambda k, wt=wt: wt[:, k, :], evac)
        for j, c in enumerate((0, 129, 130, 4227)):
            P.tt("dve", ur, ur[:, c:c + 1], ur[:, c:c + 1], hm[:, j:j + 1], ALU.mult, reads=[ur, hm])
        P.act(cv, cv[:], ur[:], AF.Identity, bias=cw[:, oc, 3:4], scale=cw[:, oc, 1:2], reads=[ur, cw])
        P.stt("dve", cv, cv[:, 1:NT], ur[:, 0:NT - 1], cw[:, oc, 0:1], cv[:, 1:NT], ALU.mult, ALU.add, reads=[ur, cw, cv])
        P.stt("dve", cv, cv[:, 0:NT - 1], ur[:, 1:NT], cw[:, oc, 2:3], cv[:, 0:NT - 1], ALU.mult, ALU.add, reads=[ur, cw, cv])
        if 4 <= oc < 8:
            P.cp("pool", x1c, x1c[:, oc - 4, :], cv[:], reads=[cv])
        else:
            o = ob[oc % 2]
            if oc < 4:
                P.cp("pool", o, o[:], cv[:], reads=[cv])
                dst = o_x0
            else:
                P.tt("pool", o, o[:], cv[:], x1c[:, oc - 8, :], ALU.mult, reads=[cv, x1c])
                dst = o_z
            r0 = (oc % 4) * 128
            for (a, b_, oo) in INT_SEGS:
                P.store(dst.ap()[r0:r0 + 128, oo:oo + (b_ - a)], o, o[:, a:b_])
    P.pop()

    P.push()
    wq = P.sb([128, 8, 256], BF16); wkv = P.sb([128, 8, 128], BF16); wkp = P.sb([128, 2, 8, 96], BF16)
    P.load(wq, wq[:], d_wq.ap(), q="pool"); P.load(wkv, wkv[:], d_wkv.ap(), q="pool")
    for j in range(2):
        P.load(wkp, wkp[:, j], d_wkp.ap()[j], q="pool")
    cosT = P.sb([96, NT], BF16); sinT = P.sb([96, NT], BF16)
    P.load(cosT, cosT[:], d_cos.ap(), q="pool"); P.load(sinT, sinT[:], d_sin.ap(), q="pool")
    gq = P.sb([128, 3], F32); P.load(gq, gq[:], d_gq.ap())
    ones = P.sb([128, 128], F32); P.memset("dve", ones, ones[:], 1.0)
    qn = P.sb([128, 3, NT], BF16)
    kpe = P.sb([96, NT], BF16)
    tA = P.sb([96, 512], F32); tB = P.sb([96, 512], F32)
    P.push()
    uq = P.sb([128, 3, NT], F32)
    for c in range(3):
        def evq(pt, s0, w, c=c):
            P.cp("act", uq, uq[:, c, s0:s0 + w], pt[:, 0:w], reads=[pt])
        if c < 2:
            project(128, wq, lambda k, c=c: wq[:, k, c * 128:(c + 1) * 128], evq)
        else:
            project(128, wkv, lambda k: wkv[:, k, :], evq)
    for (s0, w) in tiles:
        pA = bank(); pB = bank()
        for k in range(8):
            P.mm(pA, pA[0:96, 0:w], wkp[:, 0, k, :], hT[:, k, s0:s0 + w], start=(k == 0), stop=(k == 7), reads=[wkp, hT])
        for k in range(8):
            P.mm(pB, pB[0:96, 0:w], wkp[:, 1, k, :], hT[:, k, s0:s0 + w], start=(k == 0), stop=(k == 7), reads=[wkp, hT])
        P.tt("dve", tA, tA[64:96, 0:w], pA[64:96, 0:w], cosT[64:96, s0:s0 + w], ALU.mult, reads=[pA, cosT])
        P.tt("dve", tB, tB[64:96, 0:w], pB[64:96, 0:w], sinT[64:96, s0:s0 + w], ALU.mult, reads=[pB, sinT])
        P.tt("pool", kpe, kpe[64:96, s0:s0 + w], tA[64:96, 0:w], tB[64:96, 0:w], ALU.add, reads=[tA, tB])
    sq = [P.sb([128, 2, 512], F32) for _ in range(2)]
    rs = [P.sb([128, 512], F32) for _ in range(2)]
    for ti, (s0, w) in enumerate(tiles):
        for grp, chunks, R in ((0, (0, 1), 256.0), (1, (2,), 128.0)):
            sqt = sq[(2 * ti + grp) % 2]; rst = rs[(2 * ti + grp) % 2]
            pt = bank()
            for i, c in enumerate(chunks):
                P.act(sqt, sqt[:, i, 0:w], uq[:, c, s0:s0 + w], AF.Square, reads=[uq])
            for i, c in enumerate(chunks):
                P.mm(pt, pt[:, 0:w], ones[:], sqt[:, i, 0:w], start=(i == 0), stop=(i == len(chunks) - 1), reads=[ones, sqt])
            P.act(rst, rst[:, 0:w], pt[:, 0:w], AF.Sqrt, bias=epsq[:, 0:1], scale=1.0 / R, reads=[pt])
            P.op("dve", lambda e, rst=rst, w=w: e.reciprocal(out=rst[:, 0:w], in_=rst[:, 0:w]), reads=[rst], writes=[rst])
            for c in chunks:
                P.stt("dve", qn, qn[:, c, s0:s0 + w], uq[:, c, s0:s0 + w], gq[:, c:c + 1], rst[:, 0:w], ALU.mult, ALU.mult,
                      reads=[uq, gq, rst])
    P.pop()
    wqb = P.sb([128, 2, 2, 8, 96], BF16)
    for j in range(2):
        P.load(wqb, wqb[:, j], d_wqb.ap()[j], q="pool")
    wkn = P.sb([128, 8, 64], BF16); P.load(wkn, wkn[:], d_wkn.ap(), q="pool")
    wv = P.sb([128, 512], BF16); P.load(wv, wv[:], d_wv.ap(), q="pool")
    qh = [P.sb([96, NT], BF16) for _ in range(2)]
    kh = [P.sb([96, NT], BF16) for _ in range(2)]
    for h in range(8):
        qt = qh[h % 2]; kt = kh[h % 2]
        for (s0, w) in tiles:
            pA = bank(); pB = bank(); pK = bank()
            for k in range(2):
                P.mm(pA, pA[0:96, 0:w], wqb[:, 0, k, h, :], qn[:, k, s0:s0 + w], start=(k == 0), stop=(k == 1), reads=[wqb, qn])
            for k in range(2):
                P.mm(pB, pB[0:96, 0:w], wqb[:, 1, k, h, :], qn[:, k, s0:s0 + w], start=(k == 0), stop=(k == 1), reads=[wqb, qn])
            P.mm(pK, pK[0:64, 0:w], wkn[:, h, :], qn[:, 2, s0:s0 + w], reads=[wkn, qn])
            P.tt("dve", tA, tA[:, 0:w], pA[0:96, 0:w], cosT[:, s0:s0 + w], ALU.mult, reads=[pA, cosT])
            P.tt("dve", tB, tB[:, 0:w], pB[0:96, 0:w], sinT[:, s0:s0 + w], ALU.mult, reads=[pB, sinT])
            P.tt("pool", qt, qt[:, s0:s0 + w], tA[:, 0:w], tB[:, 0:w], ALU.add, reads=[tA, tB])
            P.cp("act", kt, kt[0:64, s0:s0 + w], pK[0:64, 0:w], reads=[pK])
        P.cp("pool", kt, kt[64:96, :], kpe[64:96, :], reads=[kpe])
        for (a, b_, oo) in INT_SEGS:
            P.store(o_q.ap()[h, :, oo:oo + (b_ - a)], qt, qt[:, a:b_])
            P.store(o_k.ap()[h, :, oo:oo + (b_ - a)], kt, kt[:, a:b_])
    vb = [P.sb([128, 512], BF16) for _ in range(2)]
    for ti in range(NTI // 128):
        col = 1 + ti * 128 if ti == 0 else 131 + (ti - 1) * 128
        pt = bank(); v = vb[ti % 2]
        P.mm(pt, pt[:, :], qn[:, 2, col:col + 128], wv[:], reads=[qn, wv])
        P.cp("act", v, v[:], pt[:, :], reads=[pt])
        P.store(o_v.ap()[ti * 128:(ti + 1) * 128, :], v, v[:])
    P.pop()

    cosF, sinF = _rope_tables()
    pairswap = np.arange(32) ^ 1
    win_h = c_(np.stack([pk(w_in[:, oc * 128:(oc + 1) * 128]) for oc in range(12)]))
    wq_h = pk(w_in[:, 1536:1792]); wkv_h = pk(w_in[:, 1792:1920])
    kpA = np.zeros((D, 96), np.float32); kpB = np.zeros((D, 96), np.float32)
    kpA[:, 64:] = w_in[:, 1920:1952]; kpB[:, 64:] = w_in[:, 1920:1952][:, pairswap]
    wkp_h = c_(np.stack([pk(kpA), pk(kpB)]))
    cw_h = c_(np.concatenate([conv_w.T.reshape(12, 128, 3), conv_b.reshape(12, 128, 1)], -1).transpose(1, 0, 2))
    gq_h = c_(np.stack([q_norm[:128], q_norm[128:], kv_norm], 1))
    wqbA = w_qb.reshape(256, 8, 96)
    wqbB = wqbA.copy(); wqbB[:, :, 64:] = wqbA[:, :, 64:][:, :, pairswap]
    wqb_h = c_(np.stack([wqbA.reshape(2, 128, 8, 96).transpose(1, 0, 2, 3), wqbB.reshape(2, 128, 8, 96).transpose(1, 0, 2, 3)]))
    wkvr = w_kvb.reshape(128, 8, 128)
    wkn_h = c_(wkvr[:, :, :64]); wv_h = c_(wkvr[:, :, 64:].reshape(128, 512))
    maps = []
    for core in range(NCORES):
        b, s = core // 2, core % 2
        ci, li = _core_tokens(b, s)
        xc, okc = _gather_rows(ctx[b], ci); xl, okl = _gather_rows(x[b], li)
        xw = np.concatenate([xc, xl], 0)
        cosw = np.ones((96, NT), np.float32); sinw = np.zeros((96, NT), np.float32)
        lic = np.clip(li, 0, SEQ - 1)
        cosw[:, 130:] = cosF[:, lic]; sinw[:, 130:] = sinF[:, lic]
        hmv = np.array([okc[0], okc[-1], okl[0], okl[-1]], np.float32)
        modv = np.stack([mod0[4, 1], mod0[4, 0], mod0[b, 1], mod0[b, 0]], 1)
        maps.append({
            "xT": c_(xw.T.reshape(8, 128, NT).transpose(1, 0, 2)),
            "mod": c_(modv.reshape(8, 128, 4).transpose(1, 0, 2)),
            "win": win_h, "wq": wq_h, "wkv": wkv_h, "wkp": wkp_h, "cw": cw_h,
            "hm": c_(np.broadcast_to(hmv, (128, 4))), "cos": cosw, "sin": sinw, "gq": gq_h,
            "wqb": wqb_h, "wkn": wkn_h, "wv": wv_h,
        })
    res = run_spmd(P, maps)
    return res


_EPS_TILES = {}


def RMS_EPS_AP(P):
    key = id(P)
    if key not in _EPS_TILES:
        t = P.stk[0].enter_context(P.nc.sbuf_tensor("epsc", [128, 1], F32))
        tl = Tile(t, "epsc")
        P.memset("dve", tl, tl[:], RMS_EPS)
        _EPS_TILES[key] = tl
    return _EPS_TILES[key][:, 0:1]


NK = CTX + SEQ


def launch_attn(QT, KT, V):
    P = Prog()
    HG = 4
    NKT = NK // 128
    d_q = P.dram("q", [HG, 96, NK], BF16)
    d_k = P.dram("k", [HG, 96, NK], BF16)
    d_v = P.dram("v", [HG, 128, NKT, 65], BF16)
    d_sel = P.dram("sel", [65, 64], F32)
    o_a = P.dram("o_a", [HG, 64, NK], BF16, kind="ExternalOutput")
    ones = P.sb([128, 128], F32); P.memset("dve", ones, ones[:], 1.0)
    sel = P.sb([65, 64], F32); P.load(sel, sel[:], d_sel.ap())
    qs = [P.sb([96, NK], BF16) for _ in range(2)]
    ks = [P.sb([96, NK], BF16) for _ in range(2)]
    vs = [P.sb([128, NKT, 65], BF16) for _ in range(2)]
    ats = [P.sb([64, NK], BF16) for _ in range(2)]
    sqt = [P.sb([96, 512], F32) for _ in range(2)]
    mx = P.sb([128, 4], F32)
    negc = [P.sb([128, 1], F32) for _ in range(2)]
    pT = [P.sb([128, 512], BF16) for _ in range(3)]
    oT = [P.sb([65, 512], F32) for _ in range(2)]
    rec = [P.sb([64, 512], F32) for _ in range(2)]
    psS = [P.ps([128, 512], F32) for _ in range(4)]
    psO = [P.ps([128, 512], F32) for _ in range(2)]
    psD = [P.ps([128, 512], F32) for _ in range(2)]
    cS = [0]; cO = [0]
    qtiles = _ntiles(NK)
    for h in range(HG):
        q = qs[h % 2]; k = ks[h % 2]; v = vs[h % 2]; at = ats[h % 2]; nc_ = negc[h % 2]
        P.load(q, q[:], d_q.ap()[h]); P.load(k, k[:], d_k.ap()[h], q="act"); P.load(v, v[:], d_v.ap()[h])
        for which, src in ((0, q), (1, k)):
            for ti, (s0, w) in enumerate(qtiles):
                st = sqt[ti % 2]; pt = psD[ti % 2]
                P.act(st, st[:, 0:w], src[:, s0:s0 + w], AF.Square, reads=[src])
                P.mm(pt, pt[:, 0:w], ones[0:96, :], st[:, 0:w], reads=[ones, st])
                if ti == 0:
                    P.op("dve", lambda e, pt=pt, w=w, which=which: e.reduce_max(out=mx[:, which:which + 1], in_=pt[:, 0:w], axis=AX.X),
                         reads=[pt], writes=[mx])
                else:
                    P.op("dve", lambda e, pt=pt, w=w: e.reduce_max(out=mx[:, 2:3], in_=pt[:, 0:w], axis=AX.X),
                         reads=[pt], writes=[mx])
                    P.tt("dve", mx, mx[:, which:which + 1], mx[:, which:which + 1], mx[:, 2:3], ALU.max, reads=[mx])
        P.tt("dve", mx, mx[:, 3:4], mx[:, 0:1], mx[:, 1:2], ALU.mult, reads=[mx])
        P.act(mx, mx[:, 3:4], mx[:, 3:4], AF.Sqrt, scale=MLA_SCALE * MLA_SCALE, reads=[mx])
        P.ts("dve", nc_, nc_[:], mx[:, 3:4], -1.0, None, ALU.mult, reads=[mx])
        chunks = [(0, 256, 2)] + [(256 + 512 * i, 512, NKT) for i in range(SEQ // 512)]
        for (q0, qw, nkt) in chunks:
            po = psO[cO[0] % 2]; o = oT[cO[0] % 2]; rc = rec[cO[0] % 2]; pd = psD[cO[0] % 2]; cO[0] += 1
            for kt in range(nkt):
                pS = psS[cS[0] % 4]; p_ = pT[cS[0] % 3]; cS[0] += 1
                P.mm(pS, pS[:, 0:qw], k[:, kt * 128:(kt + 1) * 128], q[:, q0:q0 + qw], reads=[k, q])
                P.act(p_, p_[:, 0:qw], pS[:, 0:qw], AF.Exp, bias=nc_[:, 0:1], scale=MLA_SCALE, reads=[pS, nc_])
                P.mm(po, po[0:65, 0:qw], v[:, kt, :], p_[:, 0:qw], start=(kt == 0), stop=(kt == nkt - 1), reads=[v, p_])
            P.cp("dve", o, o[:, 0:qw], po[0:65, 0:qw], reads=[po])
            P.mm(pd, pd[0:64, 0:qw], sel[:], o[:, 0:qw], reads=[sel, o])
            P.op("dve", lambda e, rc=rc, pd=pd, qw=qw: e.reciprocal(out=rc[:, 0:qw], in_=pd[0:64, 0:qw]), reads=[pd], writes=[rc])
            P.tt("pool", at, at[:, q0:q0 + qw], o[0:64, 0:qw], rc[:, 0:qw], ALU.mult, reads=[o, rc])
        P.store(o_a.ap()[h], at, at[:])
    selh = np.zeros((65, 64), np.float32); selh[64] = 1.0
    maps = []
    for core in range(NCORES):
        b, g = core // 2, core % 2
        hs = slice(g * HG, (g + 1) * HG)
        vv = np.asarray(V[b][:, hs, :])
        va = np.ones((HG, 128, NKT, 65), NPBF)
        va[:, :, :, :64] = vv.reshape(NKT, 128, HG, 64).transpose(2, 1, 0, 3)
        maps.append({"q": np.ascontiguousarray(QT[b, hs]), "k": np.ascontiguousarray(KT[b, hs]), "v": va, "sel": selh})
    res = run_spmd(P, maps)
    att = np.zeros((BATCH, 8, 64, NK), NPBF)
    for core in range(NCORES):
        b, g = core // 2, core % 2
        att[b, g * HG:(g + 1) * HG] = res[core]["o_a"]
    return att


def launch_hyconv(Ls, zT_lat, zT_ctx, skip):
    P = Prog()
    CH = 64
    cfgs = [(SEQ, SEQ // 128), (CTX, CTX // 128)]
    d_L = [P.dram("L%d" % i, [CH, 2 * n], BF16) for i, (n, J) in enumerate(cfgs)]
    d_z = [P.dram("z%d" % i, [128, CH, 4, J], BF16) for i, (n, J) in enumerate(cfgs)]
    d_sk = P.dram("sk", [128, CH], F32)
    o_y = [P.dram("y%d" % i, [128, CH, 4, J], F32, kind="ExternalOutput") for i, (n, J) in enumerate(cfgs)]
    sk = P.sb([128, CH], F32); P.load(sk, sk[:], d_sk.ap())
    pbank = [P.ps([128, 8, 64], F32) for _ in range(4)]
    pc = [0]
    for i, (n, J) in enumerate(cfgs):
        P.push()
        W = 2 * n - 127
        zt = P.sb([128, CH, 4, J], BF16); P.load(zt, zt[:], d_z[i].ap())
        y = P.sb([128, CH, 4, J], F32)
        kss = [P.sb([128, W], BF16) for _ in range(2)]
        ms = [0] + [s * m for m in range(1, J) for s in (1, -1)]
        for c in range(CH):
            ks = kss[c % 2]
            src = bass.AP(tensor=d_L[i], offset=c * 2 * n, ap=[[1, 128], [1, W]])
            P.load(ks, ks[:], src, q=("sp" if c % 2 == 0 else "act"))
            pt = pbank[pc[0] % 4]; pc[0] += 1
            for idx, m in enumerate(ms):
                i0, i1 = max(0, m), min(J - 1, J - 1 + m)
                u0 = n + 128 * m - 127
                P.mm(pt, pt[:, 0:4, i0:i1 + 1], ks[:, u0:u0 + 128], zt[:, c, :, i0 - m:i1 + 1 - m],
                     start=(idx == 0), stop=(idx == len(ms) - 1), reads=[ks, zt])
            P.cp("dve" if c % 2 else "act", y, y[:, c], pt[:, 0:4, 0:J], reads=[pt])
        P.store(o_y[i].ap(), y, y[:])
        P.pop()
    maps = []
    zsrc = [zT_lat, zT_ctx]
    for core in range(NCORES):
        ch = slice(core * CH, (core + 1) * CH)
        m = {"sk": c_(np.broadcast_to(skip[ch], (128, CH)))}
        for i, (n, J) in enumerate(cfgs):
            m["L%d" % i] = np.ascontiguousarray(Ls[i][ch])
            z = np.asarray(zsrc[i][:, ch, :]).reshape(4, CH, J, 128)[:, :, :, ::-1]
            m["z%d" % i] = np.ascontiguousarray(z.transpose(3, 1, 0, 2))
        maps.append(m)
    res = run_spmd(P, maps)
    outs = []
    for i, (n, J) in enumerate(cfgs):
        yy = np.zeros((BATCH, HYW, n), np.float32)
        for core in range(NCORES):
            r = res[core]["y%d" % i]
            yy[:, core * CH:(core + 1) * CH, :] = r.transpose(2, 1, 3, 0).reshape(4, CH, n)
        outs.append(yy)
    return outs


def assemble_seq(res, key, feat_major=True):
    outs = []
    for b in range(BATCH):
        r0, r1 = res[2 * b][key], res[2 * b + 1][key]
        if feat_major:
            outs.append(np.concatenate([r0[..., :128], r1[..., :128], r0[..., 128:], r1[..., 128:]], -1))
        else:
            outs.append(np.concatenate([r0[:128], r1[:128], r0[128:], r1[128:]], 0))
    return np.stack(outs)


BIGNEG = 1.0e4


def launch_post(mode, ntile, feats, xres, vecs, lnvh, w_out, router_w, router_bias, w_gu_all, w_down_all, extra, nex=NE + 1):
    import os
    STG = int(os.environ.get('POST_STAGE', '9'))
    P = Prog()
    NTK = ntile * 128
    NEX = NE + 1
    NEXR = nex
    KC = 8 if mode == 0 else 16
    d_x = P.dram("xres", [NTK, D], F32)
    NS = 2 if mode == 0 else 1
    d_vec = P.dram("vecs", [128, 4, NS, D], F32)
    d_lnv = P.dram("lnv", [128, 4, D], F32)
    d_wo = P.dram("wo", [KC * 128, D], F32)
    d_rw = P.dram("rw", [D, NE], F32)
    d_rb = P.dram("rb", [128, NE], F32)
    d_wgu = P.dram("wgu", [NEXR, D, 512], F32)
    d_wd = P.dram("wd", [NEXR, 256, D], F32)
    d_idf = P.dram("idf", [128, 128], F32)
    if mode == 0:
        d_y = P.dram("yT", [512, NTK], F32); d_z = P.dram("zT", [512, NTK], BF16)
        d_x0 = P.dram("x0T", [512, NTK], BF16); d_at = P.dram("atT", [512, NTK], BF16)
        d_sk = P.dram("sk", [128, 4], F32)
    else:
        d_yf = P.dram("yfT", [2048, NTK], BF16); d_yb = P.dram("ybT", [2048, NTK], BF16)
        d_xs = P.dram("xsT", [2048, NTK], BF16); d_zz = P.dram("zzT", [2048, NTK], BF16)
        d_dg = P.dram("dg", [128, 16, 2], F32)
    o_x1 = P.dram("o_x1", [NTK, D], F32, kind="ExternalOutput")
    o_out = P.dram("o_out", [NTK, D], F32, kind="ExternalOutput")
    x1_tiles = [Tile(None, "x1d%d" % i) for i in range(ntile)]

    vec = P.sb([128, 4, NS, D], F32); lnv = P.sb([128, 4, D], F32)
    P.load(vec, vec[:], d_vec.ap()); P.load(lnv, lnv[:], d_lnv.ap())
    P.ts("dve", vec, vec[:, 1], vec[:, 1], 1.0, None, ALU.add, reads=[vec])
    idf = P.sb([128, 128], F32); P.load(idf, idf[:], d_idf.ap())
    idb = P.sb([128, 128], BF16); P.cp("dve", idb, idb[:], idf[:], reads=[idf])
    epsl = P.sb([128, 1], F32); P.memset("dve", epsl, epsl[:], LN_EPS)
    epsr = P.sb([128, 1], F32); P.memset("dve", epsr, epsr[:], RMS_EPS)
    ones = P.sb([128, 128], F32); P.memset("dve", ones, ones[:], 1.0)
    TP = 9 if mode == 0 else 8
    acc = P.sb([128, TP, D], F32)
    ffT = P.sb([128, 8, TP * 128], BF16)
    gates = P.sb([128, TP, NEX], F32)
    P.memset("dve", gates, gates[:, :, NE:NEX], 1.0)
    st = P.sb([128, 8], F32)
    pA = [P.ps([128, 512], F32) for _ in range(2)]
    pT = [P.ps([128, 2, 128], F32) for _ in range(2)]
    pD = [P.ps([128, 1024], F32) for _ in range(2)]

    def layer_norm(t, tt_, gi, bi, out_t, out_ap, sq):
        P.op("dve", lambda e: e.reduce_sum(out=st[:, 0:1], in_=tt_, axis=AX.X), reads=[t], writes=[st])
        P.act(sq, sq[:], tt_, AF.Square, reads=[t])
        P.op("dve", lambda e: e.reduce_sum(out=st[:, 1:2], in_=sq[:], axis=AX.X), reads=[sq], writes=[st])
        P.ts("dve", st, st[:, 2:3], st[:, 0:1], 1.0 / D, None, ALU.mult, reads=[st])
        P.tt("dve", st, st[:, 3:4], st[:, 2:3], st[:, 2:3], ALU.mult, reads=[st])
        P.stt("dve", st, st[:, 4:5], st[:, 1:2], 1.0 / D, st[:, 3:4], ALU.mult, ALU.subtract, reads=[st])
        P.act(st, st[:, 5:6], st[:, 4:5], AF.Sqrt, bias=epsl[:, 0:1], scale=1.0, reads=[st, epsl])
        P.op("dve", lambda e: e.reciprocal(out=st[:, 6:7], in_=st[:, 5:6]), reads=[st], writes=[st])
        P.ts("dve", t, tt_, tt_, st[:, 2:3], st[:, 6:7], ALU.subtract, ALU.mult, reads=[t, st])
        P.tt("dve", t, tt_, tt_, lnv[:, gi, :], ALU.mult, reads=[t, lnv])
        P.tt("dve", out_t, out_ap, tt_, lnv[:, bi, :], ALU.add, reads=[t, lnv])

    passes = [list(range(s, min(s + TP, ntile))) for s in range(0, ntile, TP)]
    for tiles in passes:
        P.push()
        wo = P.sb([128, KC, D], BF16)
        P.load(wo, wo[:], d_wo.ap().rearrange("(k p) n -> p k n", p=128), q="pool")
        rw = P.sb([128, 8, NE], F32); P.load(rw, rw[:], d_rw.ap().rearrange("(k p) n -> p k n", p=128))
        rb = P.sb([128, NE], F32); P.load(rb, rb[:], d_rb.ap())
        mT = [P.sb([128, KC, 128], BF16) for _ in range(1)] * 2
        xr = [P.sb([128, D], F32)] * 2
        tb = [P.sb([128, D], F32)] * 2
        sq = P.sb([128, D], F32)
        x1b = [P.sb([128, D], F32)] * 2
        ffb = [P.sb([128, D], F32)] * 2
        fTf = [P.sb([128, 8, 128], F32)] * 2
        scr = P.sb([128, NE], F32); cho = P.sb([128, NE], F32); mc = P.sb([128, NE], F32)
        m8 = P.sb([128, 8, 8], F32); gs = P.sb([128, 8], F32); gm = P.sb([128, 16], F32); t8 = P.sb([128, 8], F32)
        if mode == 0:
            sk = P.sb([128, 4], F32); P.load(sk, sk[:], d_sk.ap())
            fin = [[P.sb([128, 4, 128], F32), P.sb([128, 4, 128], BF16), P.sb([128, 4, 128], BF16)] for _ in range(2)]
            ftmp = P.sb([128, 4, 128], F32)
        else:
            dg = P.sb([128, 16, 2], F32); P.load(dg, dg[:], d_dg.ap())
            fin = [[P.sb([128, 16, 128], BF16), P.sb([128, 16, 128], BF16), P.sb([128, 16, 128], BF16), P.sb([128, 16, 128], BF16)]] * 2
            ftmp = P.sb([128, 16, 128], F32); fsq = P.sb([128, 16, 128], F32); frs = P.sb([128, 4, 128], F32)
        for li, ti in enumerate(tiles):
            c0 = ti * 128
            vs = 0 if (mode == 0 and ti == 0) else NS - 1
            m = mT[li % 2]; x_ = xr[li % 2]; t = tb[li % 2]; x1 = x1b[li % 2]; ff = ffb[li % 2]; ftf = fTf[li % 2]
            f = fin[li % 2]
            P.load(x_, x_[:], d_x.ap()[c0:c0 + 128, :])
            if mode == 0:
                P.load(f[0], f[0][:], d_y.ap()[:, c0:c0 + 128].rearrange("(k p) n -> p k n", p=128))
                P.load(f[1], f[1][:], d_z.ap()[:, c0:c0 + 128].rearrange("(k p) n -> p k n", p=128), q="act")
                P.load(f[2], f[2][:], d_x0.ap()[:, c0:c0 + 128].rearrange("(k p) n -> p k n", p=128), q="act")
                P.load(m, m[:, 4:8, :], d_at.ap()[:, c0:c0 + 128].rearrange("(k p) n -> p k n", p=128))
                for k in range(4):
                    P.stt("dve", ftmp, ftmp[:, k], f[1][:, k], sk[:, k:k + 1], f[0][:, k], ALU.mult, ALU.add, reads=[f[1], sk, f[0]])
                P.tt("pool", m, m[:, 0:4, :], ftmp[:], f[2][:], ALU.mult, reads=[ftmp, f[2]])
            else:
                P.load(f[0], f[0][:], d_yf.ap()[:, c0:c0 + 128].rearrange("(k p) n -> p k n", p=128))
                P.load(f[1], f[1][:], d_yb.ap()[:, c0:c0 + 128].rearrange("(k p) n -> p k n", p=128), q="act")
                P.load(f[2], f[2][:], d_xs.ap()[:, c0:c0 + 128].rearrange("(k p) n -> p k n", p=128))
                P.load(f[3], f[3][:], d_zz.ap()[:, c0:c0 + 128].rearrange("(k p) n -> p k n", p=128), q="act")
                P.tt("pool", ftmp, ftmp[:], f[0][:], f[1][:], ALU.add, reads=[f[0], f[1]])
                for k in range(16):
                    P.stt("dve", ftmp, ftmp[:, k], f[2][:, k], dg[:, k, 0:1], ftmp[:, k], ALU.mult, ALU.add, reads=[f[2], dg, ftmp])
                P.act(fsq, fsq[:], f[3][:], AF.Silu, reads=[f[3]])
                P.tt("pool", ftmp, ftmp[:], ftmp[:], fsq[:], ALU.mult, reads=[ftmp, fsq])
                P.act(fsq, fsq[:], ftmp[:], AF.Square, reads=[ftmp])
                for g in range(4):
                    pq = pA[g % 2]
                    for k in range(4):
                        P.mm(pq, pq[:, 0:128], ones[:], fsq[:, 4 * g + k, :], start=(k == 0), stop=(k == 3), reads=[ones, fsq])
                    P.act(frs, frs[:, g, :], pq[:, 0:128], AF.Sqrt, bias=epsr[:, 0:1], scale=1.0 / 512, reads=[pq, epsr])
                P.op("dve", lambda e, frs=frs: e.reciprocal(out=frs[:], in_=frs[:]), reads=[frs], writes=[frs])
                for k in range(16):
                    P.stt("dve", m, m[:, k, :], ftmp[:, k, :], dg[:, k, 1:2], frs[:, k // 4, :], ALU.mult, ALU.mult, reads=[ftmp, dg, frs])
            pd = pD[li % 2]
            for hf in range(2):
                for k in range(KC):
                    P.mm(pd, pd[:, hf * 512:(hf + 1) * 512], m[:, k, :], wo[:, k, hf * 512:(hf + 1) * 512],
                         start=(k == 0), stop=(k == KC - 1), reads=[m, wo])
            P.tt("dve", t, t[:], pd[:], vec[:, 0, vs, :], ALU.mult, reads=[pd, vec])
            P.stt("dve", t, t[:], x_[:], DN_ALPHA, t[:], ALU.mult, ALU.add, reads=[x_, t])
            layer_norm(t, t[:], 0, 1, x1, x1[:], sq)
            P.dma("sp", lambda e, x1=x1, c0=c0: e.dma_start(out=o_x1.ap()[c0:c0 + 128, :], in_=x1[:]), reads=[x1], writes=[x1_tiles[ti]])
            P.tt("dve", ff, ff[:], x1[:], vec[:, 1, vs, :], ALU.mult, reads=[x1, vec])
            P.tt("dve", ff, ff[:], ff[:], vec[:, 2, vs, :], ALU.add, reads=[ff, vec])
            if STG < 2:
                continue
            for k in range(8):
                pq = pA[k % 2]
                P.mm(pq, pq[:, 0:128], ff[:, k * 128:(k + 1) * 128], idf[:], reads=[ff, idf])
                P.cp("act", ftf, ftf[:, k, :], pq[:, 0:128], reads=[pq])
                P.cp("dve", ffT, ffT[:, k, li * 128:(li + 1) * 128], ftf[:, k, :], reads=[ftf])
            pq = pA[li % 2]
            for k in range(8):
                P.mm(pq, pq[:, 0:NE], ftf[:, k, :], rw[:, k, :], start=(k == 0), stop=(k == 7), reads=[ftf, rw])
            P.act(scr, scr[:], pq[:, 0:NE], AF.Sigmoid, reads=[pq])
            P.tt("dve", cho, cho[:], scr[:], rb[:], ALU.add, reads=[scr, rb])
            if STG < 3:
                continue
            for g in range(8):
                P.op("dve", lambda e, g=g: e.max(out=m8[:, g, :], in_=cho[:, 32 * g:32 * g + 32]), reads=[cho], writes=[m8])
            P.tt("dve", gs, gs[:], m8[:, :, 0], m8[:, :, 1], ALU.add, reads=[m8])
            P.op("dve", lambda e: e.max(out=t8[:], in_=gs[:]), reads=[gs], writes=[t8])
            P.ts("dve", gm, gm[:, 0:8], gs[:], t8[:, 3:4], None, ALU.is_ge, reads=[gs, t8])
            P.ts("dve", gm, gm[:, 8:16], gm[:, 0:8], -1.0, BIGNEG, ALU.add, ALU.mult, reads=[gm])
            for g in range(8):
                P.ts("dve", mc, mc[:, 32 * g:32 * g + 32], cho[:, 32 * g:32 * g + 32], gm[:, g:g + 1], gm[:, 8 + g:9 + g],
                     ALU.mult, ALU.add, reads=[cho, gm])
            P.op("dve", lambda e: e.max(out=t8[:], in_=mc[:]), reads=[mc], writes=[t8])
            P.ts("dve", mc, mc[:], mc[:], t8[:, 7:8], None, ALU.is_ge, reads=[mc, t8])
            P.tt("dve", scr, scr[:], scr[:], mc[:], ALU.mult, reads=[scr, mc])
            P.op("dve", lambda e: e.reduce_sum(out=gs[:, 0:1], in_=scr[:], axis=AX.X), reads=[scr], writes=[gs])
            P.op("dve", lambda e: e.reciprocal(out=gs[:, 1:2], in_=gs[:, 0:1]), reads=[gs], writes=[gs])
            P.ts("dve", gates, gates[:, li, 0:NE], scr[:], gs[:, 1:2], ROUTED_SCALE, ALU.mult, ALU.mult, reads=[scr, gs])
        P.pop()
        P.push()
        wgs = [P.sb([128, 8, 512], BF16) for _ in range(2)]
        wds = [P.sb([128, 2, D], BF16) for _ in range(2)]
        sgs = [P.sb([128, 256], F32) for _ in range(2)]
        hs = [P.sb([128, 256], BF16) for _ in range(2)]
        hTs = [P.sb([128, 2, 128], BF16) for _ in range(2)]
        cnt = 0
        for e_ in range(NEXR if STG >= 4 else 0):
            wg = wgs[e_ % 2]; wd = wds[e_ % 2]
            P.load(wg, wg[:], d_wgu.ap()[e_].rearrange("(k p) n -> p k n", p=128), q="pool")
            P.load(wd, wd[:], d_wd.ap()[e_].rearrange("(k p) n -> p k n", p=128), q="pool")
            for li, ti in enumerate(tiles):
                pa = pA[cnt % 2]; pt = pT[cnt % 2]; pd = pD[cnt % 2]
                sg = sgs[cnt % 2]; h = hs[cnt % 2]; hT = hTs[cnt % 2]; cnt += 1
                for k in range(8):
                    P.mm(pa, pa[:, :], ffT[:, k, li * 128:(li + 1) * 128], wg[:, k, :], start=(k == 0), stop=(k == 7), reads=[ffT, wg])
                P.act(sg, sg[:], pa[:, 0:256], AF.Silu, reads=[pa])
                P.stt("dve", h, h[:], pa[:, 256:512], gates[:, li, e_:e_ + 1], sg[:], ALU.mult, ALU.mult, reads=[pa, gates, sg])
                for k in range(2):
                    P.mm(pt, pt[:, k, :], h[:, k * 128:(k + 1) * 128], idb[:], reads=[h, idb])
                P.cp("act", hT, hT[:], pt[:], reads=[pt])
                for hf in range(2):
                    for k in range(2):
                        P.mm(pd, pd[:, hf * 512:(hf + 1) * 512], hT[:, k, :], wd[:, k, hf * 512:(hf + 1) * 512],
                             start=(k == 0), stop=(k == 1), reads=[hT, wd])
                if e_ == 0:
                    P.cp("dve", acc, acc[:, li, :], pd[:], reads=[pd])
                else:
                    P.tt("dve", acc, acc[:, li, :], acc[:, li, :], pd[:], ALU.add, reads=[acc, pd])
        P.pop()
        P.push()
        x1r = [P.sb([128, D], F32) for _ in range(2)]
        ob = [P.sb([128, D], F32) for _ in range(2)]
        sq = P.sb([128, D], F32)
        for li, ti in enumerate(tiles if STG >= 5 else []):
            c0 = ti * 128
            vs = 0 if (mode == 0 and ti == 0) else NS - 1
            x1 = x1r[li % 2]; o = ob[li % 2]
            P.dma("sp", lambda e, x1=x1, c0=c0: e.dma_start(out=x1[:], in_=o_x1.ap()[c0:c0 + 128, :]), reads=[x1_tiles[ti]], writes=[x1])
            P.tt("dve", acc, acc[:, li, :], acc[:, li, :], vec[:, 3, vs, :], ALU.mult, reads=[acc, vec])
            P.stt("dve", acc, acc[:, li, :], x1[:], DN_ALPHA, acc[:, li, :], ALU.mult, ALU.add, reads=[x1, acc])
            layer_norm(acc, acc[:, li, :], 2, 3, o, o[:], sq)
            P.store(o_out.ap()[c0:c0 + 128, :], o, o[:])
        P.pop()

    idh = np.eye(128, dtype=np.float32)
    maps = []
    for core in range(NCORES):
        m = {"xres": c_(xres[core]), "vecs": c_(vecs[core]), "lnv": c_(lnv_h(lnvh)), "wo": c_(w_out), "rw": c_(router_w),
             "rb": c_(np.broadcast_to(router_bias, (128, NE))), "wgu": w_gu_all, "wd": w_down_all, "idf": idh}
        m.update(feats[core])
        m.update(extra)
        maps.append(m)
    res = run_spmd(P, maps)
    if os.environ.get('POST_X1'):
        return [r["o_x1"] for r in res]
    return [r["o_out"] for r in res]


def lnv_h(v):
    return np.broadcast_to(np.stack(v)[None], (128, 4, D))


def _vecs(modl, b):
    v = np.stack([np.stack([modl[4, j], modl[b, j]]) for j in (2, 4, 3, 5)])
    return np.broadcast_to(v[None], (128, 4, 2, D))


def run_layer0(inp, mod, Ls):
    x, ctx = inp["x"], inp["ctx"]
    res = launch_pre0(x, ctx, mod[0], inp["a_w_in"][0], inp["hy_conv_w"][0], inp["hy_conv_b"][0],
                      inp["mla_q_norm"][0], inp["mla_w_qb"][0], inp["mla_kv_norm"][0], inp["mla_w_kvb"][0])
    QT = assemble_seq(res, "o_q"); KT = assemble_seq(res, "o_k")
    V = assemble_seq(res, "o_v", feat_major=False).reshape(BATCH, NK, 8, 64)
    att = launch_attn(QT, KT, V)
    zT = assemble_seq(res, "o_z")
    ylat, yctx = launch_hyconv(Ls, zT[:, :, CTX:], zT[:, :, :CTX], inp["hy_skip"][0])
    feats, xres, vecs = [], [], []
    for core in range(NCORES):
        b, s = core // 2, core % 2
        cs = slice(s * 128, (s + 1) * 128); ls = slice(s * 4096, (s + 1) * 4096)
        attb = att[b].reshape(512, NK)
        feats.append({
            "yT": c_(np.concatenate([yctx[b][:, cs], ylat[b][:, ls]], 1)),
            "zT": res[core]["o_z"], "x0T": res[core]["o_x0"],
            "atT": np.ascontiguousarray(np.concatenate([attb[:, cs], attb[:, CTX + s * 4096:CTX + (s + 1) * 4096]], 1)),
        })
        xres.append(np.concatenate([ctx[b][cs], x[b][ls]], 0))
        vecs.append(_vecs(mod[0], b))
    l = 0
    wgu = np.concatenate([inp["exp_w_gu"][l], inp["sh_w_gu"][l][None]], 0)
    wd = np.concatenate([inp["exp_w_down"][l], inp["sh_w_down"][l][None]], 0)
    extra = {"sk": c_(inp["hy_skip"][0].reshape(4, 128).T)}
    outs = launch_post(0, 33, feats, xres, vecs, [inp["ln_mix_g"][l], inp["ln_mix_b"][l], inp["ln_ffn_g"][l], inp["ln_ffn_b"][l]],
                       inp["a_w_out"][0], inp["router_w"][l], inp["router_bias"][l], wgu, wd, extra)
    x2 = np.zeros_like(x); c2 = np.zeros_like(ctx)
    for core in range(NCORES):
        b, s = core // 2, core % 2
        c2[b, s * 128:(s + 1) * 128] = outs[core][:128]
        x2[b, s * 4096:(s + 1) * 4096] = outs[core][128:]
    return x2, c2


def launch_pre1(x, ctx, mod1, w_in, conv_w, conv_b, dt_bias_f, dt_bias_b):
    P = Prog()
    NT = NT0
    d_xT = P.dram("xT", [128, 8, NT], F32)
    d_mod = P.dram("mod", [128, 8, 4], F32)
    d_win = P.dram("win", [40, 128, 8, 128], F32)
    d_wdt = P.dram("wdt", [128, 8, 64], F32)
    d_cw = P.dram("cw", [128, 24, 4], F32)
    d_hm = P.dram("hm", [128, 4], F32)
    d_db = P.dram("db", [64, 1], F32)
    o_z = P.dram("o_z", [2048, NTI], BF16, kind="ExternalOutput")
    o_xbc = P.dram("o_xbc", [3072, NTI], BF16, kind="ExternalOutput")
    o_dt = P.dram("o_dt", [64, NTI], F32, kind="ExternalOutput")
    tiles = _ntiles(NT)
    pbank = [P.ps([128, 512], F32) for _ in range(8)]
    pb_i = [0]

    def bank():
        pb_i[0] = (pb_i[0] + 1) % 8
        return pbank[pb_i[0]]

    hT = P.sb([128, 8, NT], BF16, "hT")
    mods = P.sb([128, 8, 4], F32); P.load(mods, mods[:], d_mod.ap())
    m1 = P.sb([128, 8, 2], F32)
    for j in range(2):
        P.ts("dve", m1, m1[:, :, j:j + 1], mods[:, :, 2 * j:2 * j + 1], 1.0, None, ALU.add, reads=[mods])
    P.push()
    xin = [P.sb([128, 8, 512], F32) for _ in range(2)]
    for ti, (s0, w) in enumerate(tiles):
        xt = xin[ti % 2]
        P.load(xt, xt[:, :, 0:w], d_xT.ap()[:, :, s0:s0 + w])
        for k in range(8):
            for (a, b_, j) in ((s0, min(s0 + w, 130), 0), (max(s0, 130), s0 + w, 1)):
                if b_ <= a:
                    continue
                P.ts("dve" if k % 2 == 0 else "pool", hT, hT[:, k, a:b_], xt[:, k, a - s0:b_ - s0],
                     m1[:, k, j:j + 1], mods[:, k, 2 * j + 1:2 * j + 2], ALU.mult, ALU.add, reads=[xt, m1, mods])
    P.pop()
    cw = P.sb([128, 24, 4], F32); hm = P.sb([128, 4], F32); db = P.sb([64, 1], F32)
    P.load(cw, cw[:], d_cw.ap()); P.load(hm, hm[:], d_hm.ap()); P.load(db, db[:], d_db.ap())
    wbuf = [P.sb([128, 8, 128], BF16) for _ in range(2)]
    urow = [P.sb([128, NT], F32) for _ in range(2)]
    cv = P.sb([128, NT], F32)
    ob = [P.sb([128, NT], BF16) for _ in range(2)]
    for oc in range(40):
        wt = wbuf[oc % 2]; ur = urow[oc % 2]; o = ob[oc % 2]
        P.load(wt, wt[:], d_win.ap()[oc], q="pool")
        cnt = [0]
        for (s0, w) in tiles:
            pt = bank()
            for k in range(8):
                P.mm(pt, pt[:, 0:w], wt[:, k, :], hT[:, k, s0:s0 + w], start=(k == 0), stop=(k == 7), reads=[wt, hT])
            cnt[0] += 1
            P.cp("act" if cnt[0] % 2 else "dve", ur, ur[:, s0:s0 + w], pt[:, 0:w], reads=[pt])
        if oc < 16:
            P.cp("pool", o, o[:], ur[:], reads=[ur])
            dst, r0 = o_z, oc * 128
        else:
            c = oc - 16
            for j, col in enumerate((0, 129, 130, 4227)):
                P.tt("dve", ur, ur[:, col:col + 1], ur[:, col:col + 1], hm[:, j:j + 1], ALU.mult, reads=[ur, hm])
            P.act(cv, cv[:], ur[:], AF.Identity, bias=cw[:, c, 3:4], scale=cw[:, c, 1:2], reads=[ur, cw])
            P.stt("dve", cv, cv[:, 1:NT], ur[:, 0:NT - 1], cw[:, c, 0:1], cv[:, 1:NT], ALU.mult, ALU.add, reads=[ur, cw, cv])
            P.stt("dve", cv, cv[:, 0:NT - 1], ur[:, 1:NT], cw[:, c, 2:3], cv[:, 0:NT - 1], ALU.mult, ALU.add, reads=[ur, cw, cv])
            P.act(o, o[:], cv[:], AF.Silu, reads=[cv])
            dst, r0 = o_xbc, c * 128
        for (a, b_, oo) in INT_SEGS:
            P.store(dst.ap()[r0:r0 + 128, oo:oo + (b_ - a)], o, o[:, a:b_])
    wdt = P.sb([128, 8, 64], BF16); P.load(wdt, wdt[:], d_wdt.ap(), q="pool")
    xd = urow[0]; ab = urow[1]; sp = cv
    for (s0, w) in tiles:
        pt = bank()
        for k in range(8):
            P.mm(pt, pt[0:64, 0:w], wdt[:, k, :], hT[:, k, s0:s0 + w], start=(k == 0), stop=(k == 7), reads=[wdt, hT])
        P.ts("dve", xd, xd[0:64, s0:s0 + w], pt[0:64, 0:w], db[:, 0:1], None, ALU.add, reads=[pt, db])
    P.act(ab, ab[0:64, :], xd[0:64, :], AF.Abs, reads=[xd])
    P.act(ab, ab[0:64, :], ab[0:64, :], AF.Exp, scale=-1.0, reads=[ab])
    P.act(ab, ab[0:64, :], ab[0:64, :], AF.Ln, bias=1.0, reads=[ab])
    P.ts("dve", sp, sp[0:64, :], xd[0:64, :], 0.0, None, ALU.max, reads=[xd])
    P.tt("dve", sp, sp[0:64, :], sp[0:64, :], ab[0:64, :], ALU.add, reads=[sp, ab])
    for (a, b_, oo) in INT_SEGS:
        P.store(o_dt.ap()[:, oo:oo + (b_ - a)], sp, sp[0:64, a:b_])

    win_h = c_(np.stack([pk(w_in[:, oc * 128:(oc + 1) * 128]) for oc in range(40)]))
    wdt_h = pk(w_in[:, 5120:5184])
    cw_h = c_(np.concatenate([conv_w.T.reshape(24, 128, 3), conv_b.reshape(24, 128, 1)], -1).transpose(1, 0, 2))
    db_h = c_(np.concatenate([dt_bias_f, dt_bias_b])[:, None])
    maps = []
    for core in range(NCORES):
        b, s = core // 2, core % 2
        ci, li = _core_tokens(b, s)
        xc, okc = _gather_rows(ctx[b], ci); xl, okl = _gather_rows(x[b], li)
        xw = np.concatenate([xc, xl], 0)
        hmv = np.array([okc[0], okc[-1], okl[0], okl[-1]], np.float32)
        modv = np.stack([mod1[4, 1], mod1[4, 0], mod1[b, 1], mod1[b, 0]], 1)
        maps.append({"xT": c_(xw.T.reshape(8, 128, NT).transpose(1, 0, 2)), "mod": c_(modv.reshape(8, 128, 4).transpose(1, 0, 2)),
                     "win": win_h, "wdt": wdt_h, "cw": cw_h, "hm": c_(np.broadcast_to(hmv, (128, 4))), "db": db_h})
    return run_spmd(P, maps)


NCH = NK // 128


def launch_scan(xs_tm, dt_tm, B_tm, BT, CT, alog):
    P = Prog()
    d_x = P.dram("x", [NK, 2048], BF16)
    d_dt = P.dram("dt", [128, NCH, 32], F32)
    d_B = P.dram("B", [NK, 512], BF16)
    d_BT = P.dram("BT", [512, NK], BF16)
    d_CT = P.dram("CT", [512, NK], BF16)
    d_al = P.dram("al", [128, 32], F32)
    d_tri = P.dram("tri", [128, 128], F32)
    d_sel = P.dram("sel", [32, 32, 128], F32)
    o_y = P.dram("o_y", [SEQ, 2048], BF16, kind="ExternalOutput")
    dt = P.sb([128, NCH, 32], F32); P.load(dt, dt[:], d_dt.ap())
    A = P.sb([128, 32], F32); P.load(A, A[:], d_al.ap())
    P.act(A, A[:], A[:], AF.Exp, reads=[A])
    P.ts("dve", A, A[:], A[:], -1.0, None, ALU.mult, reads=[A])
    tri = P.sb([128, 128], F32); P.load(tri, tri[:], d_tri.ap())
    sel = P.sb([32, 32, 128], F32); P.load(sel, sel[:], d_sel.ap())
    ones = P.sb([128, 128], F32); P.memset("dve", ones, ones[:], 1.0)
    S32 = P.sb([128, 2048], F32); P.memset("dve", S32, S32[:], 0.0)
    Sb = P.sb([128, 2048], BF16); P.memset("pool", Sb, Sb[:], 0.0)
    xb = [P.sb([128, 2048], BF16) for _ in range(2)]
    Bb = [P.sb([128, 512], BF16) for _ in range(2)]
    BTb = [P.sb([128, 4, 128], BF16) for _ in range(2)]
    CTb = [P.sb([128, 4, 128], BF16) for _ in range(2)]
    a_ = P.sb([128, 32], F32); acs = P.sb([128, 32], F32); acsT = P.sb([32, 128], F32); sm = P.sb([128, 256], F32)
    eacs = P.sb([128, 32], F32); toend = P.sb([128, 32], F32); etot = P.sb([128, 32], F32)
    xdt = P.sb([128, 2048], BF16); xw = P.sb([128, 2048], BF16)
    cbm = P.sb([128, 4, 128], F32)
    dd = [P.sb([128, 4, 128], F32) for _ in range(2)]
    MT = [P.sb([128, 4, 128], BF16) for _ in range(2)]
    yoff = P.sb([128, 2048], F32)
    yb = [P.sb([128, 2048], BF16) for _ in range(2)]
    pS = P.ps([128, 512], F32)
    pC = P.ps([128, 4, 128], F32)
    pAc = [P.ps([128, 4, 128], F32) for _ in range(2)]
    pY = [P.ps([128, 512], F32) for _ in range(2)]
    pG = [P.ps([128, 512], F32) for _ in range(2)]
    for c in range(NCH):
        x_ = xb[c % 2]; B_ = Bb[c % 2]; BT_ = BTb[c % 2]; CT_ = CTb[c % 2]; y_ = yb[c % 2]
        cs = slice(c * 128, (c + 1) * 128)
        P.load(x_, x_[:], d_x.ap()[cs, :])
        P.load(B_, B_[:], d_B.ap()[cs, :], q="act")
        P.load(BT_, BT_[:], d_BT.ap()[:, cs].rearrange("(g p) n -> p g n", p=128))
        P.load(CT_, CT_[:], d_CT.ap()[:, cs].rearrange("(g p) n -> p g n", p=128), q="act")
        P.tt("dve", a_, a_[:], dt[:, c, :], A[:], ALU.mult, reads=[dt, A])
        if c == 0:
            P.op("dve", lambda e: e.memset(sm[:], 0.0), writes=[sm])
        P.mm(pS, pS[:, 0:32], tri[:], a_[:], reads=[tri, a_])
        P.mm(pS, pS[:, 32:64], ones[:], a_[:], reads=[ones, a_])
        P.mm(pS, pS[0:32, 128:256], a_[:], tri[:], reads=[a_, tri])
        P.cp("dve", sm, sm[:], pS[:, 0:256], reads=[pS])
        P.cp("pool", acs, acs[:], sm[:, 0:32], reads=[sm])
        P.cp("pool", acsT, acsT[:], sm[0:32, 128:256], reads=[sm])
        P.act(eacs, eacs[:], sm[:, 0:32], AF.Exp, reads=[sm])
        P.act(etot, etot[:], sm[:, 32:64], AF.Exp, reads=[sm])
        P.tt("dve", toend, toend[:], sm[:, 32:64], sm[:, 0:32], ALU.subtract, reads=[sm])
        P.act(toend, toend[:], toend[:], AF.Exp, reads=[toend])
        P.tt("dve", xdt, xdt[:].rearrange("p (h d) -> p h d", d=64), x_[:].rearrange("p (h d) -> p h d", d=64),
             dt[:, c, :].unsqueeze(2).to_broadcast([128, 32, 64]), ALU.mult, reads=[x_, dt])
        P.tt("pool", xw, xw[:].rearrange("p (h d) -> p h d", d=64), xdt[:].rearrange("p (h d) -> p h d", d=64),
             toend[:].unsqueeze(2).to_broadcast([128, 32, 64]), ALU.mult, reads=[xdt, toend])
        for g in range(4):
            P.mm(pC, pC[:, g, :], BT_[:, g, :], CT_[:, g, :], reads=[BT_, CT_])
        P.tt("dve", cbm, cbm[:], pC[:], tri[:].unsqueeze(1).to_broadcast([128, 4, 128]), ALU.mult, reads=[pC, tri])
        for g in range(4):
            py = pY[g % 2]
            for q4 in range(2):
                h0 = g * 8 + q4 * 4
                pa = pAc[q4]; d_ = dd[q4]; mt = MT[q4]
                for j in range(4):
                    P.mm(pa, pa[:, j, :], sel[:, h0 + j, :], acsT[:], reads=[sel, acsT])
                P.tt("dve", d_, d_[:], pa[:], acs[:, h0:h0 + 4].unsqueeze(2).to_broadcast([128, 4, 128]), ALU.subtract, reads=[pa, acs])
                P.act(d_, d_[:], d_[:], AF.Exp, reads=[d_])
                P.stt("dve", mt, mt[:], d_[:], 1.0, cbm[:, g, :].unsqueeze(1).to_broadcast([128, 4, 128]), ALU.min, ALU.mult, reads=[d_, cbm])
                for j in range(4):
                    h = h0 + j
                    P.mm(py, py[:, (h % 8) * 64:(h % 8 + 1) * 64], mt[:, j, :], xdt[:, h * 64:(h + 1) * 64], reads=[mt, xdt])
            if c >= 2:
                po = pG[g % 2]
                P.mm(po, po[:], CT_[:, g, :], Sb[:, g * 512:(g + 1) * 512], reads=[CT_, Sb])
                P.tt("dve", yoff, yoff[:, g * 512:(g + 1) * 512].rearrange("p (h d) -> p h d", d=64),
                     po[:].rearrange("p (h d) -> p h d", d=64),
                     eacs[:, g * 8:(g + 1) * 8].unsqueeze(2).to_broadcast([128, 8, 64]), ALU.mult, reads=[po, eacs])
                P.tt("dve", y_, y_[:, g * 512:(g + 1) * 512], py[:], yoff[:, g * 512:(g + 1) * 512], ALU.add, reads=[py, yoff])
        if c >= 2:
            P.store(o_y.ap()[(c - 2) * 128:(c - 1) * 128, :], y_, y_[:])
        if c == 0:
            pass
        P.tt("pool", S32, S32[:].rearrange("p (h d) -> p h d", d=64), S32[:].rearrange("p (h d) -> p h d", d=64),
             etot[:].unsqueeze(2).to_broadcast([128, 32, 64]), ALU.mult, reads=[S32, etot])
        for g in range(4):
            po = pG[g % 2]
            P.mm(po, po[:], B_[:, g * 128:(g + 1) * 128], xw[:, g * 512:(g + 1) * 512], reads=[B_, xw])
            P.tt("dve", S32, S32[:, g * 512:(g + 1) * 512], S32[:, g * 512:(g + 1) * 512], po[:], ALU.add, reads=[S32, po])
        P.cp("act", Sb, Sb[:], S32[:], reads=[S32])
    trih = np.triu(np.ones((128, 128), np.float32))
    selh = np.zeros((32, 32, 128), np.float32)
    for h in range(32):
        selh[h, h, :] = 1.0
    maps = []
    for core in range(NCORES):
        maps.append({"x": np.ascontiguousarray(xs_tm[core]), "dt": c_(dt_tm[core].reshape(NCH, 128, 32).transpose(1, 0, 2)),
                     "B": np.ascontiguousarray(B_tm[core]), "BT": np.ascontiguousarray(BT[core]), "CT": np.ascontiguousarray(CT[core]),
                     "al": c_(np.broadcast_to(alog[core], (128, 32))), "tri": trih, "sel": selh})
    res = run_spmd(P, maps)
    return [r["o_y"] for r in res]


def run_layer1(inp, mod, x2, c2):
    res = launch_pre1(x2, c2, mod[1], inp["ssd_w_in"][0], inp["ssd_conv_w"][0], inp["ssd_conv_b"][0],
                      inp["ssd_dt_bias_f"][0], inp["ssd_dt_bias_b"][0])
    xbc = assemble_seq(res, "o_xbc")
    dtT = assemble_seq(res, "o_dt")
    xs_tm, dt_tm, B_tm, BT, CT, alog = [], [], [], [], [], []
    for core in range(NCORES):
        b, d_ = core // 2, core % 2
        if d_ == 0:
            order = np.arange(NK)
        else:
            order = np.concatenate([np.arange(CTX)[::-1], CTX + np.arange(SEQ)[::-1]])
        xo = xbc[b][:, order]
        xs_tm.append(xo[:2048].T); B_tm.append(xo[2048:2560].T); BT.append(xo[2048:2560]); CT.append(xo[2560:3072])
        dt_tm.append(dtT[b][32 * d_:32 * (d_ + 1)][:, order].T)
        alog.append(inp["ssd_a_log_f"][0] if d_ == 0 else inp["ssd_a_log_b"][0])
    ys = launch_scan(xs_tm, dt_tm, B_tm, BT, CT, alog)
    feats, xres, vecs = [], [], []
    l = 1
    for core in range(NCORES):
        b, s = core // 2, core % 2
        ls = slice(s * 4096, (s + 1) * 4096)
        yf = ys[2 * b][ls]; ybk = ys[2 * b + 1][::-1][ls]
        feats.append({"yfT": np.ascontiguousarray(yf.T), "ybT": np.ascontiguousarray(ybk.T),
                      "xsT": np.ascontiguousarray(res[core]["o_xbc"][:2048, 128:]),
                      "zzT": np.ascontiguousarray(res[core]["o_z"][:, 128:])})
        xres.append(x2[b][ls])
        vecs.append(_vecs(mod[1], b)[:, :, 1:2, :])
    wgu = np.concatenate([inp["exp_w_gu"][l], inp["sh_w_gu"][l][None]], 0)
    wd = np.concatenate([inp["exp_w_down"][l], inp["sh_w_down"][l][None]], 0)
    dfull = np.repeat(inp["ssd_d"][0], 64)
    extra = {"dg": c_(np.stack([dfull.reshape(16, 128).T, inp["ssd_norm_g"][0].reshape(16, 128).T], -1))}
    outs = launch_post(1, 32, feats, xres, vecs, [inp["ln_mix_g"][l], inp["ln_mix_b"][l], inp["ln_ffn_g"][l], inp["ln_ffn_b"][l]],
                       inp["ssd_w_out"][0], inp["router_w"][l], inp["router_bias"][l], wgu, wd, extra)
    out = np.zeros((BATCH, SEQ, D), np.float32)
    for core in range(NCORES):
        b, s = core // 2, core % 2
        out[b, s * 4096:(s + 1) * 4096] = outs[core]
    return out


def kernel(**inp):
    inp = {k: np.asarray(v) for k, v in inp.items()}
    mod = launch_mod(inp["c"], inp["c_ctx"], inp["mod_w"], inp["mod_b"])
    Ls = launch_filt(*[inp[k][0] for k in ["hy_filt_w1", "hy_filt_b1", "hy_filt_freq1", "hy_filt_w2", "hy_filt_b2",
                                           "hy_filt_freq2", "hy_filt_w3"]])
    x2, c2 = run_layer0(inp, mod, Ls)
    return run_layer1(inp, mod, x2, c2)
```

```python
import math
from contextlib import ExitStack
import numpy as np
import ml_dtypes
import concourse.bass as bass
import concourse.mybir as mybir
from concourse.bass_utils import run_bass_kernel_spmd

F32 = mybir.dt.float32
BF16 = mybir.dt.bfloat16
I32 = mybir.dt.int32
AF = mybir.ActivationFunctionType
ALU = mybir.AluOpType
AX = mybir.AxisListType
NPBF = ml_dtypes.bfloat16

ENGS = ["pe", "act", "dve", "pool", "sp"]
NDSEM = {"sp": 8, "pool": 6, "act": 2}
NCORES = 8

D = 1024
BATCH = 4
SEQ = 8192
CTX = 256
DEPTH = 2
GRID_W = 64
HYW = 512
HY_EMB = 33
HY_BANDS = 16
HY_DECAY_MIN = math.log(1e-2) / 1.5
HY_DECAY_MAX = math.log(1e-2) / 0.3
MLA_SCALE = 96 ** -0.5
DN_ALPHA = (2 * DEPTH) ** 0.25
LN_EPS = 1e-5
RMS_EPS = 1e-6
NE = 256
ROUTED_SCALE = 2.5
PI = math.pi


class Tile:
    def __init__(self, t, name):
        self.t = t
        self.name = name
        self.w = None
        self.r = {}

    def __getitem__(self, idx):
        return self.t[idx]


class Prog:
    def __init__(self):
        self.nc = bass.Bass("TRN2", target_bir_lowering=False)
        self.stk = [ExitStack()]
        self.ops = {e: [] for e in ENGS}
        self.cnt = {e: 0 for e in ENGS}
        self.seen = {e: {} for e in ENGS}
        self.sems = {}
        for e in ["pe", "act", "dve", "pool"]:
            self.sems[e] = self.stk[0].enter_context(self.nc.semaphore("s_" + e))
        self.dtot = {}
        self.drot = {q: 0 for q in NDSEM}
        for q, n in NDSEM.items():
            for i in range(n):
                k = "d_%s_%d" % (q, i)
                self.sems[k] = self.stk[0].enter_context(self.nc.semaphore(k))
                self.dtot[k] = 0
        self.nalloc = 0

    def dram(self, name, shape, dtype, kind="ExternalInput"):
        return self.nc.dram_tensor(name, list(shape), dtype, kind=kind)

    def push(self):
        self.stk.append(ExitStack())

    def pop(self):
        self.barrier()
        self.stk.pop().close()

    def sb(self, shape, dtype, name=None):
        self.nalloc += 1
        name = (name or "sb") + "_%d" % self.nalloc
        t = self.stk[-1].enter_context(self.nc.sbuf_tensor(name, list(shape), dtype))
        return Tile(t, name)

    def ps(self, shape, dtype, name=None):
        self.nalloc += 1
        name = (name or "ps") + "_%d" % self.nalloc
        t = self.stk[-1].enter_context(self.nc.psum_tensor(name, list(shape), dtype))
        return Tile(t, name)

    def _collect(self, eng, reads, writes):
        waits = {}

        def add(ev, same_ok):
            if ev is None:
                return
            k, v = ev
            if k == eng and not same_ok:
                return
            if waits.get(k, 0) < v:
                waits[k] = v

        for t in reads:
            add(t.w, True)
        for t in writes:
            add(t.w, False)
            for k, v in t.r.items():
                add((k, v), False)
        need = []
        for k, v in waits.items():
            if self.seen[eng].get(k, 0) >= v:
                continue
            self.seen[eng][k] = v
            need.append((k, v))
        return need

    def _commit(self, ev, reads, writes):
        k, v = ev
        for t in reads:
            if t.r.get(k, 0) < v:
                t.r[k] = v
        for t in writes:
            t.w = ev
            t.r = {}

    def op(self, eng, fn, reads=(), writes=()):
        need = self._collect(eng, reads, writes)
        self.cnt[eng] += 1
        ev = (eng, self.cnt[eng])
        self.ops[eng].append((need, fn, eng))
        self._commit(ev, reads, writes)
        return ev

    def dma(self, q, fn, reads=(), writes=()):
        need = self._collect(q, reads, writes)
        i = self.drot[q]
        self.drot[q] = (i + 1) % NDSEM[q]
        k = "d_%s_%d" % (q, i)
        if self.dtot[k] > 0 and self.seen[q].get(k, 0) < self.dtot[k]:
            self.seen[q][k] = self.dtot[k]
            need.append((k, self.dtot[k]))
        self.dtot[k] += 16
        ev = (k, self.dtot[k])
        self.ops[q].append((need, fn, k))
        self._commit(ev, reads, writes)
        return ev

    def barrier(self):
        allev = [(k, tot) for k, tot in self.dtot.items() if tot > 0]
        allev += [(e, self.cnt[e]) for e in ["pe", "act", "dve", "pool"] if self.cnt[e] > 0]
        for e in ENGS:
            need = []
            for k, v in allev:
                if k == e:
                    continue
                if self.seen[e].get(k, 0) >= v:
                    continue
                self.seen[e][k] = v
                need.append((k, v))
            if need:
                self.ops[e].append((need, None, None))

    def build(self):
        self.barrier()
        nc = self.nc
        sems = self.sems

        def replay(eng_name, e):
            for need, fn, inc in self.ops[eng_name]:
                for k, v in need:
                    e.wait_ge(sems[k], v)
                if fn is None:
                    continue
                ins = fn(e)
                if inc is not None:
                    ins.then_inc(sems[inc], 16 if inc.startswith("d_") else 1)

        with nc.Block() as block:
            @block.tensor
            def _(e):
                replay("pe", e)

            @block.scalar
            def _(e):
                replay("act", e)

            @block.vector
            def _(e):
                replay("dve", e)

            @block.gpsimd
            def _(e):
                replay("pool", e)

            @block.sync
            def _(e):
                replay("sp", e)
        while self.stk:
            self.stk.pop().close()
        return nc

    def load(self, t, out, in_, q="sp"):
        self.dma(q, lambda e: e.dma_start(out=out, in_=in_), writes=[t])

    def store(self, out, t, in_, q="sp"):
        self.dma(q, lambda e: e.dma_start(out=out, in_=in_), reads=[t])

    def mm(self, pt, out, lhsT, rhs, start=True, stop=True, reads=()):
        self.op("pe", lambda e: e.matmul(out, lhsT=lhsT, rhs=rhs, start=start, stop=stop),
                reads=reads, writes=[pt])

    def tr(self, pt, out, in_, ident, reads=()):
        self.op("pe", lambda e: e.transpose(out, in_, ident), reads=reads, writes=[pt])

    def act(self, ot, out, in_, func, bias=0.0, scale=1.0, reads=(), accum=None):
        if accum is None:
            fn = lambda e: e.activation(out=out, in_=in_, func=func, bias=bias, scale=scale)
        else:
            fn = lambda e: e.activation(out=out, in_=in_, func=func, bias=bias, scale=scale, accum_out=accum)
        self.op("act", fn, reads=reads, writes=[ot] if not isinstance(ot, (list, tuple)) else list(ot))

    def ts(self, eng, ot, out, in0, s1, s2, op0, op1=None, reads=()):
        if op1 is None:
            fn = lambda e: e.tensor_scalar(out=out, in0=in0, scalar1=s1, scalar2=None, op0=op0)
        else:
            fn = lambda e: e.tensor_scalar(out=out, in0=in0, scalar1=s1, scalar2=s2, op0=op0, op1=op1)
        self.op(eng, fn, reads=reads, writes=[ot])

    def tt(self, eng, ot, out, in0, in1, op, reads=()):
        self.op(eng, lambda e: e.tensor_tensor(out=out, in0=in0, in1=in1, op=op), reads=reads, writes=[ot])

    def stt(self, eng, ot, out, in0, scalar, in1, op0, op1, reads=()):
        self.op(eng, lambda e: e.scalar_tensor_tensor(out=out, in0=in0, scalar=scalar, in1=in1, op0=op0, op1=op1),
                reads=reads, writes=[ot])

    def cp(self, eng, ot, out, in_, reads=()):
        if eng == "act":
            self.op(eng, lambda e: e.copy(out=out, in_=in_), reads=reads, writes=[ot])
        else:
            self.op(eng, lambda e: e.tensor_copy(out=out, in_=in_), reads=reads, writes=[ot])

    def memset(self, eng, ot, out, val):
        self.op(eng, lambda e: e.memset(out, val), writes=[ot])


def run_spmd(P, in_maps):
    nc = P.build()
    res = run_bass_kernel_spmd(nc, in_maps, core_ids=list(range(NCORES)))
    return res.results


def c_(a, dt=np.float32):
    return np.ascontiguousarray(a, dtype=dt)


def pk(a):
    kp, n = a.shape
    return c_(a.reshape(kp // 128, 128, n).transpose(1, 0, 2))


EPC = (NE + 1 + NCORES - 1) // NCORES
NSLOT = DEPTH * EPC


def launch_mod(c, c_ctx, mod_w, mod_b, wgu_layers, wd_layers):
    P = Prog()
    NCOL = 6 * D // NCORES
    d_cg = P.dram("cwgu", [NSLOT, D, 512], F32)
    d_cd = P.dram("cwd", [NSLOT, 256, D], F32)
    o_cg = P.dram("o_wgu", [NSLOT, 128, 8, 512], BF16, kind="ExternalOutput")
    o_cd = P.dram("o_wd", [NSLOT, 128, 2, D], BF16, kind="ExternalOutput")
    cgb = [P.sb([128, 8, 512], BF16) for _ in range(3)]
    cdb = [P.sb([128, 2, D], BF16) for _ in range(3)]
    for sl in range(NSLOT):
        g = cgb[sl % 3]; dd_ = cdb[sl % 3]
        P.load(g, g[:], d_cg.ap()[sl].rearrange("(k p) n -> p k n", p=128), q="pool")
        P.load(dd_, dd_[:], d_cd.ap()[sl].rearrange("(k p) n -> p k n", p=128), q="pool")
        P.store(o_cg.ap()[sl], g, g[:], q="sp")
        P.store(o_cd.ap()[sl], dd_, dd_[:], q="act")
    cT = P.dram("cT", [128, 8, 5], F32)
    mw = P.dram("mw", [DEPTH, 128, 8, NCOL], F32)
    mb = P.dram("mb", [DEPTH, 5, NCOL], F32)
    out = P.dram("out", [DEPTH, 5, NCOL], F32, kind="ExternalOutput")
    cs = P.sb([128, 8, 5], F32)
    sc = P.sb([128, 8, 5], F32)
    P.load(cs, cs[:], cT.ap())
    P.act(sc, sc[:], cs[:], AF.Silu, reads=[cs])
    for l in range(DEPTH):
        w = P.sb([128, 8, NCOL], F32)
        b = P.sb([5, NCOL], F32)
        o = P.sb([5, NCOL], F32)
        P.load(w, w[:], mw.ap()[l])
        P.load(b, b[:], mb.ap()[l])
        for hf in range(2):
            pt = P.ps([128, 512], F32)
            for k in range(8):
                P.mm(pt, pt[0:5, 0:384], sc[:, k, :], w[:, k, hf * 384:(hf + 1) * 384],
                     start=(k == 0), stop=(k == 7), reads=[sc, w])
            P.tt("dve", o, o[:, hf * 384:(hf + 1) * 384], pt[0:5, 0:384], b[:, hf * 384:(hf + 1) * 384],
                 ALU.add, reads=[pt, b])
        P.store(out.ap()[l], o, o[:])
    cc = np.concatenate([c, c_ctx[None]], 0)
    cTh = c_(cc.T.reshape(8, 128, 5).transpose(1, 0, 2))
    maps = []
    for i in range(NCORES):
        sl = slice(i * NCOL, (i + 1) * NCOL)
        cg = np.zeros((NSLOT, D, 512), np.float32); cd = np.zeros((NSLOT, 256, D), np.float32)
        for l in range(DEPTH):
            e0, e1 = i * EPC, min((i + 1) * EPC, NE + 1)
            cg[l * EPC:l * EPC + (e1 - e0)] = wgu_layers[l][e0:e1]
            cd[l * EPC:l * EPC + (e1 - e0)] = wd_layers[l][e0:e1]
        maps.append({
            "cT": cTh,
            "mw": c_(np.stack([pk(mod_w[l][:, sl]) for l in range(DEPTH)])),
            "mb": c_(np.stack([np.broadcast_to(mod_b[l][sl], (5, NCOL)) for l in range(DEPTH)])),
            "cwgu": cg, "cwd": cd,
        })
    res = run_spmd(P, maps)
    mod = np.concatenate([r["out"] for r in res], axis=2)
    wb = []
    for l in range(DEPTH):
        g = np.concatenate([r["o_wgu"][l * EPC:(l + 1) * EPC] for r in res], 0)[:NE + 1]
        d_ = np.concatenate([r["o_wd"][l * EPC:(l + 1) * EPC] for r in res], 0)[:NE + 1]
        wb.append((np.ascontiguousarray(g), np.ascontiguousarray(d_)))
    return mod.reshape(DEPTH, 5, 6, D), wb


def _filt_consts(n):
    f32 = np.float32
    t = np.linspace(0.0, 1.0, n, dtype=f32)
    w = (2.0 * math.pi * np.arange(n, dtype=f32) / n).astype(f32)
    fr = np.linspace(1e-4, HY_BANDS - 1, HY_BANDS, dtype=f32)
    z = np.concatenate([t[:, None], np.cos(fr[None] * w[:, None]), -np.sin(fr[None] * w[:, None])], -1).astype(f32)
    idx = np.concatenate([np.minimum(n - np.arange(n), n - 1), np.arange(n)])
    zext = c_(z[idx].T)
    text = c_(t[idx])
    return zext, text


def launch_filt(w1, b1, f1, w2, b2, f2, w3):
    P = Prog()
    CH = HYW // NCORES
    ns = [SEQ, CTX]
    d_w1 = P.dram("w1", [33, 64], F32)
    d_w2 = P.dram("w2", [64, 64], F32)
    d_vec = P.dram("vec", [64, 4], F32)
    d_w3 = P.dram("w3", [64, 2, CH], F32)
    d_nd = P.dram("nd", [CH, 1], F32)
    d_z = [P.dram("z%d" % i, [33, 2 * n], F32) for i, n in enumerate(ns)]
    d_t = [P.dram("t%d" % i, [CH, 2 * n], F32) for i, n in enumerate(ns)]
    d_o = [P.dram("o%d" % i, [CH, 2 * n], BF16, kind="ExternalOutput") for i, n in enumerate(ns)]
    w1s = P.sb([33, 64], F32); w2s = P.sb([64, 64], F32); vec = P.sb([64, 4], F32)
    w3s = P.sb([64, 2, CH], F32); nd = P.sb([CH, 1], F32)
    P.load(w1s, w1s[:], d_w1.ap()); P.load(w2s, w2s[:], d_w2.ap()); P.load(vec, vec[:], d_vec.ap())
    P.load(w3s, w3s[:], d_w3.ap()); P.load(nd, nd[:], d_nd.ap())
    off = P.sb([64, 4], F32)
    for j in range(2):
        P.ts("dve", off, off[:, j:j + 1], vec[:, 2 * j:2 * j + 1], vec[:, 2 * j + 1:2 * j + 2], 1.0 / (2 * PI),
             ALU.mult, ALU.mult, reads=[vec])
        P.ts("dve", off, off[:, 2 + j:3 + j], vec[:, 2 * j + 1:2 * j + 2], 1.0 / (2 * PI), None,
             ALU.mult, reads=[vec])
    pts = [P.ps([128, 512], F32) for _ in range(6)]
    for i, n in enumerate(ns):
        P.push()
        L = P.sb([CH, 2 * n], F32)
        zss = [P.sb([33, 512], F32) for _ in range(2)]
        txs = [P.sb([CH, 512], F32) for _ in range(2)]
        fb = [[P.sb([64, 512], F32) for _ in range(5)] for _ in range(2)]
        ki = P.sb([64, 512], I32); kf = P.sb([64, 512], F32)
        TW = min(512, n)
        for ti in range(2 * n // TW):
            sl = slice(ti * TW, (ti + 1) * TW)
            p1, p2, p3 = pts[(ti % 2) * 3:(ti % 2) * 3 + 3]
            a1, h1, a2, h2, dec = fb[ti % 2]
            zs = zss[ti % 2]; tx = txs[ti % 2]
            P.load(zs, zs[:, 0:TW], d_z[i].ap()[:, sl])
            P.load(tx, tx[:, 0:TW], d_t[i].ap()[:, sl])
            P.mm(p1, p1[0:64, 0:TW], w1s[:], zs[:, 0:TW], reads=[w1s, zs])
            P.ts("dve", a1, a1[:, 0:TW], p1[0:64, 0:TW], off[:, 2:3], off[:, 0:1], ALU.mult, ALU.add, reads=[p1, off])
            P.cp("dve", ki, ki[:, 0:TW], a1[:, 0:TW], reads=[a1])
            P.cp("dve", kf, kf[:, 0:TW], ki[:, 0:TW], reads=[ki])
            P.tt("dve", a1, a1[:, 0:TW], a1[:, 0:TW], kf[:, 0:TW], ALU.subtract, reads=[a1, kf])
            P.act(h1, h1[:, 0:TW], a1[:, 0:TW], AF.Sin, scale=2 * PI, reads=[a1])
            P.mm(p2, p2[0:64, 0:TW], w2s[:], h1[:, 0:TW], reads=[w2s, h1])
            P.ts("dve", a2, a2[:, 0:TW], p2[0:64, 0:TW], off[:, 3:4], off[:, 1:2], ALU.mult, ALU.add, reads=[p2, off])
            P.cp("dve", ki, ki[:, 0:TW], a2[:, 0:TW], reads=[a2])
            P.cp("dve", kf, kf[:, 0:TW], ki[:, 0:TW], reads=[ki])
            P.tt("dve", a2, a2[:, 0:TW], a2[:, 0:TW], kf[:, 0:TW], ALU.subtract, reads=[a2, kf])
            P.act(h2, h2[:, 0:TW], a2[:, 0:TW], AF.Sin, scale=2 * PI, reads=[a2])
            d = 1 if (ti * TW) < n else 0
            P.mm(p3, p3[0:CH, 0:TW], w3s[:, d, :], h2[:, 0:TW], reads=[w3s, h2])
            P.act(dec, dec[:, 0:TW], tx[:, 0:TW], AF.Exp, scale=nd[:, 0:1], reads=[tx, nd])
            P.tt("dve", L, L[:, sl], p3[0:CH, 0:TW], dec[:, 0:TW], ALU.mult, reads=[p3, dec])
        P.memset("dve", L, L[:, 0:1], 0.0)
        ab = P.sb([CH, 2 * n], F32)
        sm = P.sb([CH, 2], F32)
        P.act(ab, ab[:], L[:], AF.Abs, reads=[L])
        P.op("dve", lambda e, sm=sm, ab=ab: e.reduce_sum(out=sm[:, 0:1], in_=ab[:], axis=AX.X), reads=[ab], writes=[sm])
        P.op("dve", lambda e, sm=sm: e.reciprocal(out=sm[:, 1:2], in_=sm[:, 0:1]), reads=[sm], writes=[sm])
        Lb = P.sb([CH, 2 * n], BF16)
        P.ts("dve", Lb, Lb[:], L[:], sm[:, 1:2], None, ALU.mult, reads=[L, sm])
        P.store(d_o[i].ap(), Lb, Lb[:])
        P.pop()
    deltas = np.abs(np.linspace(HY_DECAY_MIN, HY_DECAY_MAX, HYW, dtype=np.float32))
    consts = [_filt_consts(n) for n in ns]
    maps = []
    for i in range(NCORES):
        ch = slice(i * CH, (i + 1) * CH)
        m = {"w1": c_(w1), "w2": c_(w2), "vec": c_(np.stack([b1, f1, b2, f2], 1)),
             "w3": c_(np.stack([w3[:, :HYW][:, ch], w3[:, HYW:][:, ch]], 1)),
             "nd": c_(-deltas[ch][:, None])}
        for j, n in enumerate(ns):
            m["z%d" % j] = consts[j][0]
            m["t%d" % j] = c_(np.broadcast_to(consts[j][1], (CH, 2 * n)))
        maps.append(m)
    res = run_spmd(P, maps)
    Ls = [np.concatenate([r["o%d" % j] for r in res], 0) for j in range(2)]
    return Ls


NT0 = 4228
INT_SEGS = [(1, 129, 0), (131, 4227, 128)]
NTI = 4224


def _ntiles(n, w=512):
    return [(s, min(w, n - s)) for s in range(0, n, w)]


def _core_tokens(b, s):
    ci = np.arange(s * 128 - 1, s * 128 + 129)
    li = np.arange(s * 4096 - 1, s * 4096 + 4097)
    return ci, li


def _gather_rows(arr, idx):
    n = arr.shape[0]
    ok = (idx >= 0) & (idx < n)
    out = arr[np.clip(idx, 0, n - 1)].copy()
    out[~ok] = 0
    return out, ok


def _rope_tables():
    f32 = np.float32
    rows = SEQ // GRID_W
    row = np.repeat(np.arange(rows), GRID_W).astype(f32)
    col = np.tile(np.arange(GRID_W), rows).astype(f32)
    half = 16
    inv = (10000.0 ** (-np.arange(0, half, 2, dtype=f32) / half)).astype(f32)
    ang = np.concatenate([row[:, None] * inv, col[:, None] * inv], -1)
    cos = np.cos(ang).astype(f32); sin = np.sin(ang).astype(f32)
    cosT = np.ones((96, SEQ), f32); sinT = np.zeros((96, SEQ), f32)
    for j in range(32):
        cosT[64 + j] = cos[:, j // 2]
        sinT[64 + j] = sin[:, j // 2] * (-1.0 if j % 2 == 0 else 1.0)
    return cosT, sinT


def launch_pre0(x, ctx, mod0, w_in, conv_w, conv_b, q_norm, w_qb, kv_norm, w_kvb):
    P = Prog()
    NT = NT0
    d_xT = P.dram("xT", [128, 8, NT], F32)
    d_mod = P.dram("mod", [128, 8, 4], F32)
    d_win = P.dram("win", [12, 128, 8, 128], F32)
    d_wq = P.dram("wq", [128, 8, 256], F32)
    d_wkv = P.dram("wkv", [128, 8, 128], F32)
    d_wkp = P.dram("wkp", [2, 128, 8, 96], F32)
    d_cw = P.dram("cw", [128, 12, 4], F32)
    d_hm = P.dram("hm", [128, 4], F32)
    d_cos = P.dram("cos", [96, NT], F32)
    d_sin = P.dram("sin", [96, NT], F32)
    d_gq = P.dram("gq", [128, 3], F32)
    d_wqb = P.dram("wqb", [2, 128, 2, 8, 96], F32)
    d_wkn = P.dram("wkn", [128, 8, 64], F32)
    d_wv = P.dram("wv", [128, 512], F32)
    o_x0 = P.dram("o_x0", [512, NTI], BF16, kind="ExternalOutput")
    o_z = P.dram("o_z", [512, NTI], BF16, kind="ExternalOutput")
    o_q = P.dram("o_q", [8, 96, NTI], BF16, kind="ExternalOutput")
    o_k = P.dram("o_k", [8, 96, NTI], BF16, kind="ExternalOutput")
    o_v = P.dram("o_v", [NTI, 512], BF16, kind="ExternalOutput")

    tiles = _ntiles(NT)
    pbank = [P.ps([128, 512], F32) for _ in range(8)]
    pb_i = [0]

    def bank():
        pb_i[0] = (pb_i[0] + 1) % 8
        return pbank[pb_i[0]]

    hT = P.sb([128, 8, NT], BF16, "hT")
    epsq = P.sb([128, 1], F32); P.memset("dve", epsq, epsq[:], RMS_EPS)
    mods = P.sb([128, 8, 4], F32)
    P.load(mods, mods[:], d_mod.ap())
    m1 = P.sb([128, 8, 2], F32)
    for j in range(2):
        P.ts("dve", m1, m1[:, :, j:j + 1], mods[:, :, 2 * j:2 * j + 1], 1.0, None, ALU.add, reads=[mods])
    P.push()
    xin = [P.sb([128, 8, 512], F32) for _ in range(2)]
    for ti, (s0, w) in enumerate(tiles):
        xt = xin[ti % 2]
        P.load(xt, xt[:, :, 0:w], d_xT.ap()[:, :, s0:s0 + w])
        for k in range(8):
            for (a, b_, j) in ((s0, min(s0 + w, 130), 0), (max(s0, 130), s0 + w, 1)):
                if b_ <= a:
                    continue
                P.ts("dve" if k % 2 == 0 else "pool", hT, hT[:, k, a:b_], xt[:, k, a - s0:b_ - s0],
                     m1[:, k, j:j + 1], mods[:, k, 2 * j + 1:2 * j + 2], ALU.mult, ALU.add, reads=[xt, m1, mods])
    P.pop()

    def project(pt_rows, wt, wsel, dst_fn):
        for (s0, w) in tiles:
            pt = bank()
            for k in range(8):
                P.mm(pt, pt[0:pt_rows, 0:w], wsel(k), hT[:, k, s0:s0 + w], start=(k == 0), stop=(k == 7),
                     reads=[wt, hT])
            dst_fn(pt, s0, w)

    P.push()
    cw = P.sb([128, 12, 4], F32); hm = P.sb([128, 4], F32)
    P.load(cw, cw[:], d_cw.ap()); P.load(hm, hm[:], d_hm.ap())
    wbuf = [P.sb([128, 8, 128], BF16) for _ in range(2)]
    urow = [P.sb([128, NT], F32) for _ in range(2)]
    cv = P.sb([128, NT], F32)
    x1c = P.sb([128, 4, NT], BF16)
    ob = [P.sb([128, NT], BF16) for _ in range(2)]
    for oc in range(12):
        wt = wbuf[oc % 2]; ur = urow[oc % 2]
        P.load(wt, wt[:], d_win.ap()[oc], q="pool")
        cnt = [0]

        def evac(pt, s0, w, ur=ur, cnt=cnt):
            cnt[0] += 1
            P.cp("act" if cnt[0] % 2 else "dve", ur, ur[:, s0:s0 + w], pt[:, 0:w], reads=[pt])
        project(128, wt, lambda k, wt=wt: wt[:, k, :], evac)
        for j, c in enumerate((0, 129, 130, 4227)):
            P.tt("dve", ur, ur[:, c:c + 1], ur[:, c:c + 1], hm[:, j:j + 1], ALU.mult, reads=[ur, hm])
        P.act(cv, cv[:], ur[:], AF.Identity, bias=cw[:, oc, 3:4], scale=cw[:, oc, 1:2], reads=[ur, cw])
        P.stt("dve", cv, cv[:, 1:NT], ur[:, 0:NT - 1], cw[:, oc, 0:1], cv[:, 1:NT], ALU.mult, ALU.add, reads=[ur, cw, cv])
        P.stt("dve", cv, cv[:, 0:NT - 1], ur[:, 1:NT], cw[:, oc, 2:3], cv[:, 0:NT - 1], ALU.mult, ALU.add, reads=[ur, cw, cv])
        if 4 <= oc < 8:
            P.cp("pool", x1c, x1c[:, oc - 4, :], cv[:], reads=[cv])
        else:
            o = ob[oc % 2]
            if oc < 4:
                P.cp("pool", o, o[:], cv[:], reads=[cv])
                dst = o_x0
            else:
                P.tt("pool", o, o[:], cv[:], x1c[:, oc - 8, :], ALU.mult, reads=[cv, x1c])
                dst = o_z
            r0 = (oc % 4) * 128
            for (a, b_, oo) in INT_SEGS:
                P.store(dst.ap()[r0:r0 + 128, oo:oo + (b_ - a)], o, o[:, a:b_])
    P.pop()

    P.push()
    wq = P.sb([128, 8, 256], BF16); wkv = P.sb([128, 8, 128], BF16); wkp = P.sb([128, 2, 8, 96], BF16)
    P.load(wq, wq[:], d_wq.ap(), q="pool"); P.load(wkv, wkv[:], d_wkv.ap(), q="pool")
    for j in range(2):
        P.load(wkp, wkp[:, j], d_wkp.ap()[j], q="pool")
    cosT = P.sb([96, NT], BF16); sinT = P.sb([96, NT], BF16)
    P.load(cosT, cosT[:], d_cos.ap(), q="pool"); P.load(sinT, sinT[:], d_sin.ap(), q="pool")
    gq = P.sb([128, 3], F32); P.load(gq, gq[:], d_gq.ap())
    ones = P.sb([128, 128], F32); P.memset("dve", ones, ones[:], 1.0)
    qn = P.sb([128, 3, NT], BF16)
    kpe = P.sb([96, NT], BF16)
    tA = P.sb([96, 512], F32); tB = P.sb([96, 512], F32)
    P.push()
    uq = P.sb([128, 3, NT], F32)
    for c in range(3):
        def evq(pt, s0, w, c=c):
            P.cp("act", uq, uq[:, c, s0:s0 + w], pt[:, 0:w], reads=[pt])
        if c < 2:
            project(128, wq, lambda k, c=c: wq[:, k, c * 128:(c + 1) * 128], evq)
        else:
            project(128, wkv, lambda k: wkv[:, k, :], evq)
    for (s0, w) in tiles:
        pA = bank(); pB = bank()
        for k in range(8):
            P.mm(pA, pA[0:96, 0:w], wkp[:, 0, k, :], hT[:, k, s0:s0 + w], start=(k == 0), stop=(k == 7), reads=[wkp, hT])
        for k in range(8):
            P.mm(pB, pB[0:96, 0:w], wkp[:, 1, k, :], hT[:, k, s0:s0 + w], start=(k == 0), stop=(k == 7), reads=[wkp, hT])
        P.tt("dve", tA, tA[64:96, 0:w], pA[64:96, 0:w], cosT[64:96, s0:s0 + w], ALU.mult, reads=[pA, cosT])
        P.tt("dve", tB, tB[64:96, 0:w], pB[64:96, 0:w], sinT[64:96, s0:s0 + w], ALU.mult, reads=[pB, sinT])
        P.tt("pool", kpe, kpe[64:96, s0:s0 + w], tA[64:96, 0:w], tB[64:96, 0:w], ALU.add, reads=[tA, tB])
    sq = [P.sb([128, 2, 512], F32) for _ in range(2)]
    rs = [P.sb([128, 512], F32) for _ in range(2)]
    for ti, (s0, w) in enumerate(tiles):
        for grp, chunks, R in ((0, (0, 1), 256.0), (1, (2,), 128.0)):
            sqt = sq[(2 * ti + grp) % 2]; rst = rs[(2 * ti + grp) % 2]
            pt = bank()
            for i, c in enumerate(chunks):
                P.act(sqt, sqt[:, i, 0:w], uq[:, c, s0:s0 + w], AF.Square, reads=[uq])
            for i, c in enumerate(chunks):
                P.mm(pt, pt[:, 0:w], ones[:], sqt[:, i, 0:w], start=(i == 0), stop=(i == len(chunks) - 1), reads=[ones, sqt])
            P.act(rst, rst[:, 0:w], pt[:, 0:w], AF.Sqrt, bias=epsq[:, 0:1], scale=1.0 / R, reads=[pt])
            P.op("dve", lambda e, rst=rst, w=w: e.reciprocal(out=rst[:, 0:w], in_=rst[:, 0:w]), reads=[rst], writes=[rst])
            for c in chunks:
                P.stt("dve", qn, qn[:, c, s0:s0 + w], uq[:, c, s0:s0 + w], gq[:, c:c + 1], rst[:, 0:w], ALU.mult, ALU.mult,
                      reads=[uq, gq, rst])
    P.pop()
    wqb = P.sb([128, 2, 2, 8, 96], BF16)
    for j in range(2):
        P.load(wqb, wqb[:, j], d_wqb.ap()[j], q="pool")
    wkn = P.sb([128, 8, 64], BF16); P.load(wkn, wkn[:], d_wkn.ap(), q="pool")
    wv = P.sb([128, 512], BF16); P.load(wv, wv[:], d_wv.ap(), q="pool")
    qh = [P.sb([96, NT], BF16) for _ in range(2)]
    kh = [P.sb([96, NT], BF16) for _ in range(2)]
    for h in range(8):
        qt = qh[h % 2]; kt = kh[h % 2]
        for (s0, w) in tiles:
            pA = bank(); pB = bank(); pK = bank()
            for k in range(2):
                P.mm(pA, pA[0:96, 0:w], wqb[:, 0, k, h, :], qn[:, k, s0:s0 + w], start=(k == 0), stop=(k == 1), reads=[wqb, qn])
            for k in range(2):
                P.mm(pB, pB[0:96, 0:w], wqb[:, 1, k, h, :], qn[:, k, s0:s0 + w], start=(k == 0), stop=(k == 1), reads=[wqb, qn])
            P.mm(pK, pK[0:64, 0:w], wkn[:, h, :], qn[:, 2, s0:s0 + w], reads=[wkn, qn])
            P.tt("dve", tA, tA[:, 0:w], pA[0:96, 0:w], cosT[:, s0:s0 + w], ALU.mult, reads=[pA, cosT])
            P.tt("dve", tB, tB[:, 0:w], pB[0:96, 0:w], sinT[:, s0:s0 + w], ALU.mult, reads=[pB, sinT])
            P.tt("pool", qt, qt[:, s0:s0 + w], tA[:, 0:w], tB[:, 0:w], ALU.add, reads=[tA, tB])
            P.cp("act", kt, kt[0:64, s0:s0 + w], pK[0:64, 0:w], reads=[pK])
        P.cp("pool", kt, kt[64:96, :], kpe[64:96, :], reads=[kpe])
        for (a, b_, oo) in INT_SEGS:
            P.store(o_q.ap()[h, :, oo:oo + (b_ - a)], qt, qt[:, a:b_])
            P.store(o_k.ap()[h, :, oo:oo + (b_ - a)], kt, kt[:, a:b_])
    vb = [P.sb([128, 512], BF16) for _ in range(2)]
    for ti in range(NTI // 128):
        col = 1 + ti * 128 if ti == 0 else 131 + (ti - 1) * 128
        pt = bank(); v = vb[ti % 2]
        P.mm(pt, pt[:, :], qn[:, 2, col:col + 128], wv[:], reads=[qn, wv])
        P.cp("act", v, v[:], pt[:, :], reads=[pt])
        P.store(o_v.ap()[ti * 128:(ti + 1) * 128, :], v, v[:])
    P.pop()

    cosF, sinF = _rope_tables()
    pairswap = np.arange(32) ^ 1
    win_h = c_(np.stack([pk(w_in[:, oc * 128:(oc + 1) * 128]) for oc in range(12)]))
    wq_h = pk(w_in[:, 1536:1792]); wkv_h = pk(w_in[:, 1792:1920])
    kpA = np.zeros((D, 96), np.float32); kpB = np.zeros((D, 96), np.float32)
    kpA[:, 64:] = w_in[:, 1920:1952]; kpB[:, 64:] = w_in[:, 1920:1952][:, pairswap]
    wkp_h = c_(np.stack([pk(kpA), pk(kpB)]))
    cw_h = c_(np.concatenate([conv_w.T.reshape(12, 128, 3), conv_b.reshape(12, 128, 1)], -1).transpose(1, 0, 2))
    gq_h = c_(np.stack([q_norm[:128], q_norm[128:], kv_norm], 1))
    wqbA = w_qb.reshape(256, 8, 96)
    wqbB = wqbA.copy(); wqbB[:, :, 64:] = wqbA[:, :, 64:][:, :, pairswap]
    wqb_h = c_(np.stack([wqbA.reshape(2, 128, 8, 96).transpose(1, 0, 2, 3), wqbB.reshape(2, 128, 8, 96).transpose(1, 0, 2, 3)]))
    wkvr = w_kvb.reshape(128, 8, 128)
    wkn_h = c_(wkvr[:, :, :64]); wv_h = c_(wkvr[:, :, 64:].reshape(128, 512))
    maps = []
    for core in range(NCORES):
        b, s = core // 2, core % 2
        ci, li = _core_tokens(b, s)
        xc, okc = _gather_rows(ctx[b], ci); xl, okl = _gather_rows(x[b], li)
        xw = np.concatenate([xc, xl], 0)
        cosw = np.ones((96, NT), np.float32); sinw = np.zeros((96, NT), np.float32)
        lic = np.clip(li, 0, SEQ - 1)
        cosw[:, 130:] = cosF[:, lic]; sinw[:, 130:] = sinF[:, lic]
        hmv = np.array([okc[0], okc[-1], okl[0], okl[-1]], np.float32)
        modv = np.stack([mod0[4, 1], mod0[4, 0], mod0[b, 1], mod0[b, 0]], 1)
        maps.append({
            "xT": c_(xw.T.reshape(8, 128, NT).transpose(1, 0, 2)),
            "mod": c_(modv.reshape(8, 128, 4).transpose(1, 0, 2)),
            "win": win_h, "wq": wq_h, "wkv": wkv_h, "wkp": wkp_h, "cw": cw_h,
            "hm": c_(np.broadcast_to(hmv, (128, 4))), "cos": cosw, "sin": sinw, "gq": gq_h,
            "wqb": wqb_h, "wkn": wkn_h, "wv": wv_h,
        })
    res = run_spmd(P, maps)
    return res


_EPS_TILES = {}


def RMS_EPS_AP(P):
    key = id(P)
    if key not in _EPS_TILES:
        t = P.stk[0].enter_context(P.nc.sbuf_tensor("epsc", [128, 1], F32))
        tl = Tile(t, "epsc")
        P.memset("dve", tl, tl[:], RMS_EPS)
        _EPS_TILES[key] = tl
    return _EPS_TILES[key][:, 0:1]


NK = CTX + SEQ


def launch_attn(QT, KT, V):
    P = Prog()
    HG = 4
    NKT = NK // 128
    d_q = P.dram("q", [HG, 96, NK], BF16)
    d_k = P.dram("k", [HG, 96, NK], BF16)
    d_v = P.dram("v", [HG, 128, NKT, 65], BF16)
    d_sel = P.dram("sel", [65, 64], F32)
    o_a = P.dram("o_a", [HG, 64, NK], BF16, kind="ExternalOutput")
    ones = P.sb([128, 128], F32); P.memset("dve", ones, ones[:], 1.0)
    sel = P.sb([65, 64], F32); P.load(sel, sel[:], d_sel.ap())
    qs = [P.sb([96, NK], BF16) for _ in range(2)]
    ks = [P.sb([96, NK], BF16) for _ in range(2)]
    vs = [P.sb([128, NKT, 65], BF16) for _ in range(2)]
    ats = [P.sb([64, NK], BF16) for _ in range(2)]
    sqt = [P.sb([96, 512], F32) for _ in range(2)]
    mx = P.sb([128, 4], F32)
    negc = [P.sb([128, 1], F32) for _ in range(2)]
    pT = [P.sb([128, 512], BF16) for _ in range(3)]
    oT = [P.sb([65, 512], F32) for _ in range(2)]
    rec = [P.sb([64, 512], F32) for _ in range(2)]
    psS = [P.ps([128, 512], F32) for _ in range(4)]
    psO = [P.ps([128, 512], F32) for _ in range(2)]
    psD = [P.ps([128, 512], F32) for _ in range(2)]
    cS = [0]; cO = [0]
    qtiles = _ntiles(NK)
    for h in range(HG):
        q = qs[h % 2]; k = ks[h % 2]; v = vs[h % 2]; at = ats[h % 2]; nc_ = negc[h % 2]
        P.load(q, q[:], d_q.ap()[h]); P.load(k, k[:], d_k.ap()[h], q="act"); P.load(v, v[:], d_v.ap()[h])
        for which, src in ((0, q), (1, k)):
            for ti, (s0, w) in enumerate(qtiles):
                st = sqt[ti % 2]; pt = psD[ti % 2]
                P.act(st, st[:, 0:w], src[:, s0:s0 + w], AF.Square, reads=[src])
                P.mm(pt, pt[:, 0:w], ones[0:96, :], st[:, 0:w], reads=[ones, st])
                if ti == 0:
                    P.op("dve", lambda e, pt=pt, w=w, which=which: e.reduce_max(out=mx[:, which:which + 1], in_=pt[:, 0:w], axis=AX.X),
                         reads=[pt], writes=[mx])
                else:
                    P.op("dve", lambda e, pt=pt, w=w: e.reduce_max(out=mx[:, 2:3], in_=pt[:, 0:w], axis=AX.X),
                         reads=[pt], writes=[mx])
                    P.tt("dve", mx, mx[:, which:which + 1], mx[:, which:which + 1], mx[:, 2:3], ALU.max, reads=[mx])
        P.tt("dve", mx, mx[:, 3:4], mx[:, 0:1], mx[:, 1:2], ALU.mult, reads=[mx])
        P.act(mx, mx[:, 3:4], mx[:, 3:4], AF.Sqrt, scale=MLA_SCALE * MLA_SCALE, reads=[mx])
        P.ts("dve", nc_, nc_[:], mx[:, 3:4], -1.0, None, ALU.mult, reads=[mx])
        chunks = [(0, 256, 2)] + [(256 + 512 * i, 512, NKT) for i in range(SEQ // 512)]
        for (q0, qw, nkt) in chunks:
            po = psO[cO[0] % 2]; o = oT[cO[0] % 2]; rc = rec[cO[0] % 2]; pd = psD[cO[0] % 2]; cO[0] += 1
            for kt in range(nkt):
                pS = psS[cS[0] % 4]; p_ = pT[cS[0] % 3]; cS[0] += 1
                P.mm(pS, pS[:, 0:qw], k[:, kt * 128:(kt + 1) * 128], q[:, q0:q0 + qw], reads=[k, q])
                P.act(p_, p_[:, 0:qw], pS[:, 0:qw], AF.Exp, bias=nc_[:, 0:1], scale=MLA_SCALE, reads=[pS, nc_])
                P.mm(po, po[0:65, 0:qw], v[:, kt, :], p_[:, 0:qw], start=(kt == 0), stop=(kt == nkt - 1), reads=[v, p_])
            P.cp("dve", o, o[:, 0:qw], po[0:65, 0:qw], reads=[po])
            P.mm(pd, pd[0:64, 0:qw], sel[:], o[:, 0:qw], reads=[sel, o])
            P.op("dve", lambda e, rc=rc, pd=pd, qw=qw: e.reciprocal(out=rc[:, 0:qw], in_=pd[0:64, 0:qw]), reads=[pd], writes=[rc])
            P.tt("pool", at, at[:, q0:q0 + qw], o[0:64, 0:qw], rc[:, 0:qw], ALU.mult, reads=[o, rc])
        P.store(o_a.ap()[h], at, at[:])
    selh = np.zeros((65, 64), np.float32); selh[64] = 1.0
    maps = []
    for core in range(NCORES):
        b, g = core // 2, core % 2
        hs = slice(g * HG, (g + 1) * HG)
        vv = np.asarray(V[b][:, hs, :])
        va = np.ones((HG, 128, NKT, 65), NPBF)
        va[:, :, :, :64] = vv.reshape(NKT, 128, HG, 64).transpose(2, 1, 0, 3)
        maps.append({"q": np.ascontiguousarray(QT[b, hs]), "k": np.ascontiguousarray(KT[b, hs]), "v": va, "sel": selh})
    res = run_spmd(P, maps)
    att = np.zeros((BATCH, 8, 64, NK), NPBF)
    for core in range(NCORES):
        b, g = core // 2, core % 2
        att[b, g * HG:(g + 1) * HG] = res[core]["o_a"]
    return att


def launch_hyconv(Ls, zT_lat, zT_ctx, skip):
    P = Prog()
    CH = 64
    cfgs = [(SEQ, SEQ // 128), (CTX, CTX // 128)]
    d_L = [P.dram("L%d" % i, [CH, 2 * n], BF16) for i, (n, J) in enumerate(cfgs)]
    d_z = [P.dram("z%d" % i, [128, CH, 4, J], BF16) for i, (n, J) in enumerate(cfgs)]
    d_sk = P.dram("sk", [128, CH], F32)
    o_y = [P.dram("y%d" % i, [128, CH, 4, J], F32, kind="ExternalOutput") for i, (n, J) in enumerate(cfgs)]
    sk = P.sb([128, CH], F32); P.load(sk, sk[:], d_sk.ap())
    pbank = [P.ps([128, 8, 64], F32) for _ in range(4)]
    pc = [0]
    for i, (n, J) in enumerate(cfgs):
        P.push()
        W = 2 * n - 127
        zt = P.sb([128, CH, 4, J], BF16); P.load(zt, zt[:], d_z[i].ap())
        y = P.sb([128, CH, 4, J], F32)
        kss = [P.sb([128, W], BF16) for _ in range(2)]
        ms = [0] + [s * m for m in range(1, J) for s in (1, -1)]
        for c in range(CH):
            ks = kss[c % 2]
            src = bass.AP(tensor=d_L[i], offset=c * 2 * n, ap=[[1, 128], [1, W]])
            P.load(ks, ks[:], src, q=("sp" if c % 2 == 0 else "act"))
            pt = pbank[pc[0] % 4]; pc[0] += 1
            for idx, m in enumerate(ms):
                i0, i1 = max(0, m), min(J - 1, J - 1 + m)
                u0 = n + 128 * m - 127
                P.mm(pt, pt[:, 0:4, i0:i1 + 1], ks[:, u0:u0 + 128], zt[:, c, :, i0 - m:i1 + 1 - m],
                     start=(idx == 0), stop=(idx == len(ms) - 1), reads=[ks, zt])
            P.cp("dve" if c % 2 else "act", y, y[:, c], pt[:, 0:4, 0:J], reads=[pt])
        P.store(o_y[i].ap(), y, y[:])
        P.pop()
    maps = []
    zsrc = [zT_lat, zT_ctx]
    for core in range(NCORES):
        ch = slice(core * CH, (core + 1) * CH)
        m = {"sk": c_(np.broadcast_to(skip[ch], (128, CH)))}
        for i, (n, J) in enumerate(cfgs):
            m["L%d" % i] = np.ascontiguousarray(Ls[i][ch])
            z = np.asarray(zsrc[i][:, ch, :]).reshape(4, CH, J, 128)[:, :, :, ::-1]
            m["z%d" % i] = np.ascontiguousarray(z.transpose(3, 1, 0, 2))
        maps.append(m)
    res = run_spmd(P, maps)
    outs = []
    for i, (n, J) in enumerate(cfgs):
        yy = np.zeros((BATCH, HYW, n), np.float32)
        for core in range(NCORES):
            r = res[core]["y%d" % i]
            yy[:, core * CH:(core + 1) * CH, :] = r.transpose(2, 1, 3, 0).reshape(4, CH, n)
        outs.append(yy)
    return outs


def assemble_seq(res, key, feat_major=True):
    outs = []
    for b in range(BATCH):
        r0, r1 = res[2 * b][key], res[2 * b + 1][key]
        if feat_major:
            outs.append(np.concatenate([r0[..., :128], r1[..., :128], r0[..., 128:], r1[..., 128:]], -1))
        else:
            outs.append(np.concatenate([r0[:128], r1[:128], r0[128:], r1[128:]], 0))
    return np.stack(outs)


BIGNEG = 1.0e4


def launch_post(mode, ntile, feats, xres, vecs, lnvh, w_out, router_w, router_bias, w_gu_all, w_down_all, extra, nex=NE + 1):
    import os
    STG = int(os.environ.get('POST_STAGE', '9'))
    P = Prog()
    NTK = ntile * 128
    NEX = NE + 1
    NEXR = nex
    KC = 8 if mode == 0 else 16
    d_x = P.dram("xres", [NTK, D], F32)
    NS = 2 if mode == 0 else 1
    d_vec = P.dram("vecs", [128, 4, NS, D], F32)
    d_lnv = P.dram("lnv", [128, 4, D], F32)
    d_wo = P.dram("wo", [KC * 128, D], F32)
    d_rw = P.dram("rw", [D, NE], F32)
    d_rb = P.dram("rb", [128, NE], F32)
    d_wgu = P.dram("wgu", [NEXR, 128, 8, 512], BF16)
    d_wd = P.dram("wd", [NEXR, 128, 2, D], BF16)
    d_idf = P.dram("idf", [128, 128], F32)
    if mode == 0:
        d_y = P.dram("yT", [512, NTK], F32); d_z = P.dram("zT", [512, NTK], BF16)
        d_x0 = P.dram("x0T", [512, NTK], BF16); d_at = P.dram("atT", [512, NTK], BF16)
        d_sk = P.dram("sk", [128, 4], F32)
    else:
        d_yf = P.dram("yfT", [2048, NTK], BF16); d_yb = P.dram("ybT", [2048, NTK], BF16)
        d_xs = P.dram("xsT", [2048, NTK], BF16); d_zz = P.dram("zzT", [2048, NTK], BF16)
        d_dg = P.dram("dg", [128, 16, 2], F32)
    o_x1 = P.dram("o_x1", [NTK, D], F32, kind="ExternalOutput")
    o_out = P.dram("o_out", [NTK, D], F32, kind="ExternalOutput")
    x1_tiles = [Tile(None, "x1d%d" % i) for i in range(ntile)]

    vec = P.sb([128, 4, NS, D], F32); lnv = P.sb([128, 4, D], F32)
    P.load(vec, vec[:], d_vec.ap()); P.load(lnv, lnv[:], d_lnv.ap())
    P.ts("dve", vec, vec[:, 1], vec[:, 1], 1.0, None, ALU.add, reads=[vec])
    idf = P.sb([128, 128], F32); P.load(idf, idf[:], d_idf.ap())
    idb = P.sb([128, 128], BF16); P.cp("dve", idb, idb[:], idf[:], reads=[idf])
    epsl = P.sb([128, 1], F32); P.memset("dve", epsl, epsl[:], LN_EPS)
    epsr = P.sb([128, 1], F32); P.memset("dve", epsr, epsr[:], RMS_EPS)
    ones = P.sb([128, 128], F32); P.memset("dve", ones, ones[:], 1.0)
    TP = 9 if mode == 0 else 8
    acc = P.sb([128, TP, D], F32)
    ffT = P.sb([128, 8, TP * 128], BF16)
    gates = P.sb([128, TP, NEX], F32)
    P.memset("dve", gates, gates[:, :, NE:NEX], 1.0)
    st = P.sb([128, 8], F32)
    pA = [P.ps([128, 512], F32) for _ in range(2)]
    pT = [P.ps([128, 2, 128], F32) for _ in range(2)]
    pD = [P.ps([128, 1024], F32) for _ in range(2)]

    def layer_norm(t, tt_, gi, bi, out_t, out_ap, sq):
        P.op("dve", lambda e: e.reduce_sum(out=st[:, 0:1], in_=tt_, axis=AX.X), reads=[t], writes=[st])
        P.act(sq, sq[:], tt_, AF.Square, reads=[t])
        P.op("dve", lambda e: e.reduce_sum(out=st[:, 1:2], in_=sq[:], axis=AX.X), reads=[sq], writes=[st])
        P.ts("dve", st, st[:, 2:3], st[:, 0:1], 1.0 / D, None, ALU.mult, reads=[st])
        P.tt("dve", st, st[:, 3:4], st[:, 2:3], st[:, 2:3], ALU.mult, reads=[st])
        P.stt("dve", st, st[:, 4:5], st[:, 1:2], 1.0 / D, st[:, 3:4], ALU.mult, ALU.subtract, reads=[st])
        P.act(st, st[:, 5:6], st[:, 4:5], AF.Sqrt, bias=epsl[:, 0:1], scale=1.0, reads=[st, epsl])
        P.op("dve", lambda e: e.reciprocal(out=st[:, 6:7], in_=st[:, 5:6]), reads=[st], writes=[st])
        P.ts("dve", t, tt_, tt_, st[:, 2:3], st[:, 6:7], ALU.subtract, ALU.mult, reads=[t, st])
        P.tt("dve", t, tt_, tt_, lnv[:, gi, :], ALU.mult, reads=[t, lnv])
        P.tt("dve", out_t, out_ap, tt_, lnv[:, bi, :], ALU.add, reads=[t, lnv])

    passes = [list(range(s, min(s + TP, ntile))) for s in range(0, ntile, TP)]
    for tiles in passes:
        P.push()
        wo = P.sb([128, KC, D], BF16)
        P.load(wo, wo[:], d_wo.ap().rearrange("(k p) n -> p k n", p=128), q="pool")
        rw = P.sb([128, 8, NE], F32); P.load(rw, rw[:], d_rw.ap().rearrange("(k p) n -> p k n", p=128))
        rb = P.sb([128, NE], F32); P.load(rb, rb[:], d_rb.ap())
        mT = [P.sb([128, KC, 128], BF16) for _ in range(1)] * 2
        xr = [P.sb([128, D], F32)] * 2
        tb = [P.sb([128, D], F32)] * 2
        sq = P.sb([128, D], F32)
        x1b = [P.sb([128, D], F32)] * 2
        ffb = [P.sb([128, D], F32)] * 2
        fTf = [P.sb([128, 8, 128], F32)] * 2
        scr = P.sb([128, NE], F32); cho = P.sb([128, NE], F32); mc = P.sb([128, NE], F32)
        m8 = P.sb([128, 8, 8], F32); gs = P.sb([128, 8], F32); gm = P.sb([128, 16], F32); t8 = P.sb([128, 8], F32)
        if mode == 0:
            sk = P.sb([128, 4], F32); P.load(sk, sk[:], d_sk.ap())
            fin = [[P.sb([128, 4, 128], F32), P.sb([128, 4, 128], BF16), P.sb([128, 4, 128], BF16)] for _ in range(2)]
            ftmp = P.sb([128, 4, 128], F32)
        else:
            dg = P.sb([128, 16, 2], F32); P.load(dg, dg[:], d_dg.ap())
            fin = [[P.sb([128, 16, 128], BF16), P.sb([128, 16, 128], BF16), P.sb([128, 16, 128], BF16), P.sb([128, 16, 128], BF16)]] * 2
            ftmp = P.sb([128, 16, 128], F32); fsq = P.sb([128, 16, 128], F32); frs = P.sb([128, 4, 128], F32)
        for li, ti in enumerate(tiles):
            c0 = ti * 128
            vs = 0 if (mode == 0 and ti == 0) else NS - 1
            m = mT[li % 2]; x_ = xr[li % 2]; t = tb[li % 2]; x1 = x1b[li % 2]; ff = ffb[li % 2]; ftf = fTf[li % 2]
            f = fin[li % 2]
            P.load(x_, x_[:], d_x.ap()[c0:c0 + 128, :])
            if mode == 0:
                P.load(f[0], f[0][:], d_y.ap()[:, c0:c0 + 128].rearrange("(k p) n -> p k n", p=128))
                P.load(f[1], f[1][:], d_z.ap()[:, c0:c0 + 128].rearrange("(k p) n -> p k n", p=128), q="act")
                P.load(f[2], f[2][:], d_x0.ap()[:, c0:c0 + 128].rearrange("(k p) n -> p k n", p=128), q="act")
                P.load(m, m[:, 4:8, :], d_at.ap()[:, c0:c0 + 128].rearrange("(k p) n -> p k n", p=128))
                for k in range(4):
                    P.stt("dve", ftmp, ftmp[:, k], f[1][:, k], sk[:, k:k + 1], f[0][:, k], ALU.mult, ALU.add, reads=[f[1], sk, f[0]])
                P.tt("pool", m, m[:, 0:4, :], ftmp[:], f[2][:], ALU.mult, reads=[ftmp, f[2]])
            else:
                P.load(f[0], f[0][:], d_yf.ap()[:, c0:c0 + 128].rearrange("(k p) n -> p k n", p=128))
                P.load(f[1], f[1][:], d_yb.ap()[:, c0:c0 + 128].rearrange("(k p) n -> p k n", p=128), q="act")
                P.load(f[2], f[2][:], d_xs.ap()[:, c0:c0 + 128].rearrange("(k p) n -> p k n", p=128))
                P.load(f[3], f[3][:], d_zz.ap()[:, c0:c0 + 128].rearrange("(k p) n -> p k n", p=128), q="act")
                P.tt("pool", ftmp, ftmp[:], f[0][:], f[1][:], ALU.add, reads=[f[0], f[1]])
                for k in range(16):
                    P.stt("dve", ftmp, ftmp[:, k], f[2][:, k], dg[:, k, 0:1], ftmp[:, k], ALU.mult, ALU.add, reads=[f[2], dg, ftmp])
                P.act(fsq, fsq[:], f[3][:], AF.Silu, reads=[f[3]])
                P.tt("pool", ftmp, ftmp[:], ftmp[:], fsq[:], ALU.mult, reads=[ftmp, fsq])
                P.act(fsq, fsq[:], ftmp[:], AF.Square, reads=[ftmp])
                for g in range(4):
                    pq = pA[g % 2]
                    for k in range(4):
                        P.mm(pq, pq[:, 0:128], ones[:], fsq[:, 4 * g + k, :], start=(k == 0), stop=(k == 3), reads=[ones, fsq])
                    P.act(frs, frs[:, g, :], pq[:, 0:128], AF.Sqrt, bias=epsr[:, 0:1], scale=1.0 / 512, reads=[pq, epsr])
                P.op("dve", lambda e, frs=frs: e.reciprocal(out=frs[:], in_=frs[:]), reads=[frs], writes=[frs])
                for k in range(16):
                    P.stt("dve", m, m[:, k, :], ftmp[:, k, :], dg[:, k, 1:2], frs[:, k // 4, :], ALU.mult, ALU.mult, reads=[ftmp, dg, frs])
            pd = pD[li % 2]
            for hf in range(2):
                for k in range(KC):
                    P.mm(pd, pd[:, hf * 512:(hf + 1) * 512], m[:, k, :], wo[:, k, hf * 512:(hf + 1) * 512],
                         start=(k == 0), stop=(k == KC - 1), reads=[m, wo])
            P.tt("dve", t, t[:], pd[:], vec[:, 0, vs, :], ALU.mult, reads=[pd, vec])
            P.stt("dve", t, t[:], x_[:], DN_ALPHA, t[:], ALU.mult, ALU.add, reads=[x_, t])
            layer_norm(t, t[:], 0, 1, x1, x1[:], sq)
            P.dma("sp", lambda e, x1=x1, c0=c0: e.dma_start(out=o_x1.ap()[c0:c0 + 128, :], in_=x1[:]), reads=[x1], writes=[x1_tiles[ti]])
            P.tt("dve", ff, ff[:], x1[:], vec[:, 1, vs, :], ALU.mult, reads=[x1, vec])
            P.tt("dve", ff, ff[:], ff[:], vec[:, 2, vs, :], ALU.add, reads=[ff, vec])
            if STG < 2:
                continue
            for k in range(8):
                pq = pA[k % 2]
                P.mm(pq, pq[:, 0:128], ff[:, k * 128:(k + 1) * 128], idf[:], reads=[ff, idf])
                P.cp("act", ftf, ftf[:, k, :], pq[:, 0:128], reads=[pq])
                P.cp("dve", ffT, ffT[:, k, li * 128:(li + 1) * 128], ftf[:, k, :], reads=[ftf])
            pq = pA[li % 2]
            for k in range(8):
                P.mm(pq, pq[:, 0:NE], ftf[:, k, :], rw[:, k, :], start=(k == 0), stop=(k == 7), reads=[ftf, rw])
            P.act(scr, scr[:], pq[:, 0:NE], AF.Sigmoid, reads=[pq])
            P.tt("dve", cho, cho[:], scr[:], rb[:], ALU.add, reads=[scr, rb])
            if STG < 3:
                continue
            for g in range(8):
                P.op("dve", lambda e, g=g: e.max(out=m8[:, g, :], in_=cho[:, 32 * g:32 * g + 32]), reads=[cho], writes=[m8])
            P.tt("dve", gs, gs[:], m8[:, :, 0], m8[:, :, 1], ALU.add, reads=[m8])
            P.op("dve", lambda e: e.max(out=t8[:], in_=gs[:]), reads=[gs], writes=[t8])
            P.ts("dve", gm, gm[:, 0:8], gs[:], t8[:, 3:4], None, ALU.is_ge, reads=[gs, t8])
            P.ts("dve", gm, gm[:, 8:16], gm[:, 0:8], -1.0, BIGNEG, ALU.add, ALU.mult, reads=[gm])
            for g in range(8):
                P.ts("dve", mc, mc[:, 32 * g:32 * g + 32], cho[:, 32 * g:32 * g + 32], gm[:, g:g + 1], gm[:, 8 + g:9 + g],
                     ALU.mult, ALU.add, reads=[cho, gm])
            P.op("dve", lambda e: e.max(out=t8[:], in_=mc[:]), reads=[mc], writes=[t8])
            P.ts("dve", mc, mc[:], mc[:], t8[:, 7:8], None, ALU.is_ge, reads=[mc, t8])
            P.tt("dve", scr, scr[:], scr[:], mc[:], ALU.mult, reads=[scr, mc])
            P.op("dve", lambda e: e.reduce_sum(out=gs[:, 0:1], in_=scr[:], axis=AX.X), reads=[scr], writes=[gs])
            P.op("dve", lambda e: e.reciprocal(out=gs[:, 1:2], in_=gs[:, 0:1]), reads=[gs], writes=[gs])
            P.ts("dve", gates, gates[:, li, 0:NE], scr[:], gs[:, 1:2], ROUTED_SCALE, ALU.mult, ALU.mult, reads=[scr, gs])
        P.pop()
        P.push()
        wgs = [P.sb([128, 8, 512], BF16) for _ in range(2)]
        wds = [P.sb([128, 2, D], BF16) for _ in range(2)]
        sgs = [P.sb([128, 256], F32) for _ in range(2)]
        hs = [P.sb([128, 256], BF16) for _ in range(2)]
        hTs = [P.sb([128, 2, 128], BF16) for _ in range(2)]
        items = [(e_, li) for e_ in range(NEXR if STG >= 4 else 0) for li in range(len(tiles))]
        wcur = {}

        def stage_a(idx):
            e_, li = items[idx]
            wg = wgs[e_ % 2]; wd = wds[e_ % 2]
            if li == 0:
                P.load(wg, wg[:], d_wgu.ap()[e_], q="sp")
                P.load(wd, wd[:], d_wd.ap()[e_], q="sp")
            pa = pA[idx % 2]; sg = sgs[idx % 2]; h = hs[idx % 2]
            for k in range(8):
                P.mm(pa, pa[:, :], ffT[:, k, li * 128:(li + 1) * 128], wg[:, k, :], start=(k == 0), stop=(k == 7), reads=[ffT, wg])
            P.act(sg, sg[:], pa[:, 0:256], AF.Silu, reads=[pa])
            P.stt("dve", h, h[:], pa[:, 256:512], gates[:, li, e_:e_ + 1], sg[:], ALU.mult, ALU.mult, reads=[pa, gates, sg])

        def stage_b(idx):
            e_, li = items[idx]
            wd = wds[e_ % 2]
            pt = pT[idx % 2]; pd = pD[idx % 2]; h = hs[idx % 2]; hT = hTs[idx % 2]
            for k in range(2):
                P.mm(pt, pt[:, k, :], h[:, k * 128:(k + 1) * 128], idb[:], reads=[h, idb])
            P.cp("act", hT, hT[:], pt[:], reads=[pt])
            for hf in range(2):
                for k in range(2):
                    P.mm(pd, pd[:, hf * 512:(hf + 1) * 512], hT[:, k, :], wd[:, k, hf * 512:(hf + 1) * 512],
                         start=(k == 0), stop=(k == 1), reads=[hT, wd])
            if e_ == 0:
                P.cp("dve", acc, acc[:, li, :], pd[:], reads=[pd])
            else:
                P.tt("dve", acc, acc[:, li, :], acc[:, li, :], pd[:], ALU.add, reads=[acc, pd])

        if items:
            stage_a(0)
        for idx in range(len(items)):
            if idx + 1 < len(items):
                stage_a(idx + 1)
            stage_b(idx)
        P.pop()
        P.push()
        x1r = [P.sb([128, D], F32) for _ in range(2)]
        ob = [P.sb([128, D], F32) for _ in range(2)]
        sq = P.sb([128, D], F32)
        for li, ti in enumerate(tiles if STG >= 5 else []):
            c0 = ti * 128
            vs = 0 if (mode == 0 and ti == 0) else NS - 1
            x1 = x1r[li % 2]; o = ob[li % 2]
            P.dma("sp", lambda e, x1=x1, c0=c0: e.dma_start(out=x1[:], in_=o_x1.ap()[c0:c0 + 128, :]), reads=[x1_tiles[ti]], writes=[x1])
            P.tt("dve", acc, acc[:, li, :], acc[:, li, :], vec[:, 3, vs, :], ALU.mult, reads=[acc, vec])
            P.stt("dve", acc, acc[:, li, :], x1[:], DN_ALPHA, acc[:, li, :], ALU.mult, ALU.add, reads=[x1, acc])
            layer_norm(acc, acc[:, li, :], 2, 3, o, o[:], sq)
            P.store(o_out.ap()[c0:c0 + 128, :], o, o[:])
        P.pop()

    idh = np.eye(128, dtype=np.float32)
    maps = []
    for core in range(NCORES):
        m = {"xres": c_(xres[core]), "vecs": c_(vecs[core]), "lnv": c_(lnv_h(lnvh)), "wo": c_(w_out), "rw": c_(router_w),
             "rb": c_(np.broadcast_to(router_bias, (128, NE))), "wgu": w_gu_all, "wd": w_down_all, "idf": idh}
        m.update(feats[core])
        m.update(extra)
        maps.append(m)
    res = run_spmd(P, maps)
    if os.environ.get('POST_X1'):
        return [r["o_x1"] for r in res]
    return [r["o_out"] for r in res]


def lnv_h(v):
    return np.broadcast_to(np.stack(v)[None], (128, 4, D))


def _vecs(modl, b):
    v = np.stack([np.stack([modl[4, j], modl[b, j]]) for j in (2, 4, 3, 5)])
    return np.broadcast_to(v[None], (128, 4, 2, D))


def run_layer0(inp, mod, Ls, wb):
    x, ctx = inp["x"], inp["ctx"]
    res = launch_pre0(x, ctx, mod[0], inp["a_w_in"][0], inp["hy_conv_w"][0], inp["hy_conv_b"][0],
                      inp["mla_q_norm"][0], inp["mla_w_qb"][0], inp["mla_kv_norm"][0], inp["mla_w_kvb"][0])
    QT = assemble_seq(res, "o_q"); KT = assemble_seq(res, "o_k")
    V = assemble_seq(res, "o_v", feat_major=False).reshape(BATCH, NK, 8, 64)
    att = launch_attn(QT, KT, V)
    zT = assemble_seq(res, "o_z")
    ylat, yctx = launch_hyconv(Ls, zT[:, :, CTX:], zT[:, :, :CTX], inp["hy_skip"][0])
    feats, xres, vecs = [], [], []
    for core in range(NCORES):
        b, s = core // 2, core % 2
        cs = slice(s * 128, (s + 1) * 128); ls = slice(s * 4096, (s + 1) * 4096)
        attb = att[b].reshape(512, NK)
        feats.append({
            "yT": c_(np.concatenate([yctx[b][:, cs], ylat[b][:, ls]], 1)),
            "zT": res[core]["o_z"], "x0T": res[core]["o_x0"],
            "atT": np.ascontiguousarray(np.concatenate([attb[:, cs], attb[:, CTX + s * 4096:CTX + (s + 1) * 4096]], 1)),
        })
        xres.append(np.concatenate([ctx[b][cs], x[b][ls]], 0))
        vecs.append(_vecs(mod[0], b))
    l = 0
    wgu, wd = wb[l]
    extra = {"sk": c_(inp["hy_skip"][0].reshape(4, 128).T)}
    outs = launch_post(0, 33, feats, xres, vecs, [inp["ln_mix_g"][l], inp["ln_mix_b"][l], inp["ln_ffn_g"][l], inp["ln_ffn_b"][l]],
                       inp["a_w_out"][0], inp["router_w"][l], inp["router_bias"][l], wgu, wd, extra)
    x2 = np.zeros_like(x); c2 = np.zeros_like(ctx)
    for core in range(NCORES):
        b, s = core // 2, core % 2
        c2[b, s * 128:(s + 1) * 128] = outs[core][:128]
        x2[b, s * 4096:(s + 1) * 4096] = outs[core][128:]
    return x2, c2


def launch_pre1(x, ctx, mod1, w_in, conv_w, conv_b, dt_bias_f, dt_bias_b):
    P = Prog()
    NT = NT0
    d_xT = P.dram("xT", [128, 8, NT], F32)
    d_mod = P.dram("mod", [128, 8, 4], F32)
    d_win = P.dram("win", [40, 128, 8, 128], F32)
    d_wdt = P.dram("wdt", [128, 8, 64], F32)
    d_cw = P.dram("cw", [128, 24, 4], F32)
    d_hm = P.dram("hm", [128, 4], F32)
    d_db = P.dram("db", [64, 1], F32)
    o_z = P.dram("o_z", [2048, NTI], BF16, kind="ExternalOutput")
    o_xbc = P.dram("o_xbc", [3072, NTI], BF16, kind="ExternalOutput")
    o_dt = P.dram("o_dt", [64, NTI], F32, kind="ExternalOutput")
    tiles = _ntiles(NT)
    pbank = [P.ps([128, 512], F32) for _ in range(8)]
    pb_i = [0]

    def bank():
        pb_i[0] = (pb_i[0] + 1) % 8
        return pbank[pb_i[0]]

    hT = P.sb([128, 8, NT], BF16, "hT")
    mods = P.sb([128, 8, 4], F32); P.load(mods, mods[:], d_mod.ap())
    m1 = P.sb([128, 8, 2], F32)
    for j in range(2):
        P.ts("dve", m1, m1[:, :, j:j + 1], mods[:, :, 2 * j:2 * j + 1], 1.0, None, ALU.add, reads=[mods])
    P.push()
    xin = [P.sb([128, 8, 512], F32) for _ in range(2)]
    for ti, (s0, w) in enumerate(tiles):
        xt = xin[ti % 2]
        P.load(xt, xt[:, :, 0:w], d_xT.ap()[:, :, s0:s0 + w])
        for k in range(8):
            for (a, b_, j) in ((s0, min(s0 + w, 130), 0), (max(s0, 130), s0 + w, 1)):
                if b_ <= a:
                    continue
                P.ts("dve" if k % 2 == 0 else "pool", hT, hT[:, k, a:b_], xt[:, k, a - s0:b_ - s0],
                     m1[:, k, j:j + 1], mods[:, k, 2 * j + 1:2 * j + 2], ALU.mult, ALU.add, reads=[xt, m1, mods])
    P.pop()
    cw = P.sb([128, 24, 4], F32); hm = P.sb([128, 4], F32); db = P.sb([64, 1], F32)
    P.load(cw, cw[:], d_cw.ap()); P.load(hm, hm[:], d_hm.ap()); P.load(db, db[:], d_db.ap())
    wbuf = [P.sb([128, 8, 128], BF16) for _ in range(2)]
    urow = [P.sb([128, NT], F32) for _ in range(2)]
    cv = P.sb([128, NT], F32)
    ob = [P.sb([128, NT], BF16) for _ in range(2)]
    for oc in range(40):
        wt = wbuf[oc % 2]; ur = urow[oc % 2]; o = ob[oc % 2]
        P.load(wt, wt[:], d_win.ap()[oc], q="pool")
        cnt = [0]
        for (s0, w) in tiles:
            pt = bank()
            for k in range(8):
                P.mm(pt, pt[:, 0:w], wt[:, k, :], hT[:, k, s0:s0 + w], start=(k == 0), stop=(k == 7), reads=[wt, hT])
            cnt[0] += 1
            P.cp("act" if cnt[0] % 2 else "dve", ur, ur[:, s0:s0 + w], pt[:, 0:w], reads=[pt])
        if oc < 16:
            P.cp("pool", o, o[:], ur[:], reads=[ur])
            dst, r0 = o_z, oc * 128
        else:
            c = oc - 16
            for j, col in enumerate((0, 129, 130, 4227)):
                P.tt("dve", ur, ur[:, col:col + 1], ur[:, col:col + 1], hm[:, j:j + 1], ALU.mult, reads=[ur, hm])
            P.act(cv, cv[:], ur[:], AF.Identity, bias=cw[:, c, 3:4], scale=cw[:, c, 1:2], reads=[ur, cw])
            P.stt("dve", cv, cv[:, 1:NT], ur[:, 0:NT - 1], cw[:, c, 0:1], cv[:, 1:NT], ALU.mult, ALU.add, reads=[ur, cw, cv])
            P.stt("dve", cv, cv[:, 0:NT - 1], ur[:, 1:NT], cw[:, c, 2:3], cv[:, 0:NT - 1], ALU.mult, ALU.add, reads=[ur, cw, cv])
            P.act(o, o[:], cv[:], AF.Silu, reads=[cv])
            dst, r0 = o_xbc, c * 128
        for (a, b_, oo) in INT_SEGS:
            P.store(dst.ap()[r0:r0 + 128, oo:oo + (b_ - a)], o, o[:, a:b_])
    wdt = P.sb([128, 8, 64], BF16); P.load(wdt, wdt[:], d_wdt.ap(), q="pool")
    xd = urow[0]; ab = urow[1]; sp = cv
    for (s0, w) in tiles:
        pt = bank()
        for k in range(8):
            P.mm(pt, pt[0:64, 0:w], wdt[:, k, :], hT[:, k, s0:s0 + w], start=(k == 0), stop=(k == 7), reads=[wdt, hT])
        P.ts("dve", xd, xd[0:64, s0:s0 + w], pt[0:64, 0:w], db[:, 0:1], None, ALU.add, reads=[pt, db])
    P.act(ab, ab[0:64, :], xd[0:64, :], AF.Abs, reads=[xd])
    P.act(ab, ab[0:64, :], ab[0:64, :], AF.Exp, scale=-1.0, reads=[ab])
    P.act(ab, ab[0:64, :], ab[0:64, :], AF.Ln, bias=1.0, reads=[ab])
    P.ts("dve", sp, sp[0:64, :], xd[0:64, :], 0.0, None, ALU.max, reads=[xd])
    P.tt("dve", sp, sp[0:64, :], sp[0:64, :], ab[0:64, :], ALU.add, reads=[sp, ab])
    for (a, b_, oo) in INT_SEGS:
        P.store(o_dt.ap()[:, oo:oo + (b_ - a)], sp, sp[0:64, a:b_])

    win_h = c_(np.stack([pk(w_in[:, oc * 128:(oc + 1) * 128]) for oc in range(40)]))
    wdt_h = pk(w_in[:, 5120:5184])
    cw_h = c_(np.concatenate([conv_w.T.reshape(24, 128, 3), conv_b.reshape(24, 128, 1)], -1).transpose(1, 0, 2))
    db_h = c_(np.concatenate([dt_bias_f, dt_bias_b])[:, None])
    maps = []
    for core in range(NCORES):
        b, s = core // 2, core % 2
        ci, li = _core_tokens(b, s)
        xc, okc = _gather_rows(ctx[b], ci); xl, okl = _gather_rows(x[b], li)
        xw = np.concatenate([xc, xl], 0)
        hmv = np.array([okc[0], okc[-1], okl[0], okl[-1]], np.float32)
        modv = np.stack([mod1[4, 1], mod1[4, 0], mod1[b, 1], mod1[b, 0]], 1)
        maps.append({"xT": c_(xw.T.reshape(8, 128, NT).transpose(1, 0, 2)), "mod": c_(modv.reshape(8, 128, 4).transpose(1, 0, 2)),
                     "win": win_h, "wdt": wdt_h, "cw": cw_h, "hm": c_(np.broadcast_to(hmv, (128, 4))), "db": db_h})
    return run_spmd(P, maps)


NCH = NK // 128


def launch_scan(xs_tm, dt_tm, B_tm, BT, CT, alog):
    P = Prog()
    d_x = P.dram("x", [NK, 2048], BF16)
    d_dt = P.dram("dt", [128, NCH, 32], F32)
    d_B = P.dram("B", [NK, 512], BF16)
    d_BT = P.dram("BT", [512, NK], BF16)
    d_CT = P.dram("CT", [512, NK], BF16)
    d_al = P.dram("al", [128, 32], F32)
    d_tri = P.dram("tri", [128, 128], F32)
    d_sel = P.dram("sel", [32, 32, 128], F32)
    o_y = P.dram("o_y", [SEQ, 2048], BF16, kind="ExternalOutput")
    dt = P.sb([128, NCH, 32], F32); P.load(dt, dt[:], d_dt.ap())
    A = P.sb([128, 32], F32); P.load(A, A[:], d_al.ap())
    P.act(A, A[:], A[:], AF.Exp, reads=[A])
    P.ts("dve", A, A[:], A[:], -1.0, None, ALU.mult, reads=[A])
    tri = P.sb([128, 128], F32); P.load(tri, tri[:], d_tri.ap())
    sel = P.sb([32, 32, 128], F32); P.load(sel, sel[:], d_sel.ap())
    ones = P.sb([128, 128], F32); P.memset("dve", ones, ones[:], 1.0)
    S32 = P.sb([128, 2048], F32); P.memset("dve", S32, S32[:], 0.0)
    Sb = P.sb([128, 2048], BF16); P.memset("pool", Sb, Sb[:], 0.0)
    xb = [P.sb([128, 2048], BF16) for _ in range(2)]
    Bb = [P.sb([128, 512], BF16) for _ in range(2)]
    BTb = [P.sb([128, 4, 128], BF16) for _ in range(2)]
    CTb = [P.sb([128, 4, 128], BF16) for _ in range(2)]
    a_ = P.sb([128, 32], F32); acs = P.sb([128, 32], F32); acsT = P.sb([32, 128], F32); sm = P.sb([128, 256], F32)
    eacs = P.sb([128, 32], F32); toend = P.sb([128, 32], F32); etot = P.sb([128, 32], F32)
    xdt = P.sb([128, 2048], BF16); xw = P.sb([128, 2048], BF16)
    cbm = P.sb([128, 4, 128], F32)
    dd = [P.sb([128, 4, 128], F32) for _ in range(2)]
    MT = [P.sb([128, 4, 128], BF16) for _ in range(2)]
    yoff = P.sb([128, 2048], F32)
    yb = [P.sb([128, 2048], BF16) for _ in range(2)]
    pS = P.ps([128, 512], F32)
    pC = P.ps([128, 4, 128], F32)
    pAc = [P.ps([128, 4, 128], F32) for _ in range(2)]
    pY = [P.ps([128, 512], F32) for _ in range(2)]
    pG = [P.ps([128, 512], F32) for _ in range(2)]
    for c in range(NCH):
        x_ = xb[c % 2]; B_ = Bb[c % 2]; BT_ = BTb[c % 2]; CT_ = CTb[c % 2]; y_ = yb[c % 2]
        cs = slice(c * 128, (c + 1) * 128)
        P.load(x_, x_[:], d_x.ap()[cs, :])
        P.load(B_, B_[:], d_B.ap()[cs, :], q="act")
        P.load(BT_, BT_[:], d_BT.ap()[:, cs].rearrange("(g p) n -> p g n", p=128))
        P.load(CT_, CT_[:], d_CT.ap()[:, cs].rearrange("(g p) n -> p g n", p=128), q="act")
        P.tt("dve", a_, a_[:], dt[:, c, :], A[:], ALU.mult, reads=[dt, A])
        if c == 0:
            P.op("dve", lambda e: e.memset(sm[:], 0.0), writes=[sm])
        P.mm(pS, pS[:, 0:32], tri[:], a_[:], reads=[tri, a_])
        P.mm(pS, pS[:, 32:64], ones[:], a_[:], reads=[ones, a_])
        P.mm(pS, pS[0:32, 128:256], a_[:], tri[:], reads=[a_, tri])
        P.cp("dve", sm, sm[:], pS[:, 0:256], reads=[pS])
        P.cp("pool", acs, acs[:], sm[:, 0:32], reads=[sm])
        P.cp("pool", acsT, acsT[:], sm[0:32, 128:256], reads=[sm])
        P.act(eacs, eacs[:], sm[:, 0:32], AF.Exp, reads=[sm])
        P.act(etot, etot[:], sm[:, 32:64], AF.Exp, reads=[sm])
        P.tt("dve", toend, toend[:], sm[:, 32:64], sm[:, 0:32], ALU.subtract, reads=[sm])
        P.act(toend, toend[:], toend[:], AF.Exp, reads=[toend])
        P.tt("dve", xdt, xdt[:].rearrange("p (h d) -> p h d", d=64), x_[:].rearrange("p (h d) -> p h d", d=64),
             dt[:, c, :].unsqueeze(2).to_broadcast([128, 32, 64]), ALU.mult, reads=[x_, dt])
        P.tt("pool", xw, xw[:].rearrange("p (h d) -> p h d", d=64), xdt[:].rearrange("p (h d) -> p h d", d=64),
             toend[:].unsqueeze(2).to_broadcast([128, 32, 64]), ALU.mult, reads=[xdt, toend])
        for g in range(4):
            P.mm(pC, pC[:, g, :], BT_[:, g, :], CT_[:, g, :], reads=[BT_, CT_])
        P.tt("dve", cbm, cbm[:], pC[:], tri[:].unsqueeze(1).to_broadcast([128, 4, 128]), ALU.mult, reads=[pC, tri])
        for g in range(4):
            py = pY[g % 2]
            for q4 in range(2):
                h0 = g * 8 + q4 * 4
                pa = pAc[q4]; d_ = dd[q4]; mt = MT[q4]
                for j in range(4):
                    P.mm(pa, pa[:, j, :], sel[:, h0 + j, :], acsT[:], reads=[sel, acsT])
                P.tt("dve", d_, d_[:], pa[:], acs[:, h0:h0 + 4].unsqueeze(2).to_broadcast([128, 4, 128]), ALU.subtract, reads=[pa, acs])
                P.act(d_, d_[:], d_[:], AF.Exp, reads=[d_])
                P.stt("dve", mt, mt[:], d_[:], 1.0, cbm[:, g, :].unsqueeze(1).to_broadcast([128, 4, 128]), ALU.min, ALU.mult, reads=[d_, cbm])
                for j in range(4):
                    h = h0 + j
                    P.mm(py, py[:, (h % 8) * 64:(h % 8 + 1) * 64], mt[:, j, :], xdt[:, h * 64:(h + 1) * 64], reads=[mt, xdt])
            if c >= 2:
                po = pG[g % 2]
                P.mm(po, po[:], CT_[:, g, :], Sb[:, g * 512:(g + 1) * 512], reads=[CT_, Sb])
                P.tt("dve", yoff, yoff[:, g * 512:(g + 1) * 512].rearrange("p (h d) -> p h d", d=64),
                     po[:].rearrange("p (h d) -> p h d", d=64),
                     eacs[:, g * 8:(g + 1) * 8].unsqueeze(2).to_broadcast([128, 8, 64]), ALU.mult, reads=[po, eacs])
                P.tt("dve", y_, y_[:, g * 512:(g + 1) * 512], py[:], yoff[:, g * 512:(g + 1) * 512], ALU.add, reads=[py, yoff])
        if c >= 2:
            P.store(o_y.ap()[(c - 2) * 128:(c - 1) * 128, :], y_, y_[:])
        if c == 0:
            pass
        P.tt("pool", S32, S32[:].rearrange("p (h d) -> p h d", d=64), S32[:].rearrange("p (h d) -> p h d", d=64),
             etot[:].unsqueeze(2).to_broadcast([128, 32, 64]), ALU.mult, reads=[S32, etot])
        for g in range(4):
            po = pG[g % 2]
            P.mm(po, po[:], B_[:, g * 128:(g + 1) * 128], xw[:, g * 512:(g + 1) * 512], reads=[B_, xw])
            P.tt("dve", S32, S32[:, g * 512:(g + 1) * 512], S32[:, g * 512:(g + 1) * 512], po[:], ALU.add, reads=[S32, po])
        P.cp("act", Sb, Sb[:], S32[:], reads=[S32])
    trih = np.triu(np.ones((128, 128), np.float32))
    selh = np.zeros((32, 32, 128), np.float32)
    for h in range(32):
        selh[h, h, :] = 1.0
    maps = []
    for core in range(NCORES):
        maps.append({"x": np.ascontiguousarray(xs_tm[core]), "dt": c_(dt_tm[core].reshape(NCH, 128, 32).transpose(1, 0, 2)),
                     "B": np.ascontiguousarray(B_tm[core]), "BT": np.ascontiguousarray(BT[core]), "CT": np.ascontiguousarray(CT[core]),
                     "al": c_(np.broadcast_to(alog[core], (128, 32))), "tri": trih, "sel": selh})
    res = run_spmd(P, maps)
    return [r["o_y"] for r in res]


def run_layer1(inp, mod, x2, c2, wb):
    res = launch_pre1(x2, c2, mod[1], inp["ssd_w_in"][0], inp["ssd_conv_w"][0], inp["ssd_conv_b"][0],
                      inp["ssd_dt_bias_f"][0], inp["ssd_dt_bias_b"][0])
    xbc = assemble_seq(res, "o_xbc")
    dtT = assemble_seq(res, "o_dt")
    xs_tm, dt_tm, B_tm, BT, CT, alog = [], [], [], [], [], []
    for core in range(NCORES):
        b, d_ = core // 2, core % 2
        if d_ == 0:
            order = np.arange(NK)
        else:
            order = np.concatenate([np.arange(CTX)[::-1], CTX + np.arange(SEQ)[::-1]])
        xo = xbc[b][:, order]
        xs_tm.append(xo[:2048].T); B_tm.append(xo[2048:2560].T); BT.append(xo[2048:2560]); CT.append(xo[2560:3072])
        dt_tm.append(dtT[b][32 * d_:32 * (d_ + 1)][:, order].T)
        alog.append(inp["ssd_a_log_f"][0] if d_ == 0 else inp["ssd_a_log_b"][0])
    ys = launch_scan(xs_tm, dt_tm, B_tm, BT, CT, alog)
    feats, xres, vecs = [], [], []
    l = 1
    for core in range(NCORES):
        b, s = core // 2, core % 2
        ls = slice(s * 4096, (s + 1) * 4096)
        yf = ys[2 * b][ls]; ybk = ys[2 * b + 1][::-1][ls]
        feats.append({"yfT": np.ascontiguousarray(yf.T), "ybT": np.ascontiguousarray(ybk.T),
                      "xsT": np.ascontiguousarray(res[core]["o_xbc"][:2048, 128:]),
                      "zzT": np.ascontiguousarray(res[core]["o_z"][:, 128:])})
        xres.append(x2[b][ls])
        vecs.append(_vecs(mod[1], b)[:, :, 1:2, :])
    wgu, wd = wb[l]
    dfull = np.repeat(inp["ssd_d"][0], 64)
    extra = {"dg": c_(np.stack([dfull.reshape(16, 128).T, inp["ssd_norm_g"][0].reshape(16, 128).T], -1))}
    outs = launch_post(1, 32, feats, xres, vecs, [inp["ln_mix_g"][l], inp["ln_mix_b"][l], inp["ln_ffn_g"][l], inp["ln_ffn_b"][l]],
                       inp["ssd_w_out"][0], inp["router_w"][l], inp["router_bias"][l], wgu, wd, extra)
    out = np.zeros((BATCH, SEQ, D), np.float32)
    for core in range(NCORES):
        b, s = core // 2, core % 2
        out[b, s * 4096:(s + 1) * 4096] = outs[core]
    return out


def kernel(**inp):
    inp = {k: np.asarray(v) for k, v in inp.items()}
    wgu_l = [np.concatenate([inp["exp_w_gu"][l], inp["sh_w_gu"][l][None]], 0) for l in range(DEPTH)]
    wd_l = [np.concatenate([inp["exp_w_down"][l], inp["sh_w_down"][l][None]], 0) for l in range(DEPTH)]
    mod, wb = launch_mod(inp["c"], inp["c_ctx"], inp["mod_w"], inp["mod_b"], wgu_l, wd_l)
    del wgu_l, wd_l
    Ls = launch_filt(*[inp[k][0] for k in ["hy_filt_w1", "hy_filt_b1", "hy_filt_freq1", "hy_filt_w2", "hy_filt_b2",
                                           "hy_filt_freq2", "hy_filt_w3"]])
    x2, c2 = run_layer0(inp, mod, Ls, wb)
    return run_layer1(inp, mod, x2, c2, wb)
```

```python
import math
from contextlib import ExitStack
import numpy as np
import ml_dtypes
import concourse.bass as bass
import concourse.mybir as mybir
from concourse.bass_utils import run_bass_kernel_spmd

F32 = mybir.dt.float32
BF16 = mybir.dt.bfloat16
I32 = mybir.dt.int32
AF = mybir.ActivationFunctionType
ALU = mybir.AluOpType
AX = mybir.AxisListType
NPBF = ml_dtypes.bfloat16

ENGS = ["pe", "act", "dve", "pool", "sp"]
NDSEM = {"sp": 8, "pool": 6, "act": 2}
NCORES = 8

D = 1024
BATCH = 4
SEQ = 8192
CTX = 256
DEPTH = 2
GRID_W = 64
HYW = 512
HY_EMB = 33
HY_BANDS = 16
HY_DECAY_MIN = math.log(1e-2) / 1.5
HY_DECAY_MAX = math.log(1e-2) / 0.3
MLA_SCALE = 96 ** -0.5
DN_ALPHA = (2 * DEPTH) ** 0.25
LN_EPS = 1e-5
RMS_EPS = 1e-6
NE = 256
ROUTED_SCALE = 2.5
PI = math.pi


class Tile:
    def __init__(self, t, name):
        self.t = t
        self.name = name
        self.w = None
        self.r = {}

    def __getitem__(self, idx):
        return self.t[idx]


class Prog:
    def __init__(self):
        self.nc = bass.Bass("TRN2", target_bir_lowering=False)
        self.stk = [ExitStack()]
        self.ops = {e: [] for e in ENGS}
        self.cnt = {e: 0 for e in ENGS}
        self.seen = {e: {} for e in ENGS}
        self.sems = {}
        for e in ["pe", "act", "dve", "pool"]:
            self.sems[e] = self.stk[0].enter_context(self.nc.semaphore("s_" + e))
        self.dtot = {}
        self.drot = {q: 0 for q in NDSEM}
        for q, n in NDSEM.items():
            for i in range(n):
                k = "d_%s_%d" % (q, i)
                self.sems[k] = self.stk[0].enter_context(self.nc.semaphore(k))
                self.dtot[k] = 0
        self.nalloc = 0

    def dram(self, name, shape, dtype, kind="ExternalInput"):
        return self.nc.dram_tensor(name, list(shape), dtype, kind=kind)

    def push(self):
        self.stk.append(ExitStack())

    def pop(self):
        self.barrier()
        self.stk.pop().close()

    def sb(self, shape, dtype, name=None):
        self.nalloc += 1
        name = (name or "sb") + "_%d" % self.nalloc
        t = self.stk[-1].enter_context(self.nc.sbuf_tensor(name, list(shape), dtype))
        return Tile(t, name)

    def ps(self, shape, dtype, name=None):
        self.nalloc += 1
        name = (name or "ps") + "_%d" % self.nalloc
        t = self.stk[-1].enter_context(self.nc.psum_tensor(name, list(shape), dtype))
        return Tile(t, name)

    def _collect(self, eng, reads, writes):
        waits = {}

        def add(ev, same_ok):
            if ev is None:
                return
            k, v = ev
            if k == eng and not same_ok:
                return
            if waits.get(k, 0) < v:
                waits[k] = v

        for t in reads:
            add(t.w, True)
        for t in writes:
            add(t.w, False)
            for k, v in t.r.items():
                add((k, v), False)
        need = []
        for k, v in waits.items():
            if self.seen[eng].get(k, 0) >= v:
                continue
            self.seen[eng][k] = v
            need.append((k, v))
        return need

    def _commit(self, ev, reads, writes):
        k, v = ev
        for t in reads:
            if t.r.get(k, 0) < v:
                t.r[k] = v
        for t in writes:
            t.w = ev
            t.r = {}

    def op(self, eng, fn, reads=(), writes=()):
        need = self._collect(eng, reads, writes)
        self.cnt[eng] += 1
        ev = (eng, self.cnt[eng])
        self.ops[eng].append((need, fn, eng))
        self._commit(ev, reads, writes)
        return ev

    def dma(self, q, fn, reads=(), writes=()):
        need = self._collect(q, reads, writes)
        i = self.drot[q]
        self.drot[q] = (i + 1) % NDSEM[q]
        k = "d_%s_%d" % (q, i)
        if self.dtot[k] > 0 and self.seen[q].get(k, 0) < self.dtot[k]:
            self.seen[q][k] = self.dtot[k]
            need.append((k, self.dtot[k]))
        self.dtot[k] += 16
        ev = (k, self.dtot[k])
        self.ops[q].append((need, fn, k))
        self._commit(ev, reads, writes)
        return ev

    def barrier(self):
        allev = [(k, tot) for k, tot in self.dtot.items() if tot > 0]
        allev += [(e, self.cnt[e]) for e in ["pe", "act", "dve", "pool"] if self.cnt[e] > 0]
        for e in ENGS:
            need = []
            for k, v in allev:
                if k == e:
                    continue
                if self.seen[e].get(k, 0) >= v:
                    continue
                self.seen[e][k] = v
                need.append((k, v))
            if need:
                self.ops[e].append((need, None, None))

    def build(self):
        self.barrier()
        nc = self.nc
        sems = self.sems

        def replay(eng_name, e):
            for need, fn, inc in self.ops[eng_name]:
                for k, v in need:
                    e.wait_ge(sems[k], v)
                if fn is None:
                    continue
                ins = fn(e)
                if inc is not None:
                    ins.then_inc(sems[inc], 16 if inc.startswith("d_") else 1)

        with nc.Block() as block:
            @block.tensor
            def _(e):
                replay("pe", e)

            @block.scalar
            def _(e):
                replay("act", e)

            @block.vector
            def _(e):
                replay("dve", e)

            @block.gpsimd
            def _(e):
                replay("pool", e)

            @block.sync
            def _(e):
                replay("sp", e)
        while self.stk:
            self.stk.pop().close()
        return nc

    def load(self, t, out, in_, q="sp"):
        self.dma(q, lambda e: e.dma_start(out=out, in_=in_), writes=[t])

    def store(self, out, t, in_, q="sp"):
        self.dma(q, lambda e: e.dma_start(out=out, in_=in_), reads=[t])

    def mm(self, pt, out, lhsT, rhs, start=True, stop=True, reads=()):
        self.op("pe", lambda e: e.matmul(out, lhsT=lhsT, rhs=rhs, start=start, stop=stop),
                reads=reads, writes=[pt])

    def tr(self, pt, out, in_, ident, reads=()):
        self.op("pe", lambda e: e.transpose(out, in_, ident), reads=reads, writes=[pt])

    def act(self, ot, out, in_, func, bias=0.0, scale=1.0, reads=(), accum=None):
        if accum is None:
            fn = lambda e: e.activation(out=out, in_=in_, func=func, bias=bias, scale=scale)
        else:
            fn = lambda e: e.activation(out=out, in_=in_, func=func, bias=bias, scale=scale, accum_out=accum)
        self.op("act", fn, reads=reads, writes=[ot] if not isinstance(ot, (list, tuple)) else list(ot))

    def ts(self, eng, ot, out, in0, s1, s2, op0, op1=None, reads=()):
        if op1 is None:
            fn = lambda e: e.tensor_scalar(out=out, in0=in0, scalar1=s1, scalar2=None, op0=op0)
        else:
            fn = lambda e: e.tensor_scalar(out=out, in0=in0, scalar1=s1, scalar2=s2, op0=op0, op1=op1)
        self.op(eng, fn, reads=reads, writes=[ot])

    def tt(self, eng, ot, out, in0, in1, op, reads=()):
        self.op(eng, lambda e: e.tensor_tensor(out=out, in0=in0, in1=in1, op=op), reads=reads, writes=[ot])

    def stt(self, eng, ot, out, in0, scalar, in1, op0, op1, reads=()):
        self.op(eng, lambda e: e.scalar_tensor_tensor(out=out, in0=in0, scalar=scalar, in1=in1, op0=op0, op1=op1),
                reads=reads, writes=[ot])

    def cp(self, eng, ot, out, in_, reads=()):
        if eng == "act":
            self.op(eng, lambda e: e.copy(out=out, in_=in_), reads=reads, writes=[ot])
        else:
            self.op(eng, lambda e: e.tensor_copy(out=out, in_=in_), reads=reads, writes=[ot])

    def memset(self, eng, ot, out, val):
        self.op(eng, lambda e: e.memset(out, val), writes=[ot])


def run_spmd(P, in_maps):
    nc = P.build()
    res = run_bass_kernel_spmd(nc, in_maps, core_ids=list(range(NCORES)))
    return res.results


def c_(a, dt=np.float32):
    return np.ascontiguousarray(a, dtype=dt)


def pk(a):
    kp, n = a.shape
    return c_(a.reshape(kp // 128, 128, n).transpose(1, 0, 2))


EPC = (NE + 1 + NCORES - 1) // NCORES
NSLOT = DEPTH * EPC


def launch_mod(c, c_ctx, mod_w, mod_b, wgu_layers, wd_layers):
    P = Prog()
    NCOL = 6 * D // NCORES
    d_cg = P.dram("cwgu", [NSLOT, D, 512], F32)
    d_cd = P.dram("cwd", [NSLOT, 256, D], F32)
    o_cg = P.dram("o_wgu", [NSLOT, 128, 8, 512], BF16, kind="ExternalOutput")
    o_cd = P.dram("o_wd", [NSLOT, 128, 2, D], BF16, kind="ExternalOutput")
    cgb = [P.sb([128, 8, 512], BF16) for _ in range(3)]
    cdb = [P.sb([128, 2, D], BF16) for _ in range(3)]
    for sl in range(NSLOT):
        g = cgb[sl % 3]; dd_ = cdb[sl % 3]
        P.load(g, g[:], d_cg.ap()[sl].rearrange("(k p) n -> p k n", p=128), q="pool")
        P.load(dd_, dd_[:], d_cd.ap()[sl].rearrange("(k p) n -> p k n", p=128), q="pool")
        P.store(o_cg.ap()[sl], g, g[:], q="sp")
        P.store(o_cd.ap()[sl], dd_, dd_[:], q="act")
    cT = P.dram("cT", [128, 8, 5], F32)
    mw = P.dram("mw", [DEPTH, 128, 8, NCOL], F32)
    mb = P.dram("mb", [DEPTH, 5, NCOL], F32)
    out = P.dram("out", [DEPTH, 5, NCOL], F32, kind="ExternalOutput")
    cs = P.sb([128, 8, 5], F32)
    sc = P.sb([128, 8, 5], F32)
    P.load(cs, cs[:], cT.ap())
    P.act(sc, sc[:], cs[:], AF.Silu, reads=[cs])
    for l in range(DEPTH):
        w = P.sb([128, 8, NCOL], F32)
        b = P.sb([5, NCOL], F32)
        o = P.sb([5, NCOL], F32)
        P.load(w, w[:], mw.ap()[l])
        P.load(b, b[:], mb.ap()[l])
        for hf in range(2):
            pt = P.ps([128, 512], F32)
            for k in range(8):
                P.mm(pt, pt[0:5, 0:384], sc[:, k, :], w[:, k, hf * 384:(hf + 1) * 384],
                     start=(k == 0), stop=(k == 7), reads=[sc, w])
            P.tt("dve", o, o[:, hf * 384:(hf + 1) * 384], pt[0:5, 0:384], b[:, hf * 384:(hf + 1) * 384],
                 ALU.add, reads=[pt, b])
        P.store(out.ap()[l], o, o[:])
    cc = np.concatenate([c, c_ctx[None]], 0)
    cTh = c_(cc.T.reshape(8, 128, 5).transpose(1, 0, 2))
    maps = []
    for i in range(NCORES):
        sl = slice(i * NCOL, (i + 1) * NCOL)
        cg = np.zeros((NSLOT, D, 512), np.float32); cd = np.zeros((NSLOT, 256, D), np.float32)
        for l in range(DEPTH):
            e0, e1 = i * EPC, min((i + 1) * EPC, NE + 1)
            cg[l * EPC:l * EPC + (e1 - e0)] = wgu_layers[l][e0:e1]
            cd[l * EPC:l * EPC + (e1 - e0)] = wd_layers[l][e0:e1]
        maps.append({
            "cT": cTh,
            "mw": c_(np.stack([pk(mod_w[l][:, sl]) for l in range(DEPTH)])),
            "mb": c_(np.stack([np.broadcast_to(mod_b[l][sl], (5, NCOL)) for l in range(DEPTH)])),
            "cwgu": cg, "cwd": cd,
        })
    res = run_spmd(P, maps)
    mod = np.concatenate([r["out"] for r in res], axis=2)
    wb = []
    for l in range(DEPTH):
        g = np.concatenate([r["o_wgu"][l * EPC:(l + 1) * EPC] for r in res], 0)[:NE + 1]
        d_ = np.concatenate([r["o_wd"][l * EPC:(l + 1) * EPC] for r in res], 0)[:NE + 1]
        wb.append((np.ascontiguousarray(g), np.ascontiguousarray(d_)))
    return mod.reshape(DEPTH, 5, 6, D), wb


def _filt_consts(n):
    f32 = np.float32
    t = np.linspace(0.0, 1.0, n, dtype=f32)
    w = (2.0 * math.pi * np.arange(n, dtype=f32) / n).astype(f32)
    fr = np.linspace(1e-4, HY_BANDS - 1, HY_BANDS, dtype=f32)
    z = np.concatenate([t[:, None], np.cos(fr[None] * w[:, None]), -np.sin(fr[None] * w[:, None])], -1).astype(f32)
    idx = np.concatenate([np.minimum(n - np.arange(n), n - 1), np.arange(n)])
    zext = c_(z[idx].T)
    text = c_(t[idx])
    return zext, text


def launch_filt(w1, b1, f1, w2, b2, f2, w3):
    P = Prog()
    CH = HYW // NCORES
    ns = [SEQ, CTX]
    d_w1 = P.dram("w1", [33, 64], F32)
    d_w2 = P.dram("w2", [64, 64], F32)
    d_vec = P.dram("vec", [64, 4], F32)
    d_w3 = P.dram("w3", [64, 2, CH], F32)
    d_nd = P.dram("nd", [CH, 1], F32)
    d_z = [P.dram("z%d" % i, [33, 2 * n], F32) for i, n in enumerate(ns)]
    d_t = [P.dram("t%d" % i, [CH, 2 * n], F32) for i, n in enumerate(ns)]
    d_o = [P.dram("o%d" % i, [CH, 2 * n], BF16, kind="ExternalOutput") for i, n in enumerate(ns)]
    w1s = P.sb([33, 64], F32); w2s = P.sb([64, 64], F32); vec = P.sb([64, 4], F32)
    w3s = P.sb([64, 2, CH], F32); nd = P.sb([CH, 1], F32)
    P.load(w1s, w1s[:], d_w1.ap()); P.load(w2s, w2s[:], d_w2.ap()); P.load(vec, vec[:], d_vec.ap())
    P.load(w3s, w3s[:], d_w3.ap()); P.load(nd, nd[:], d_nd.ap())
    off = P.sb([64, 4], F32)
    for j in range(2):
        P.ts("dve", off, off[:, j:j + 1], vec[:, 2 * j:2 * j + 1], vec[:, 2 * j + 1:2 * j + 2], 1.0 / (2 * PI),
             ALU.mult, ALU.mult, reads=[vec])
        P.ts("dve", off, off[:, 2 + j:3 + j], vec[:, 2 * j + 1:2 * j + 2], 1.0 / (2 * PI), None,
             ALU.mult, reads=[vec])
    pts = [P.ps([128, 512], F32) for _ in range(6)]
    for i, n in enumerate(ns):
        P.push()
        L = P.sb([CH, 2 * n], F32)
        zss = [P.sb([33, 512], F32) for _ in range(2)]
        txs = [P.sb([CH, 512], F32) for _ in range(2)]
        fb = [[P.sb([64, 512], F32) for _ in range(5)] for _ in range(2)]
        ki = P.sb([64, 512], I32); kf = P.sb([64, 512], F32)
        TW = min(512, n)
        for ti in range(2 * n // TW):
            sl = slice(ti * TW, (ti + 1) * TW)
            p1, p2, p3 = pts[(ti % 2) * 3:(ti % 2) * 3 + 3]
            a1, h1, a2, h2, dec = fb[ti % 2]
            zs = zss[ti % 2]; tx = txs[ti % 2]
            P.load(zs, zs[:, 0:TW], d_z[i].ap()[:, sl])
            P.load(tx, tx[:, 0:TW], d_t[i].ap()[:, sl])
            P.mm(p1, p1[0:64, 0:TW], w1s[:], zs[:, 0:TW], reads=[w1s, zs])
            P.ts("dve", a1, a1[:, 0:TW], p1[0:64, 0:TW], off[:, 2:3], off[:, 0:1], ALU.mult, ALU.add, reads=[p1, off])
            P.cp("dve", ki, ki[:, 0:TW], a1[:, 0:TW], reads=[a1])
            P.cp("dve", kf, kf[:, 0:TW], ki[:, 0:TW], reads=[ki])
            P.tt("dve", a1, a1[:, 0:TW], a1[:, 0:TW], kf[:, 0:TW], ALU.subtract, reads=[a1, kf])
            P.act(h1, h1[:, 0:TW], a1[:, 0:TW], AF.Sin, scale=2 * PI, reads=[a1])
            P.mm(p2, p2[0:64, 0:TW], w2s[:], h1[:, 0:TW], reads=[w2s, h1])
            P.ts("dve", a2, a2[:, 0:TW], p2[0:64, 0:TW], off[:, 3:4], off[:, 1:2], ALU.mult, ALU.add, reads=[p2, off])
            P.cp("dve", ki, ki[:, 0:TW], a2[:, 0:TW], reads=[a2])
            P.cp("dve", kf, kf[:, 0:TW], ki[:, 0:TW], reads=[ki])
            P.tt("dve", a2, a2[:, 0:TW], a2[:, 0:TW], kf[:, 0:TW], ALU.subtract, reads=[a2, kf])
            P.act(h2, h2[:, 0:TW], a2[:, 0:TW], AF.Sin, scale=2 * PI, reads=[a2])
            d = 1 if (ti * TW) < n else 0
            P.mm(p3, p3[0:CH, 0:TW], w3s[:, d, :], h2[:, 0:TW], reads=[w3s, h2])
            P.act(dec, dec[:, 0:TW], tx[:, 0:TW], AF.Exp, scale=nd[:, 0:1], reads=[tx, nd])
            P.tt("dve", L, L[:, sl], p3[0:CH, 0:TW], dec[:, 0:TW], ALU.mult, reads=[p3, dec])
        P.memset("dve", L, L[:, 0:1], 0.0)
        ab = P.sb([CH, 2 * n], F32)
        sm = P.sb([CH, 2], F32)
        P.act(ab, ab[:], L[:], AF.Abs, reads=[L])
        P.op("dve", lambda e, sm=sm, ab=ab: e.reduce_sum(out=sm[:, 0:1], in_=ab[:], axis=AX.X), reads=[ab], writes=[sm])
        P.op("dve", lambda e, sm=sm: e.reciprocal(out=sm[:, 1:2], in_=sm[:, 0:1]), reads=[sm], writes=[sm])
        Lb = P.sb([CH, 2 * n], BF16)
        P.ts("dve", Lb, Lb[:], L[:], sm[:, 1:2], None, ALU.mult, reads=[L, sm])
        P.store(d_o[i].ap(), Lb, Lb[:])
        P.pop()
    deltas = np.abs(np.linspace(HY_DECAY_MIN, HY_DECAY_MAX, HYW, dtype=np.float32))
    consts = [_filt_consts(n) for n in ns]
    maps = []
    for i in range(NCORES):
        ch = slice(i * CH, (i + 1) * CH)
        m = {"w1": c_(w1), "w2": c_(w2), "vec": c_(np.stack([b1, f1, b2, f2], 1)),
             "w3": c_(np.stack([w3[:, :HYW][:, ch], w3[:, HYW:][:, ch]], 1)),
             "nd": c_(-deltas[ch][:, None])}
        for j, n in enumerate(ns):
            m["z%d" % j] = consts[j][0]
            m["t%d" % j] = c_(np.broadcast_to(consts[j][1], (CH, 2 * n)))
        maps.append(m)
    res = run_spmd(P, maps)
    Ls = [np.concatenate([r["o%d" % j] for r in res], 0) for j in range(2)]
    return Ls


NT0 = 4228
INT_SEGS = [(1, 129, 0), (131, 4227, 128)]
NTI = 4224


def _ntiles(n, w=512):
    return [(s, min(w, n - s)) for s in range(0, n, w)]


def _core_tokens(b, s):
    ci = np.arange(s * 128 - 1, s * 128 + 129)
    li = np.arange(s * 4096 - 1, s * 4096 + 4097)
    return ci, li


def _gather_rows(arr, idx):
    n = arr.shape[0]
    ok = (idx >= 0) & (idx < n)
    out = arr[np.clip(idx, 0, n - 1)].copy()
    out[~ok] = 0
    return out, ok


def _rope_tables():
    f32 = np.float32
    rows = SEQ // GRID_W
    row = np.repeat(np.arange(rows), GRID_W).astype(f32)
    col = np.tile(np.arange(GRID_W), rows).astype(f32)
    half = 16
    inv = (10000.0 ** (-np.arange(0, half, 2, dtype=f32) / half)).astype(f32)
    ang = np.concatenate([row[:, None] * inv, col[:, None] * inv], -1)
    cos = np.cos(ang).astype(f32); sin = np.sin(ang).astype(f32)
    cosT = np.ones((96, SEQ), f32); sinT = np.zeros((96, SEQ), f32)
    for j in range(32):
        cosT[64 + j] = cos[:, j // 2]
        sinT[64 + j] = sin[:, j // 2] * (-1.0 if j % 2 == 0 else 1.0)
    return cosT, sinT


def launch_pre0(x, ctx, mod0, w_in, conv_w, conv_b, q_norm, w_qb, kv_norm, w_kvb):
    P = Prog()
    NT = NT0
    d_xT = P.dram("xT", [128, 8, NT], F32)
    d_mod = P.dram("mod", [128, 8, 4], F32)
    d_win = P.dram("win", [12, 128, 8, 128], F32)
    d_wq = P.dram("wq", [128, 8, 256], F32)
    d_wkv = P.dram("wkv", [128, 8, 128], F32)
    d_wkp = P.dram("wkp", [2, 128, 8, 96], F32)
    d_cw = P.dram("cw", [128, 12, 4], F32)
    d_hm = P.dram("hm", [128, 4], F32)
    d_cos = P.dram("cos", [96, NT], F32)
    d_sin = P.dram("sin", [96, NT], F32)
    d_gq = P.dram("gq", [128, 3], F32)
    d_wqb = P.dram("wqb", [2, 128, 2, 8, 96], F32)
    d_wkn = P.dram("wkn", [128, 8, 64], F32)
    d_wv = P.dram("wv", [128, 512], F32)
    o_x0 = P.dram("o_x0", [512, NTI], BF16, kind="ExternalOutput")
    o_z = P.dram("o_z", [512, NTI], BF16, kind="ExternalOutput")
    o_q = P.dram("o_q", [8, 96, NTI], BF16, kind="ExternalOutput")
    o_k = P.dram("o_k", [8, 96, NTI], BF16, kind="ExternalOutput")
    o_v = P.dram("o_v", [NTI, 512], BF16, kind="ExternalOutput")

    tiles = _ntiles(NT)
    pbank = [P.ps([128, 512], F32) for _ in range(8)]
    pb_i = [0]

    def bank():
        pb_i[0] = (pb_i[0] + 1) % 8
        return pbank[pb_i[0]]

    hT = P.sb([128, 8, NT], BF16, "hT")
    epsq = P.sb([128, 1], F32); P.memset("dve", epsq, epsq[:], RMS_EPS)
    mods = P.sb([128, 8, 4], F32)
    P.load(mods, mods[:], d_mod.ap())
    m1 = P.sb([128, 8, 2], F32)
    for j in range(2):
        P.ts("dve", m1, m1[:, :, j:j + 1], mods[:, :, 2 * j:2 * j + 1], 1.0, None, ALU.add, reads=[mods])
    P.push()
    xin = [P.sb([128, 8, 512], F32) for _ in range(2)]
    for ti, (s0, w) in enumerate(tiles):
        xt = xin[ti % 2]
        P.load(xt, xt[:, :, 0:w], d_xT.ap()[:, :, s0:s0 + w])
        for k in range(8):
            for (a, b_, j) in ((s0, min(s0 + w, 130), 0), (max(s0, 130), s0 + w, 1)):
                if b_ <= a:
                    continue
                P.ts("dve" if k % 2 == 0 else "pool", hT, hT[:, k, a:b_], xt[:, k, a - s0:b_ - s0],
                     m1[:, k, j:j + 1], mods[:, k, 2 * j + 1:2 * j + 2], ALU.mult, ALU.add, reads=[xt, m1, mods])
    P.pop()

    def project(pt_rows, wt, wsel, dst_fn):
        for (s0, w) in tiles:
            pt = bank()
            for k in range(8):
                P.mm(pt, pt[0:pt_rows, 0:w], wsel(k), hT[:, k, s0:s0 + w], start=(k == 0), stop=(k == 7),
                     reads=[wt, hT])
            dst_fn(pt, s0, w)

    P.push()
    cw = P.sb([128, 12, 4], F32); hm = P.sb([128, 4], F32)
    P.load(cw, cw[:], d_cw.ap()); P.load(hm, hm[:], d_hm.ap())
    wbuf = [P.sb([128, 8, 128], BF16) for _ in range(2)]
    urow = [P.sb([128, NT], F32) for _ in range(2)]
    cv = P.sb([128, NT], F32)
    x1c = P.sb([128, 4, NT], BF16)
    ob = [P.sb([128, NT], BF16) for _ in range(2)]
    for oc in range(12):
        wt = wbuf[oc % 2]; ur = urow[oc % 2]
        P.load(wt, wt[:], d_win.ap()[oc], q="pool")
        cnt = [0]

        def evac(pt, s0, w, ur=ur, cnt=cnt):
            cnt[0] += 1
            P.cp("act" if cnt[0] % 2 else "dve", ur, ur[:, s0:s0 + w], pt[:, 0:w], reads=[pt])
        project(128, wt, lambda k, wt=wt: wt[:, k, :], evac)
        for j, c in enumerate((0, 129, 130, 4227)):
            P.tt("dve", ur, ur[:, c:c + 1], ur[:, c:c + 1], hm[:, j:j + 1], ALU.mult, reads=[ur, hm])
        P.act(cv, cv[:], ur[:], AF.Identity, bias=cw[:, oc, 3:4], scale=cw[:, oc, 1:2], reads=[ur, cw])
        P.stt("dve", cv, cv[:, 1:NT], ur[:, 0:NT - 1], cw[:, oc, 0:1], cv[:, 1:NT], ALU.mult, ALU.add, reads=[ur, cw, cv])
        P.stt("dve", cv, cv[:, 0:NT - 1], ur[:, 1:NT], cw[:, oc, 2:3], cv[:, 0:NT - 1], ALU.mult, ALU.add, reads=[ur, cw, cv])
        if 4 <= oc < 8:
            P.cp("pool", x1c, x1c[:, oc - 4, :], cv[:], reads=[cv])
        else:
            o = ob[oc % 2]
            if oc < 4:
                P.cp("pool", o, o[:], cv[:], reads=[cv])
                dst = o_x0
            else:
                P.tt("pool", o, o[:], cv[:], x1c[:, oc - 8, :], ALU.mult, reads=[cv, x1c])
                dst = o_z
            r0 = (oc % 4) * 128
            for (a, b_, oo) in INT_SEGS:
                P.store(dst.ap()[r0:r0 + 128, oo:oo + (b_ - a)], o, o[:, a:b_])
    P.pop()

    P.push()
    wq = P.sb([128, 8, 256], BF16); wkv = P.sb([128, 8, 128], BF16); wkp = P.sb([128, 2, 8, 96], BF16)
    P.load(wq, wq[:], d_wq.ap(), q="pool"); P.load(wkv, wkv[:], d_wkv.ap(), q="pool")
    for j in range(2):
        P.load(wkp, wkp[:, j], d_wkp.ap()[j], q="pool")
    cosT = P.sb([96, NT], BF16); sinT = P.sb([96, NT], BF16)
    P.load(cosT, cosT[:], d_cos.ap(), q="pool"); P.load(sinT, sinT[:], d_sin.ap(), q="pool")
    gq = P.sb([128, 3], F32); P.load(gq, gq[:], d_gq.ap())
    ones = P.sb([128, 128], F32); P.memset("dve", ones, ones[:], 1.0)
    qn = P.sb([128, 3, NT], BF16)
    kpe = P.sb([96, NT], BF16)
    tA = P.sb([96, 512], F32); tB = P.sb([96, 512], F32)
    P.push()
    uq = P.sb([128, 3, NT], F32)
    for c in range(3):
        def evq(pt, s0, w, c=c):
            P.cp("act", uq, uq[:, c, s0:s0 + w], pt[:, 0:w], reads=[pt])
        if c < 2:
            project(128, wq, lambda k, c=c: wq[:, k, c * 128:(c + 1) * 128], evq)
        else:
            project(128, wkv, lambda k: wkv[:, k, :], evq)
    for (s0, w) in tiles:
        pA = bank(); pB = bank()
        for k in range(8):
            P.mm(pA, pA[0:96, 0:w], wkp[:, 0, k, :], hT[:, k, s0:s0 + w], start=(k == 0), stop=(k == 7), reads=[wkp, hT])
        for k in range(8):
            P.mm(pB, pB[0:96, 0:w], wkp[:, 1, k, :], hT[:, k, s0:s0 + w], start=(k == 0), stop=(k == 7), reads=[wkp, hT])
        P.tt("dve", tA, tA[64:96, 0:w], pA[64:96, 0:w], cosT[64:96, s0:s0 + w], ALU.mult, reads=[pA, cosT])
        P.tt("dve", tB, tB[64:96, 0:w], pB[64:96, 0:w], sinT[64:96, s0:s0 + w], ALU.mult, reads=[pB, sinT])
        P.tt("pool", kpe, kpe[64:96, s0:s0 + w], tA[64:96, 0:w], tB[64:96, 0:w], ALU.add, reads=[tA, tB])
    sq = [P.sb([128, 2, 512], F32) for _ in range(2)]
    rs = [P.sb([128, 512], F32) for _ in range(2)]
    for ti, (s0, w) in enumerate(tiles):
        for grp, chunks, R in ((0, (0, 1), 256.0), (1, (2,), 128.0)):
            sqt = sq[(2 * ti + grp) % 2]; rst = rs[(2 * ti + grp) % 2]
            pt = bank()
            for i, c in enumerate(chunks):
                P.act(sqt, sqt[:, i, 0:w], uq[:, c, s0:s0 + w], AF.Square, reads=[uq])
            for i, c in enumerate(chunks):
                P.mm(pt, pt[:, 0:w], ones[:], sqt[:, i, 0:w], start=(i == 0), stop=(i == len(chunks) - 1), reads=[ones, sqt])
            P.act(rst, rst[:, 0:w], pt[:, 0:w], AF.Sqrt, bias=epsq[:, 0:1], scale=1.0 / R, reads=[pt])
            P.op("dve", lambda e, rst=rst, w=w: e.reciprocal(out=rst[:, 0:w], in_=rst[:, 0:w]), reads=[rst], writes=[rst])
            for c in chunks:
                P.stt("dve", qn, qn[:, c, s0:s0 + w], uq[:, c, s0:s0 + w], gq[:, c:c + 1], rst[:, 0:w], ALU.mult, ALU.mult,
                      reads=[uq, gq, rst])
    P.pop()
    wqb = P.sb([128, 2, 2, 8, 96], BF16)
    for j in range(2):
        P.load(wqb, wqb[:, j], d_wqb.ap()[j], q="pool")
    wkn = P.sb([128, 8, 64], BF16); P.load(wkn, wkn[:], d_wkn.ap(), q="pool")
    wv = P.sb([128, 512], BF16); P.load(wv, wv[:], d_wv.ap(), q="pool")
    qh = [P.sb([96, NT], BF16) for _ in range(2)]
    kh = [P.sb([96, NT], BF16) for _ in range(2)]
    for h in range(8):
        qt = qh[h % 2]; kt = kh[h % 2]
        for (s0, w) in tiles:
            pA = bank(); pB = bank(); pK = bank()
            for k in range(2):
                P.mm(pA, pA[0:96, 0:w], wqb[:, 0, k, h, :], qn[:, k, s0:s0 + w], start=(k == 0), stop=(k == 1), reads=[wqb, qn])
            for k in range(2):
                P.mm(pB, pB[0:96, 0:w], wqb[:, 1, k, h, :], qn[:, k, s0:s0 + w], start=(k == 0), stop=(k == 1), reads=[wqb, qn])
            P.mm(pK, pK[0:64, 0:w], wkn[:, h, :], qn[:, 2, s0:s0 + w], reads=[wkn, qn])
            P.tt("dve", tA, tA[:, 0:w], pA[0:96, 0:w], cosT[:, s0:s0 + w], ALU.mult, reads=[pA, cosT])
            P.tt("dve", tB, tB[:, 0:w], pB[0:96, 0:w], sinT[:, s0:s0 + w], ALU.mult, reads=[pB, sinT])
            P.tt("pool", qt, qt[:, s0:s0 + w], tA[:, 0:w], tB[:, 0:w], ALU.add, reads=[tA, tB])
            P.cp("act", kt, kt[0:64, s0:s0 + w], pK[0:64, 0:w], reads=[pK])
        P.cp("pool", kt, kt[64:96, :], kpe[64:96, :], reads=[kpe])
        for (a, b_, oo) in INT_SEGS:
            P.store(o_q.ap()[h, :, oo:oo + (b_ - a)], qt, qt[:, a:b_])
            P.store(o_k.ap()[h, :, oo:oo + (b_ - a)], kt, kt[:, a:b_])
    vb = [P.sb([128, 512], BF16) for _ in range(2)]
    for ti in range(NTI // 128):
        col = 1 + ti * 128 if ti == 0 else 131 + (ti - 1) * 128
        pt = bank(); v = vb[ti % 2]
        P.mm(pt, pt[:, :], qn[:, 2, col:col + 128], wv[:], reads=[qn, wv])
        P.cp("act", v, v[:], pt[:, :], reads=[pt])
        P.store(o_v.ap()[ti * 128:(ti + 1) * 128, :], v, v[:])
    P.pop()

    cosF, sinF = _rope_tables()
    pairswap = np.arange(32) ^ 1
    win_h = c_(np.stack([pk(w_in[:, oc * 128:(oc + 1) * 128]) for oc in range(12)]))
    wq_h = pk(w_in[:, 1536:1792]); wkv_h = pk(w_in[:, 1792:1920])
    kpA = np.zeros((D, 96), np.float32); kpB = np.zeros((D, 96), np.float32)
    kpA[:, 64:] = w_in[:, 1920:1952]; kpB[:, 64:] = w_in[:, 1920:1952][:, pairswap]
    wkp_h = c_(np.stack([pk(kpA), pk(kpB)]))
    cw_h = c_(np.concatenate([conv_w.T.reshape(12, 128, 3), conv_b.reshape(12, 128, 1)], -1).transpose(1, 0, 2))
    gq_h = c_(np.stack([q_norm[:128], q_norm[128:], kv_norm], 1))
    wqbA = w_qb.reshape(256, 8, 96)
    wqbB = wqbA.copy(); wqbB[:, :, 64:] = wqbA[:, :, 64:][:, :, pairswap]
    wqb_h = c_(np.stack([wqbA.reshape(2, 128, 8, 96).transpose(1, 0, 2, 3), wqbB.reshape(2, 128, 8, 96).transpose(1, 0, 2, 3)]))
    wkvr = w_kvb.reshape(128, 8, 128)
    wkn_h = c_(wkvr[:, :, :64]); wv_h = c_(wkvr[:, :, 64:].reshape(128, 512))
    maps = []
    for core in range(NCORES):
        b, s = core // 2, core % 2
        ci, li = _core_tokens(b, s)
        xc, okc = _gather_rows(ctx[b], ci); xl, okl = _gather_rows(x[b], li)
        xw = np.concatenate([xc, xl], 0)
        cosw = np.ones((96, NT), np.float32); sinw = np.zeros((96, NT), np.float32)
        lic = np.clip(li, 0, SEQ - 1)
        cosw[:, 130:] = cosF[:, lic]; sinw[:, 130:] = sinF[:, lic]
        hmv = np.array([okc[0], okc[-1], okl[0], okl[-1]], np.float32)
        modv = np.stack([mod0[4, 1], mod0[4, 0], mod0[b, 1], mod0[b, 0]], 1)
        maps.append({
            "xT": c_(xw.T.reshape(8, 128, NT).transpose(1, 0, 2)),
            "mod": c_(modv.reshape(8, 128, 4).transpose(1, 0, 2)),
            "win": win_h, "wq": wq_h, "wkv": wkv_h, "wkp": wkp_h, "cw": cw_h,
            "hm": c_(np.broadcast_to(hmv, (128, 4))), "cos": cosw, "sin": sinw, "gq": gq_h,
            "wqb": wqb_h, "wkn": wkn_h, "wv": wv_h,
        })
    res = run_spmd(P, maps)
    return res


_EPS_TILES = {}


def RMS_EPS_AP(P):
    key = id(P)
    if key not in _EPS_TILES:
        t = P.stk[0].enter_context(P.nc.sbuf_tensor("epsc", [128, 1], F32))
        tl = Tile(t, "epsc")
        P.memset("dve", tl, tl[:], RMS_EPS)
        _EPS_TILES[key] = tl
    return _EPS_TILES[key][:, 0:1]


NK = CTX + SEQ


def launch_attn(QT, KT, V):
    P = Prog()
    HG = 4
    NKT = NK // 128
    d_q = P.dram("q", [HG, 96, NK], BF16)
    d_k = P.dram("k", [HG, 96, NK], BF16)
    d_v = P.dram("v", [HG, 128, NKT, 65], BF16)
    d_sel = P.dram("sel", [65, 64], F32)
    o_a = P.dram("o_a", [HG, 64, NK], BF16, kind="ExternalOutput")
    ones = P.sb([128, 128], F32); P.memset("dve", ones, ones[:], 1.0)
    sel = P.sb([65, 64], F32); P.load(sel, sel[:], d_sel.ap())
    qs = [P.sb([96, NK], BF16) for _ in range(2)]
    ks = [P.sb([96, NK], BF16) for _ in range(2)]
    vs = [P.sb([128, NKT, 65], BF16) for _ in range(2)]
    ats = [P.sb([64, NK], BF16) for _ in range(2)]
    sqt = [P.sb([96, 512], F32) for _ in range(2)]
    mx = P.sb([128, 4], F32)
    negc = [P.sb([128, 1], F32) for _ in range(2)]
    pT = [P.sb([128, 512], BF16) for _ in range(4)]
    oT = [P.sb([65, 512], F32) for _ in range(2)]
    rec = [P.sb([64, 512], F32) for _ in range(2)]
    psS = [P.ps([128, 512], F32) for _ in range(4)]
    psO = [P.ps([128, 512], F32) for _ in range(2)]
    psD = [P.ps([128, 512], F32) for _ in range(2)]
    cS = [0]; cO = [0]
    qtiles = _ntiles(NK)
    for h in range(HG):
        q = qs[h % 2]; k = ks[h % 2]; v = vs[h % 2]; at = ats[h % 2]; nc_ = negc[h % 2]
        P.load(q, q[:], d_q.ap()[h]); P.load(k, k[:], d_k.ap()[h], q="act"); P.load(v, v[:], d_v.ap()[h])
        for which, src in ((0, q), (1, k)):
            for ti, (s0, w) in enumerate(qtiles):
                st = sqt[ti % 2]; pt = psD[ti % 2]
                P.act(st, st[:, 0:w], src[:, s0:s0 + w], AF.Square, reads=[src])
                P.mm(pt, pt[:, 0:w], ones[0:96, :], st[:, 0:w], reads=[ones, st])
                if ti == 0:
                    P.op("dve", lambda e, pt=pt, w=w, which=which: e.reduce_max(out=mx[:, which:which + 1], in_=pt[:, 0:w], axis=AX.X),
                         reads=[pt], writes=[mx])
                else:
                    P.op("dve", lambda e, pt=pt, w=w: e.reduce_max(out=mx[:, 2:3], in_=pt[:, 0:w], axis=AX.X),
                         reads=[pt], writes=[mx])
                    P.tt("dve", mx, mx[:, which:which + 1], mx[:, which:which + 1], mx[:, 2:3], ALU.max, reads=[mx])
        P.tt("dve", mx, mx[:, 3:4], mx[:, 0:1], mx[:, 1:2], ALU.mult, reads=[mx])
        P.act(mx, mx[:, 3:4], mx[:, 3:4], AF.Sqrt, scale=MLA_SCALE * MLA_SCALE, reads=[mx])
        P.ts("dve", nc_, nc_[:], mx[:, 3:4], -1.0, None, ALU.mult, reads=[mx])
        chunks = [(0, 256, 2)] + [(256 + 512 * i, 512, NKT) for i in range(SEQ // 512)]
        for (q0, qw, nkt) in chunks:
            po = psO[cO[0] % 2]; o = oT[cO[0] % 2]; rc = rec[cO[0] % 2]; pd = psD[cO[0] % 2]; cO[0] += 1
            pend = []

            def emit_s(kt):
                pS = psS[cS[0] % 4]; p_ = pT[cS[0] % 4]; cS[0] += 1
                P.mm(pS, pS[:, 0:qw], k[:, kt * 128:(kt + 1) * 128], q[:, q0:q0 + qw], reads=[k, q])
                P.act(p_, p_[:, 0:qw], pS[:, 0:qw], AF.Exp, bias=nc_[:, 0:1], scale=MLA_SCALE, reads=[pS, nc_])
                pend.append(p_)

            def emit_pv(kt):
                p_ = pend[kt]
                P.mm(po, po[0:65, 0:qw], v[:, kt, :], p_[:, 0:qw], start=(kt == 0), stop=(kt == nkt - 1), reads=[v, p_])

            for kt in range(nkt):
                emit_s(kt)
                if kt >= 2:
                    emit_pv(kt - 2)
            for kt in range(max(0, nkt - 2), nkt):
                emit_pv(kt)
            P.cp("dve", o, o[:, 0:qw], po[0:65, 0:qw], reads=[po])
            P.mm(pd, pd[0:64, 0:qw], sel[:], o[:, 0:qw], reads=[sel, o])
            P.op("dve", lambda e, rc=rc, pd=pd, qw=qw: e.reciprocal(out=rc[:, 0:qw], in_=pd[0:64, 0:qw]), reads=[pd], writes=[rc])
            P.tt("pool", at, at[:, q0:q0 + qw], o[0:64, 0:qw], rc[:, 0:qw], ALU.mult, reads=[o, rc])
        P.store(o_a.ap()[h], at, at[:])
    selh = np.zeros((65, 64), np.float32); selh[64] = 1.0
    maps = []
    for core in range(NCORES):
        b, g = core // 2, core % 2
        hs = slice(g * HG, (g + 1) * HG)
        vv = np.asarray(V[b][:, hs, :])
        va = np.ones((HG, 128, NKT, 65), NPBF)
        va[:, :, :, :64] = vv.reshape(NKT, 128, HG, 64).transpose(2, 1, 0, 3)
        maps.append({"q": np.ascontiguousarray(QT[b, hs]), "k": np.ascontiguousarray(KT[b, hs]), "v": va, "sel": selh})
    res = run_spmd(P, maps)
    att = np.zeros((BATCH, 8, 64, NK), NPBF)
    for core in range(NCORES):
        b, g = core // 2, core % 2
        att[b, g * HG:(g + 1) * HG] = res[core]["o_a"]
    return att


def launch_hyconv(Ls, zT_lat, zT_ctx, skip):
    P = Prog()
    CH = 64
    cfgs = [(SEQ, SEQ // 128), (CTX, CTX // 128)]
    d_L = [P.dram("L%d" % i, [CH, 2 * n], BF16) for i, (n, J) in enumerate(cfgs)]
    d_z = [P.dram("z%d" % i, [128, CH, 4, J], BF16) for i, (n, J) in enumerate(cfgs)]
    d_sk = P.dram("sk", [128, CH], F32)
    o_y = [P.dram("y%d" % i, [128, CH, 4, J], F32, kind="ExternalOutput") for i, (n, J) in enumerate(cfgs)]
    sk = P.sb([128, CH], F32); P.load(sk, sk[:], d_sk.ap())
    pbank = [P.ps([128, 8, 64], F32) for _ in range(4)]
    pc = [0]
    for i, (n, J) in enumerate(cfgs):
        P.push()
        W = 2 * n - 127
        zt = P.sb([128, CH, 4, J], BF16); P.load(zt, zt[:], d_z[i].ap())
        y = P.sb([128, CH, 4, J], F32)
        kss = [P.sb([128, W], BF16) for _ in range(2)]
        ms = [0] + [s * m for m in range(1, J) for s in (1, -1)]
        for c in range(CH):
            ks = kss[c % 2]
            src = bass.AP(tensor=d_L[i], offset=c * 2 * n, ap=[[1, 128], [1, W]])
            P.load(ks, ks[:], src, q=("sp" if c % 2 == 0 else "act"))
            pt = pbank[pc[0] % 4]; pc[0] += 1
            for idx, m in enumerate(ms):
                i0, i1 = max(0, m), min(J - 1, J - 1 + m)
                u0 = n + 128 * m - 127
                P.mm(pt, pt[:, 0:4, i0:i1 + 1], ks[:, u0:u0 + 128], zt[:, c, :, i0 - m:i1 + 1 - m],
                     start=(idx == 0), stop=(idx == len(ms) - 1), reads=[ks, zt])
            P.cp("dve" if c % 2 else "act", y, y[:, c], pt[:, 0:4, 0:J], reads=[pt])
        P.store(o_y[i].ap(), y, y[:])
        P.pop()
    maps = []
    zsrc = [zT_lat, zT_ctx]
    for core in range(NCORES):
        ch = slice(core * CH, (core + 1) * CH)
        m = {"sk": c_(np.broadcast_to(skip[ch], (128, CH)))}
        for i, (n, J) in enumerate(cfgs):
            m["L%d" % i] = np.ascontiguousarray(Ls[i][ch])
            z = np.asarray(zsrc[i][:, ch, :]).reshape(4, CH, J, 128)[:, :, :, ::-1]
            m["z%d" % i] = np.ascontiguousarray(z.transpose(3, 1, 0, 2))
        maps.append(m)
    res = run_spmd(P, maps)
    outs = []
    for i, (n, J) in enumerate(cfgs):
        yy = np.zeros((BATCH, HYW, n), np.float32)
        for core in range(NCORES):
            r = res[core]["y%d" % i]
            yy[:, core * CH:(core + 1) * CH, :] = r.transpose(2, 1, 3, 0).reshape(4, CH, n)
        outs.append(yy)
    return outs


def assemble_seq(res, key, feat_major=True):
    outs = []
    for b in range(BATCH):
        r0, r1 = res[2 * b][key], res[2 * b + 1][key]
        if feat_major:
            outs.append(np.concatenate([r0[..., :128], r1[..., :128], r0[..., 128:], r1[..., 128:]], -1))
        else:
            outs.append(np.concatenate([r0[:128], r1[:128], r0[128:], r1[128:]], 0))
    return np.stack(outs)


BIGNEG = 1.0e4


def launch_post(mode, ntile, feats, xres, vecs, lnvh, w_out, router_w, router_bias, w_gu_all, w_down_all, extra, nex=NE + 1):
    import os
    STG = int(os.environ.get('POST_STAGE', '9'))
    P = Prog()
    NTK = ntile * 128
    NEX = NE + 1
    NEXR = nex
    KC = 8 if mode == 0 else 16
    d_x = P.dram("xres", [NTK, D], F32)
    NS = 2 if mode == 0 else 1
    d_vec = P.dram("vecs", [128, 4, NS, D], F32)
    d_lnv = P.dram("lnv", [128, 4, D], F32)
    d_wo = P.dram("wo", [KC * 128, D], F32)
    d_rw = P.dram("rw", [D, NE], F32)
    d_rb = P.dram("rb", [128, NE], F32)
    d_wgu = P.dram("wgu", [NEXR, 128, 8, 512], BF16)
    d_wd = P.dram("wd", [NEXR, 128, 2, D], BF16)
    d_idf = P.dram("idf", [128, 128], F32)
    if mode == 0:
        d_y = P.dram("yT", [512, NTK], F32); d_z = P.dram("zT", [512, NTK], BF16)
        d_x0 = P.dram("x0T", [512, NTK], BF16); d_at = P.dram("atT", [512, NTK], BF16)
        d_sk = P.dram("sk", [128, 4], F32)
    else:
        d_yf = P.dram("yfT", [2048, NTK], BF16); d_yb = P.dram("ybT", [2048, NTK], BF16)
        d_xs = P.dram("xsT", [2048, NTK], BF16); d_zz = P.dram("zzT", [2048, NTK], BF16)
        d_dg = P.dram("dg", [128, 16, 2], F32)
    o_x1 = P.dram("o_x1", [NTK, D], F32, kind="ExternalOutput")
    o_out = P.dram("o_out", [NTK, D], F32, kind="ExternalOutput")
    x1_tiles = [Tile(None, "x1d%d" % i) for i in range(ntile)]

    vec = P.sb([128, 4, NS, D], F32); lnv = P.sb([128, 4, D], F32)
    P.load(vec, vec[:], d_vec.ap()); P.load(lnv, lnv[:], d_lnv.ap())
    P.ts("dve", vec, vec[:, 1], vec[:, 1], 1.0, None, ALU.add, reads=[vec])
    idf = P.sb([128, 128], F32); P.load(idf, idf[:], d_idf.ap())
    idb = P.sb([128, 128], BF16); P.cp("dve", idb, idb[:], idf[:], reads=[idf])
    epsl = P.sb([128, 1], F32); P.memset("dve", epsl, epsl[:], LN_EPS)
    epsr = P.sb([128, 1], F32); P.memset("dve", epsr, epsr[:], RMS_EPS)
    ones = P.sb([128, 128], F32); P.memset("dve", ones, ones[:], 1.0)
    TP = 9 if mode == 0 else 8
    acc = P.sb([128, TP, D], F32)
    ffT = P.sb([128, 8, TP * 128], BF16)
    gates = P.sb([128, TP, NEX], F32)
    P.memset("dve", gates, gates[:, :, NE:NEX], 1.0)
    st = P.sb([128, 8], F32)
    pA = [P.ps([128, 512], F32) for _ in range(2)]
    pT = [P.ps([128, 2, 128], F32) for _ in range(2)]
    pD = [P.ps([128, 1024], F32) for _ in range(2)]

    def layer_norm(t, tt_, gi, bi, out_t, out_ap, sq):
        P.op("dve", lambda e: e.reduce_sum(out=st[:, 0:1], in_=tt_, axis=AX.X), reads=[t], writes=[st])
        P.act(sq, sq[:], tt_, AF.Square, reads=[t])
        P.op("dve", lambda e: e.reduce_sum(out=st[:, 1:2], in_=sq[:], axis=AX.X), reads=[sq], writes=[st])
        P.ts("dve", st, st[:, 2:3], st[:, 0:1], 1.0 / D, None, ALU.mult, reads=[st])
        P.tt("dve", st, st[:, 3:4], st[:, 2:3], st[:, 2:3], ALU.mult, reads=[st])
        P.stt("dve", st, st[:, 4:5], st[:, 1:2], 1.0 / D, st[:, 3:4], ALU.mult, ALU.subtract, reads=[st])
        P.act(st, st[:, 5:6], st[:, 4:5], AF.Sqrt, bias=epsl[:, 0:1], scale=1.0, reads=[st, epsl])
        P.op("dve", lambda e: e.reciprocal(out=st[:, 6:7], in_=st[:, 5:6]), reads=[st], writes=[st])
        P.ts("dve", t, tt_, tt_, st[:, 2:3], st[:, 6:7], ALU.subtract, ALU.mult, reads=[t, st])
        P.tt("dve", t, tt_, tt_, lnv[:, gi, :], ALU.mult, reads=[t, lnv])
        P.tt("dve", out_t, out_ap, tt_, lnv[:, bi, :], ALU.add, reads=[t, lnv])

    passes = [list(range(s, min(s + TP, ntile))) for s in range(0, ntile, TP)]
    for tiles in passes:
        P.push()
        wo = P.sb([128, KC, D], BF16)
        P.load(wo, wo[:], d_wo.ap().rearrange("(k p) n -> p k n", p=128), q="pool")
        rw = P.sb([128, 8, NE], F32); P.load(rw, rw[:], d_rw.ap().rearrange("(k p) n -> p k n", p=128))
        rb = P.sb([128, NE], F32); P.load(rb, rb[:], d_rb.ap())
        mT = [P.sb([128, KC, 128], BF16) for _ in range(1)] * 2
        xr = [P.sb([128, D], F32)] * 2
        tb = [P.sb([128, D], F32)] * 2
        sq = P.sb([128, D], F32)
        x1b = [P.sb([128, D], F32)] * 2
        ffb = [P.sb([128, D], F32)] * 2
        fTf = [P.sb([128, 8, 128], F32)] * 2
        scr = P.sb([128, NE], F32); cho = P.sb([128, NE], F32); mc = P.sb([128, NE], F32)
        m8 = P.sb([128, 8, 8], F32); gs = P.sb([128, 8], F32); gm = P.sb([128, 16], F32); t8 = P.sb([128, 8], F32)
        if mode == 0:
            sk = P.sb([128, 4], F32); P.load(sk, sk[:], d_sk.ap())
            fin = [[P.sb([128, 4, 128], F32), P.sb([128, 4, 128], BF16), P.sb([128, 4, 128], BF16)] for _ in range(2)]
            ftmp = P.sb([128, 4, 128], F32)
        else:
            dg = P.sb([128, 16, 2], F32); P.load(dg, dg[:], d_dg.ap())
            fin = [[P.sb([128, 16, 128], BF16), P.sb([128, 16, 128], BF16), P.sb([128, 16, 128], BF16), P.sb([128, 16, 128], BF16)]] * 2
            ftmp = P.sb([128, 16, 128], F32); fsq = P.sb([128, 16, 128], F32); frs = P.sb([128, 4, 128], F32)
        for li, ti in enumerate(tiles):
            c0 = ti * 128
            vs = 0 if (mode == 0 and ti == 0) else NS - 1
            m = mT[li % 2]; x_ = xr[li % 2]; t = tb[li % 2]; x1 = x1b[li % 2]; ff = ffb[li % 2]; ftf = fTf[li % 2]
            f = fin[li % 2]
            P.load(x_, x_[:], d_x.ap()[c0:c0 + 128, :])
            if mode == 0:
                P.load(f[0], f[0][:], d_y.ap()[:, c0:c0 + 128].rearrange("(k p) n -> p k n", p=128))
                P.load(f[1], f[1][:], d_z.ap()[:, c0:c0 + 128].rearrange("(k p) n -> p k n", p=128), q="act")
                P.load(f[2], f[2][:], d_x0.ap()[:, c0:c0 + 128].rearrange("(k p) n -> p k n", p=128), q="act")
                P.load(m, m[:, 4:8, :], d_at.ap()[:, c0:c0 + 128].rearrange("(k p) n -> p k n", p=128))
                for k in range(4):
                    P.stt("dve", ftmp, ftmp[:, k], f[1][:, k], sk[:, k:k + 1], f[0][:, k], ALU.mult, ALU.add, reads=[f[1], sk, f[0]])
                P.tt("pool", m, m[:, 0:4, :], ftmp[:], f[2][:], ALU.mult, reads=[ftmp, f[2]])
            else:
                P.load(f[0], f[0][:], d_yf.ap()[:, c0:c0 + 128].rearrange("(k p) n -> p k n", p=128))
                P.load(f[1], f[1][:], d_yb.ap()[:, c0:c0 + 128].rearrange("(k p) n -> p k n", p=128), q="act")
                P.load(f[2], f[2][:], d_xs.ap()[:, c0:c0 + 128].rearrange("(k p) n -> p k n", p=128))
                P.load(f[3], f[3][:], d_zz.ap()[:, c0:c0 + 128].rearrange("(k p) n -> p k n", p=128), q="act")
                P.tt("pool", ftmp, ftmp[:], f[0][:], f[1][:], ALU.add, reads=[f[0], f[1]])
                for k in range(16):
                    P.stt("dve", ftmp, ftmp[:, k], f[2][:, k], dg[:, k, 0:1], ftmp[:, k], ALU.mult, ALU.add, reads=[f[2], dg, ftmp])
                P.act(fsq, fsq[:], f[3][:], AF.Silu, reads=[f[3]])
                P.tt("pool", ftmp, ftmp[:], ftmp[:], fsq[:], ALU.mult, reads=[ftmp, fsq])
                P.act(fsq, fsq[:], ftmp[:], AF.Square, reads=[ftmp])
                for g in range(4):
                    pq = pA[g % 2]
                    for k in range(4):
                        P.mm(pq, pq[:, 0:128], ones[:], fsq[:, 4 * g + k, :], start=(k == 0), stop=(k == 3), reads=[ones, fsq])
                    P.act(frs, frs[:, g, :], pq[:, 0:128], AF.Sqrt, bias=epsr[:, 0:1], scale=1.0 / 512, reads=[pq, epsr])
                P.op("dve", lambda e, frs=frs: e.reciprocal(out=frs[:], in_=frs[:]), reads=[frs], writes=[frs])
                for k in range(16):
                    P.stt("dve", m, m[:, k, :], ftmp[:, k, :], dg[:, k, 1:2], frs[:, k // 4, :], ALU.mult, ALU.mult, reads=[ftmp, dg, frs])
            pd = pD[li % 2]
            for hf in range(2):
                for k in range(KC):
                    P.mm(pd, pd[:, hf * 512:(hf + 1) * 512], m[:, k, :], wo[:, k, hf * 512:(hf + 1) * 512],
                         start=(k == 0), stop=(k == KC - 1), reads=[m, wo])
            P.tt("dve", t, t[:], pd[:], vec[:, 0, vs, :], ALU.mult, reads=[pd, vec])
            P.stt("dve", t, t[:], x_[:], DN_ALPHA, t[:], ALU.mult, ALU.add, reads=[x_, t])
            layer_norm(t, t[:], 0, 1, x1, x1[:], sq)
            P.dma("sp", lambda e, x1=x1, c0=c0: e.dma_start(out=o_x1.ap()[c0:c0 + 128, :], in_=x1[:]), reads=[x1], writes=[x1_tiles[ti]])
            P.tt("dve", ff, ff[:], x1[:], vec[:, 1, vs, :], ALU.mult, reads=[x1, vec])
            P.tt("dve", ff, ff[:], ff[:], vec[:, 2, vs, :], ALU.add, reads=[ff, vec])
            if STG < 2:
                continue
            for k in range(8):
                pq = pA[k % 2]
                P.mm(pq, pq[:, 0:128], ff[:, k * 128:(k + 1) * 128], idf[:], reads=[ff, idf])
                P.cp("act", ftf, ftf[:, k, :], pq[:, 0:128], reads=[pq])
                P.cp("dve", ffT, ffT[:, k, li * 128:(li + 1) * 128], ftf[:, k, :], reads=[ftf])
            pq = pA[li % 2]
            for k in range(8):
                P.mm(pq, pq[:, 0:NE], ftf[:, k, :], rw[:, k, :], start=(k == 0), stop=(k == 7), reads=[ftf, rw])
            P.act(scr, scr[:], pq[:, 0:NE], AF.Sigmoid, reads=[pq])
            P.tt("dve", cho, cho[:], scr[:], rb[:], ALU.add, reads=[scr, rb])
            if STG < 3:
                continue
            for g in range(8):
                P.op("dve", lambda e, g=g: e.max(out=m8[:, g, :], in_=cho[:, 32 * g:32 * g + 32]), reads=[cho], writes=[m8])
            P.tt("dve", gs, gs[:], m8[:, :, 0], m8[:, :, 1], ALU.add, reads=[m8])
            P.op("dve", lambda e: e.max(out=t8[:], in_=gs[:]), reads=[gs], writes=[t8])
            P.ts("dve", gm, gm[:, 0:8], gs[:], t8[:, 3:4], None, ALU.is_ge, reads=[gs, t8])
            P.ts("dve", gm, gm[:, 8:16], gm[:, 0:8], -1.0, BIGNEG, ALU.add, ALU.mult, reads=[gm])
            for g in range(8):
                P.ts("dve", mc, mc[:, 32 * g:32 * g + 32], cho[:, 32 * g:32 * g + 32], gm[:, g:g + 1], gm[:, 8 + g:9 + g],
                     ALU.mult, ALU.add, reads=[cho, gm])
            P.op("dve", lambda e: e.max(out=t8[:], in_=mc[:]), reads=[mc], writes=[t8])
            P.ts("dve", mc, mc[:], mc[:], t8[:, 7:8], None, ALU.is_ge, reads=[mc, t8])
            P.tt("dve", scr, scr[:], scr[:], mc[:], ALU.mult, reads=[scr, mc])
            P.op("dve", lambda e: e.reduce_sum(out=gs[:, 0:1], in_=scr[:], axis=AX.X), reads=[scr], writes=[gs])
            P.op("dve", lambda e: e.reciprocal(out=gs[:, 1:2], in_=gs[:, 0:1]), reads=[gs], writes=[gs])
            P.ts("dve", gates, gates[:, li, 0:NE], scr[:], gs[:, 1:2], ROUTED_SCALE, ALU.mult, ALU.mult, reads=[scr, gs])
        P.pop()
        P.push()
        wgs = [P.sb([128, 8, 512], BF16) for _ in range(2)]
        wds = [P.sb([128, 2, D], BF16) for _ in range(2)]
        sgs = [P.sb([128, 256], F32) for _ in range(2)]
        hs = [P.sb([128, 256], BF16) for _ in range(2)]
        hTs = [P.sb([128, 2, 128], BF16) for _ in range(3)]
        items = [(e_, li) for e_ in range(NEXR if STG >= 4 else 0) for li in range(len(tiles))]
        wcur = {}

        def stage_a(idx):
            e_, li = items[idx]
            wg = wgs[e_ % 2]; wd = wds[e_ % 2]
            if li == 0:
                P.load(wg, wg[:], d_wgu.ap()[e_], q="sp")
                P.load(wd, wd[:], d_wd.ap()[e_], q="sp")
            pa = pA[idx % 2]; sg = sgs[idx % 2]; h = hs[idx % 2]
            for k in range(8):
                P.mm(pa, pa[:, :], ffT[:, k, li * 128:(li + 1) * 128], wg[:, k, :], start=(k == 0), stop=(k == 7), reads=[ffT, wg])
            P.act(sg, sg[:], pa[:, 0:256], AF.Silu, reads=[pa])
            P.stt("dve", h, h[:], pa[:, 256:512], gates[:, li, e_:e_ + 1], sg[:], ALU.mult, ALU.mult, reads=[pa, gates, sg])

        def stage_b(idx):
            pt = pT[idx % 2]; h = hs[idx % 2]; hT = hTs[idx % 3]
            for k in range(2):
                P.mm(pt, pt[:, k, :], h[:, k * 128:(k + 1) * 128], idb[:], reads=[h, idb])
            P.cp("act", hT, hT[:], pt[:], reads=[pt])

        def stage_c(idx):
            e_, li = items[idx]
            wd = wds[e_ % 2]
            pd = pD[idx % 2]; hT = hTs[idx % 3]
            for hf in range(2):
                for k in range(2):
                    P.mm(pd, pd[:, hf * 512:(hf + 1) * 512], hT[:, k, :], wd[:, k, hf * 512:(hf + 1) * 512],
                         start=(k == 0), stop=(k == 1), reads=[hT, wd])
            if e_ == 0:
                P.cp("dve", acc, acc[:, li, :], pd[:], reads=[pd])
            else:
                P.tt("dve", acc, acc[:, li, :], acc[:, li, :], pd[:], ALU.add, reads=[acc, pd])

        if items:
            stage_a(0)
        for idx in range(len(items)):
            if idx + 1 < len(items):
                stage_a(idx + 1)
            stage_b(idx)
            if idx >= 1:
                stage_c(idx - 1)
        if items:
            stage_c(len(items) - 1)
        P.pop()
        P.push()
        x1r = [P.sb([128, D], F32) for _ in range(2)]
        ob = [P.sb([128, D], F32) for _ in range(2)]
        sq = P.sb([128, D], F32)
        for li, ti in enumerate(tiles if STG >= 5 else []):
            c0 = ti * 128
            vs = 0 if (mode == 0 and ti == 0) else NS - 1
            x1 = x1r[li % 2]; o = ob[li % 2]
            P.dma("sp", lambda e, x1=x1, c0=c0: e.dma_start(out=x1[:], in_=o_x1.ap()[c0:c0 + 128, :]), reads=[x1_tiles[ti]], writes=[x1])
            P.tt("dve", acc, acc[:, li, :], acc[:, li, :], vec[:, 3, vs, :], ALU.mult, reads=[acc, vec])
            P.stt("dve", acc, acc[:, li, :], x1[:], DN_ALPHA, acc[:, li, :], ALU.mult, ALU.add, reads=[x1, acc])
            layer_norm(acc, acc[:, li, :], 2, 3, o, o[:], sq)
            P.store(o_out.ap()[c0:c0 + 128, :], o, o[:])
        P.pop()

    idh = np.eye(128, dtype=np.float32)
    maps = []
    for core in range(NCORES):
        m = {"xres": c_(xres[core]), "vecs": c_(vecs[core]), "lnv": c_(lnv_h(lnvh)), "wo": c_(w_out), "rw": c_(router_w),
             "rb": c_(np.broadcast_to(router_bias, (128, NE))), "wgu": w_gu_all, "wd": w_down_all, "idf": idh}
        m.update(feats[core])
        m.update(extra)
        maps.append(m)
    res = run_spmd(P, maps)
    if os.environ.get('POST_X1'):
        return [r["o_x1"] for r in res]
    return [r["o_out"] for r in res]


def lnv_h(v):
    return np.broadcast_to(np.stack(v)[None], (128, 4, D))


def _vecs(modl, b):
    v = np.stack([np.stack([modl[4, j], modl[b, j]]) for j in (2, 4, 3, 5)])
    return np.broadcast_to(v[None], (128, 4, 2, D))


def run_layer0(inp, mod, Ls, wb):
    x, ctx = inp["x"], inp["ctx"]
    res = launch_pre0(x, ctx, mod[0], inp["a_w_in"][0], inp["hy_conv_w"][0], inp["hy_conv_b"][0],
                      inp["mla_q_norm"][0], inp["mla_w_qb"][0], inp["mla_kv_norm"][0], inp["mla_w_kvb"][0])
    QT = assemble_seq(res, "o_q"); KT = assemble_seq(res, "o_k")
    V = assemble_seq(res, "o_v", feat_major=False).reshape(BATCH, NK, 8, 64)
    att = launch_attn(QT, KT, V)
    zT = assemble_seq(res, "o_z")
    ylat, yctx = launch_hyconv(Ls, zT[:, :, CTX:], zT[:, :, :CTX], inp["hy_skip"][0])
    feats, xres, vecs = [], [], []
    for core in range(NCORES):
        b, s = core // 2, core % 2
        cs = slice(s * 128, (s + 1) * 128); ls = slice(s * 4096, (s + 1) * 4096)
        attb = att[b].reshape(512, NK)
        feats.append({
            "yT": c_(np.concatenate([yctx[b][:, cs], ylat[b][:, ls]], 1)),
            "zT": res[core]["o_z"], "x0T": res[core]["o_x0"],
            "atT": np.ascontiguousarray(np.concatenate([attb[:, cs], attb[:, CTX + s * 4096:CTX + (s + 1) * 4096]], 1)),
        })
        xres.append(np.concatenate([ctx[b][cs], x[b][ls]], 0))
        vecs.append(_vecs(mod[0], b))
    l = 0
    wgu, wd = wb[l]
    extra = {"sk": c_(inp["hy_skip"][0].reshape(4, 128).T)}
    outs = launch_post(0, 33, feats, xres, vecs, [inp["ln_mix_g"][l], inp["ln_mix_b"][l], inp["ln_ffn_g"][l], inp["ln_ffn_b"][l]],
                       inp["a_w_out"][0], inp["router_w"][l], inp["router_bias"][l], wgu, wd, extra)
    x2 = np.zeros_like(x); c2 = np.zeros_like(ctx)
    for core in range(NCORES):
        b, s = core // 2, core % 2
        c2[b, s * 128:(s + 1) * 128] = outs[core][:128]
        x2[b, s * 4096:(s + 1) * 4096] = outs[core][128:]
    return x2, c2


def launch_pre1(x, ctx, mod1, w_in, conv_w, conv_b, dt_bias_f, dt_bias_b):
    P = Prog()
    NT = NT0
    d_xT = P.dram("xT", [128, 8, NT], F32)
    d_mod = P.dram("mod", [128, 8, 4], F32)
    d_win = P.dram("win", [40, 128, 8, 128], F32)
    d_wdt = P.dram("wdt", [128, 8, 64], F32)
    d_cw = P.dram("cw", [128, 24, 4], F32)
    d_hm = P.dram("hm", [128, 4], F32)
    d_db = P.dram("db", [64, 1], F32)
    o_z = P.dram("o_z", [2048, NTI], BF16, kind="ExternalOutput")
    o_xbc = P.dram("o_xbc", [3072, NTI], BF16, kind="ExternalOutput")
    o_dt = P.dram("o_dt", [64, NTI], F32, kind="ExternalOutput")
    tiles = _ntiles(NT)
    pbank = [P.ps([128, 512], F32) for _ in range(8)]
    pb_i = [0]

    def bank():
        pb_i[0] = (pb_i[0] + 1) % 8
        return pbank[pb_i[0]]

    hT = P.sb([128, 8, NT], BF16, "hT")
    mods = P.sb([128, 8, 4], F32); P.load(mods, mods[:], d_mod.ap())
    m1 = P.sb([128, 8, 2], F32)
    for j in range(2):
        P.ts("dve", m1, m1[:, :, j:j + 1], mods[:, :, 2 * j:2 * j + 1], 1.0, None, ALU.add, reads=[mods])
    P.push()
    xin = [P.sb([128, 8, 512], F32) for _ in range(2)]
    for ti, (s0, w) in enumerate(tiles):
        xt = xin[ti % 2]
        P.load(xt, xt[:, :, 0:w], d_xT.ap()[:, :, s0:s0 + w])
        for k in range(8):
            for (a, b_, j) in ((s0, min(s0 + w, 130), 0), (max(s0, 130), s0 + w, 1)):
                if b_ <= a:
                    continue
                P.ts("dve" if k % 2 == 0 else "pool", hT, hT[:, k, a:b_], xt[:, k, a - s0:b_ - s0],
                     m1[:, k, j:j + 1], mods[:, k, 2 * j + 1:2 * j + 2], ALU.mult, ALU.add, reads=[xt, m1, mods])
    P.pop()
    cw = P.sb([128, 24, 4], F32); hm = P.sb([128, 4], F32); db = P.sb([64, 1], F32)
    P.load(cw, cw[:], d_cw.ap()); P.load(hm, hm[:], d_hm.ap()); P.load(db, db[:], d_db.ap())
    wbuf = [P.sb([128, 8, 128], BF16) for _ in range(2)]
    urow = [P.sb([128, NT], F32) for _ in range(2)]
    cv = P.sb([128, NT], F32)
    ob = [P.sb([128, NT], BF16) for _ in range(2)]
    for oc in range(40):
        wt = wbuf[oc % 2]; ur = urow[oc % 2]; o = ob[oc % 2]
        P.load(wt, wt[:], d_win.ap()[oc], q="pool")
        cnt = [0]
        for (s0, w) in tiles:
            pt = bank()
            for k in range(8):
                P.mm(pt, pt[:, 0:w], wt[:, k, :], hT[:, k, s0:s0 + w], start=(k == 0), stop=(k == 7), reads=[wt, hT])
            cnt[0] += 1
            P.cp("act" if cnt[0] % 2 else "dve", ur, ur[:, s0:s0 + w], pt[:, 0:w], reads=[pt])
        if oc < 16:
            P.cp("pool", o, o[:], ur[:], reads=[ur])
            dst, r0 = o_z, oc * 128
        else:
            c = oc - 16
            for j, col in enumerate((0, 129, 130, 4227)):
                P.tt("dve", ur, ur[:, col:col + 1], ur[:, col:col + 1], hm[:, j:j + 1], ALU.mult, reads=[ur, hm])
            P.act(cv, cv[:], ur[:], AF.Identity, bias=cw[:, c, 3:4], scale=cw[:, c, 1:2], reads=[ur, cw])
            P.stt("dve", cv, cv[:, 1:NT], ur[:, 0:NT - 1], cw[:, c, 0:1], cv[:, 1:NT], ALU.mult, ALU.add, reads=[ur, cw, cv])
            P.stt("dve", cv, cv[:, 0:NT - 1], ur[:, 1:NT], cw[:, c, 2:3], cv[:, 0:NT - 1], ALU.mult, ALU.add, reads=[ur, cw, cv])
            P.act(o, o[:], cv[:], AF.Silu, reads=[cv])
            dst, r0 = o_xbc, c * 128
        for (a, b_, oo) in INT_SEGS:
            P.store(dst.ap()[r0:r0 + 128, oo:oo + (b_ - a)], o, o[:, a:b_])
    wdt = P.sb([128, 8, 64], BF16); P.load(wdt, wdt[:], d_wdt.ap(), q="pool")
    xd = urow[0]; ab = urow[1]; sp = cv
    for (s0, w) in tiles:
        pt = bank()
        for k in range(8):
            P.mm(pt, pt[0:64, 0:w], wdt[:, k, :], hT[:, k, s0:s0 + w], start=(k == 0), stop=(k == 7), reads=[wdt, hT])
        P.ts("dve", xd, xd[0:64, s0:s0 + w], pt[0:64, 0:w], db[:, 0:1], None, ALU.add, reads=[pt, db])
    P.act(ab, ab[0:64, :], xd[0:64, :], AF.Abs, reads=[xd])
    P.act(ab, ab[0:64, :], ab[0:64, :], AF.Exp, scale=-1.0, reads=[ab])
    P.act(ab, ab[0:64, :], ab[0:64, :], AF.Ln, bias=1.0, reads=[ab])
    P.ts("dve", sp, sp[0:64, :], xd[0:64, :], 0.0, None, ALU.max, reads=[xd])
    P.tt("dve", sp, sp[0:64, :], sp[0:64, :], ab[0:64, :], ALU.add, reads=[sp, ab])
    for (a, b_, oo) in INT_SEGS:
        P.store(o_dt.ap()[:, oo:oo + (b_ - a)], sp, sp[0:64, a:b_])

    win_h = c_(np.stack([pk(w_in[:, oc * 128:(oc + 1) * 128]) for oc in range(40)]))
    wdt_h = pk(w_in[:, 5120:5184])
    cw_h = c_(np.concatenate([conv_w.T.reshape(24, 128, 3), conv_b.reshape(24, 128, 1)], -1).transpose(1, 0, 2))
    db_h = c_(np.concatenate([dt_bias_f, dt_bias_b])[:, None])
    maps = []
    for core in range(NCORES):
        b, s = core // 2, core % 2
        ci, li = _core_tokens(b, s)
        xc, okc = _gather_rows(ctx[b], ci); xl, okl = _gather_rows(x[b], li)
        xw = np.concatenate([xc, xl], 0)
        hmv = np.array([okc[0], okc[-1], okl[0], okl[-1]], np.float32)
        modv = np.stack([mod1[4, 1], mod1[4, 0], mod1[b, 1], mod1[b, 0]], 1)
        maps.append({"xT": c_(xw.T.reshape(8, 128, NT).transpose(1, 0, 2)), "mod": c_(modv.reshape(8, 128, 4).transpose(1, 0, 2)),
                     "win": win_h, "wdt": wdt_h, "cw": cw_h, "hm": c_(np.broadcast_to(hmv, (128, 4))), "db": db_h})
    return run_spmd(P, maps)


NCH = NK // 128


def launch_scan(xs_tm, dt_tm, B_tm, BT, CT, alog):
    P = Prog()
    d_x = P.dram("x", [NK, 2048], BF16)
    d_dt = P.dram("dt", [128, NCH, 32], F32)
    d_B = P.dram("B", [NK, 512], BF16)
    d_BT = P.dram("BT", [512, NK], BF16)
    d_CT = P.dram("CT", [512, NK], BF16)
    d_al = P.dram("al", [128, 32], F32)
    d_tri = P.dram("tri", [128, 128], F32)
    d_sel = P.dram("sel", [32, 32, 128], F32)
    o_y = P.dram("o_y", [SEQ, 2048], BF16, kind="ExternalOutput")
    dt = P.sb([128, NCH, 32], F32); P.load(dt, dt[:], d_dt.ap())
    A = P.sb([128, 32], F32); P.load(A, A[:], d_al.ap())
    P.act(A, A[:], A[:], AF.Exp, reads=[A])
    P.ts("dve", A, A[:], A[:], -1.0, None, ALU.mult, reads=[A])
    tri = P.sb([128, 128], F32); P.load(tri, tri[:], d_tri.ap())
    sel = P.sb([32, 32, 128], F32); P.load(sel, sel[:], d_sel.ap())
    ones = P.sb([128, 128], F32); P.memset("dve", ones, ones[:], 1.0)
    S32 = P.sb([128, 2048], F32); P.memset("dve", S32, S32[:], 0.0)
    Sb = P.sb([128, 2048], BF16); P.memset("pool", Sb, Sb[:], 0.0)
    xb = [P.sb([128, 2048], BF16) for _ in range(2)]
    Bb = [P.sb([128, 512], BF16) for _ in range(2)]
    BTb = [P.sb([128, 4, 128], BF16) for _ in range(2)]
    CTb = [P.sb([128, 4, 128], BF16) for _ in range(2)]
    a_ = P.sb([128, 32], F32); acs = P.sb([128, 32], F32); acsT = P.sb([32, 128], F32); sm = P.sb([128, 256], F32)
    eacs = P.sb([128, 32], F32); toend = P.sb([128, 32], F32); etot = P.sb([128, 32], F32)
    xdt = P.sb([128, 2048], BF16); xw = P.sb([128, 2048], BF16)
    cbm = P.sb([128, 4, 128], F32)
    dd = [P.sb([128, 4, 128], F32) for _ in range(2)]
    MT = [P.sb([128, 4, 128], BF16) for _ in range(2)]
    yoff = P.sb([128, 2048], F32)
    yb = [P.sb([128, 2048], BF16) for _ in range(2)]
    pS = P.ps([128, 512], F32)
    pC = P.ps([128, 4, 128], F32)
    pAc = [P.ps([128, 4, 128], F32) for _ in range(2)]
    pY = [P.ps([128, 512], F32) for _ in range(2)]
    pG = [P.ps([128, 512], F32) for _ in range(2)]
    for c in range(NCH):
        x_ = xb[c % 2]; B_ = Bb[c % 2]; BT_ = BTb[c % 2]; CT_ = CTb[c % 2]; y_ = yb[c % 2]
        cs = slice(c * 128, (c + 1) * 128)
        P.load(x_, x_[:], d_x.ap()[cs, :])
        P.load(B_, B_[:], d_B.ap()[cs, :], q="act")
        P.load(BT_, BT_[:], d_BT.ap()[:, cs].rearrange("(g p) n -> p g n", p=128))
        P.load(CT_, CT_[:], d_CT.ap()[:, cs].rearrange("(g p) n -> p g n", p=128), q="act")
        P.tt("dve", a_, a_[:], dt[:, c, :], A[:], ALU.mult, reads=[dt, A])
        if c == 0:
            P.op("dve", lambda e: e.memset(sm[:], 0.0), writes=[sm])
        P.mm(pS, pS[:, 0:32], tri[:], a_[:], reads=[tri, a_])
        P.mm(pS, pS[:, 32:64], ones[:], a_[:], reads=[ones, a_])
        P.mm(pS, pS[0:32, 128:256], a_[:], tri[:], reads=[a_, tri])
        P.cp("dve", sm, sm[:], pS[:, 0:256], reads=[pS])
        P.cp("pool", acs, acs[:], sm[:, 0:32], reads=[sm])
        P.cp("pool", acsT, acsT[:], sm[0:32, 128:256], reads=[sm])
        P.act(eacs, eacs[:], sm[:, 0:32], AF.Exp, reads=[sm])
        P.act(etot, etot[:], sm[:, 32:64], AF.Exp, reads=[sm])
        P.tt("dve", toend, toend[:], sm[:, 32:64], sm[:, 0:32], ALU.subtract, reads=[sm])
        P.act(toend, toend[:], toend[:], AF.Exp, reads=[toend])
        P.tt("dve", xdt, xdt[:].rearrange("p (h d) -> p h d", d=64), x_[:].rearrange("p (h d) -> p h d", d=64),
             dt[:, c, :].unsqueeze(2).to_broadcast([128, 32, 64]), ALU.mult, reads=[x_, dt])
        P.tt("pool", xw, xw[:].rearrange("p (h d) -> p h d", d=64), xdt[:].rearrange("p (h d) -> p h d", d=64),
             toend[:].unsqueeze(2).to_broadcast([128, 32, 64]), ALU.mult, reads=[xdt, toend])
        for g in range(4):
            P.mm(pC, pC[:, g, :], BT_[:, g, :], CT_[:, g, :], reads=[BT_, CT_])
        P.tt("dve", cbm, cbm[:], pC[:], tri[:].unsqueeze(1).to_broadcast([128, 4, 128]), ALU.mult, reads=[pC, tri])
        for g in range(4):
            py = pY[g % 2]
            for q4 in range(2):
                h0 = g * 8 + q4 * 4
                pa = pAc[q4]; d_ = dd[q4]; mt = MT[q4]
                for j in range(4):
                    P.mm(pa, pa[:, j, :], sel[:, h0 + j, :], acsT[:], reads=[sel, acsT])
                P.tt("dve", d_, d_[:], pa[:], acs[:, h0:h0 + 4].unsqueeze(2).to_broadcast([128, 4, 128]), ALU.subtract, reads=[pa, acs])
                P.act(d_, d_[:], d_[:], AF.Exp, reads=[d_])
                P.stt("dve", mt, mt[:], d_[:], 1.0, cbm[:, g, :].unsqueeze(1).to_broadcast([128, 4, 128]), ALU.min, ALU.mult, reads=[d_, cbm])
                for j in range(4):
                    h = h0 + j
                    P.mm(py, py[:, (h % 8) * 64:(h % 8 + 1) * 64], mt[:, j, :], xdt[:, h * 64:(h + 1) * 64], reads=[mt, xdt])
            if c >= 2:
                po = pG[g % 2]
                P.mm(po, po[:], CT_[:, g, :], Sb[:, g * 512:(g + 1) * 512], reads=[CT_, Sb])
                P.tt("dve", yoff, yoff[:, g * 512:(g + 1) * 512].rearrange("p (h d) -> p h d", d=64),
                     po[:].rearrange("p (h d) -> p h d", d=64),
                     eacs[:, g * 8:(g + 1) * 8].unsqueeze(2).to_broadcast([128, 8, 64]), ALU.mult, reads=[po, eacs])
                P.tt("dve", y_, y_[:, g * 512:(g + 1) * 512], py[:], yoff[:, g * 512:(g + 1) * 512], ALU.add, reads=[py, yoff])
        if c >= 2:
            P.store(o_y.ap()[(c - 2) * 128:(c - 1) * 128, :], y_, y_[:])
        if c == 0:
            pass
        P.tt("pool", S32, S32[:].rearrange("p (h d) -> p h d", d=64), S32[:].rearrange("p (h d) -> p h d", d=64),
             etot[:].unsqueeze(2).to_broadcast([128, 32, 64]), ALU.mult, reads=[S32, etot])
        for g in range(4):
            po = pG[g % 2]
            P.mm(po, po[:], B_[:, g * 128:(g + 1) * 128], xw[:, g * 512:(g + 1) * 512], reads=[B_, xw])
            P.tt("dve", S32, S32[:, g * 512:(g + 1) * 512], S32[:, g * 512:(g + 1) * 512], po[:], ALU.add, reads=[S32, po])
        P.cp("act", Sb, Sb[:], S32[:], reads=[S32])
    trih = np.triu(np.ones((128, 128), np.float32))
    selh = np.zeros((32, 32, 128), np.float32)
    for h in range(32):
        selh[h, h, :] = 1.0
    maps = []
    for core in range(NCORES):
        maps.append({"x": np.ascontiguousarray(xs_tm[core]), "dt": c_(dt_tm[core].reshape(NCH, 128, 32).transpose(1, 0, 2)),
                     "B": np.ascontiguousarray(B_tm[core]), "BT": np.ascontiguousarray(BT[core]), "CT": np.ascontiguousarray(CT[core]),
                     "al": c_(np.broadcast_to(alog[core], (128, 32))), "tri": trih, "sel": selh})
    res = run_spmd(P, maps)
    return [r["o_y"] for r in res]


def run_layer1(inp, mod, x2, c2, wb):
    res = launch_pre1(x2, c2, mod[1], inp["ssd_w_in"][0], inp["ssd_conv_w"][0], inp["ssd_conv_b"][0],
                      inp["ssd_dt_bias_f"][0], inp["ssd_dt_bias_b"][0])
    xbc = assemble_seq(res, "o_xbc")
    dtT = assemble_seq(res, "o_dt")
    xs_tm, dt_tm, B_tm, BT, CT, alog = [], [], [], [], [], []
    for core in range(NCORES):
        b, d_ = core // 2, core % 2
        if d_ == 0:
            order = np.arange(NK)
        else:
            order = np.concatenate([np.arange(CTX)[::-1], CTX + np.arange(SEQ)[::-1]])
        xo = xbc[b][:, order]
        xs_tm.append(xo[:2048].T); B_tm.append(xo[2048:2560].T); BT.append(xo[2048:2560]); CT.append(xo[2560:3072])
        dt_tm.append(dtT[b][32 * d_:32 * (d_ + 1)][:, order].T)
        alog.append(inp["ssd_a_log_f"][0] if d_ == 0 else inp["ssd_a_log_b"][0])
    ys = launch_scan(xs_tm, dt_tm, B_tm, BT, CT, alog)
    feats, xres, vecs = [], [], []
    l = 1
    for core in range(NCORES):
        b, s = core // 2, core % 2
        ls = slice(s * 4096, (s + 1) * 4096)
        yf = ys[2 * b][ls]; ybk = ys[2 * b + 1][::-1][ls]
        feats.append({"yfT": np.ascontiguousarray(yf.T), "ybT": np.ascontiguousarray(ybk.T),
                      "xsT": np.ascontiguousarray(res[core]["o_xbc"][:2048, 128:]),
                      "zzT": np.ascontiguousarray(res[core]["o_z"][:, 128:])})
        xres.append(x2[b][ls])
        vecs.append(_vecs(mod[1], b)[:, :, 1:2, :])
    wgu, wd = wb[l]
    dfull = np.repeat(inp["ssd_d"][0], 64)
    extra = {"dg": c_(np.stack([dfull.reshape(16, 128).T, inp["ssd_norm_g"][0].reshape(16, 128).T], -1))}
    outs = launch_post(1, 32, feats, xres, vecs, [inp["ln_mix_g"][l], inp["ln_mix_b"][l], inp["ln_ffn_g"][l], inp["ln_ffn_b"][l]],
                       inp["ssd_w_out"][0], inp["router_w"][l], inp["router_bias"][l], wgu, wd, extra)
    out = np.zeros((BATCH, SEQ, D), np.float32)
    for core in range(NCORES):
        b, s = core // 2, core % 2
        out[b, s * 4096:(s + 1) * 4096] = outs[core]
    return out


def kernel(**inp):
    inp = {k: np.asarray(v) for k, v in inp.items()}
    wgu_l = [np.concatenate([inp["exp_w_gu"][l], inp["sh_w_gu"][l][None]], 0) for l in range(DEPTH)]
    wd_l = [np.concatenate([inp["exp_w_down"][l], inp["sh_w_down"][l][None]], 0) for l in range(DEPTH)]
    mod, wb = launch_mod(inp["c"], inp["c_ctx"], inp["mod_w"], inp["mod_b"], wgu_l, wd_l)
    del wgu_l, wd_l
    Ls = launch_filt(*[inp[k][0] for k in ["hy_filt_w1", "hy_filt_b1", "hy_filt_freq1", "hy_filt_w2", "hy_filt_b2",
                                           "hy_filt_freq2", "hy_filt_w3"]])
    x2, c2 = run_layer0(inp, mod, Ls, wb)
    return run_layer1(inp, mod, x2, c2, wb)
```
